# Optimizing a Trainium2 kernel written in Bass

```python
import math
import jax, jax.numpy as jnp
from jax import lax
import numpy as np

D_MODEL = 1024
BATCH = 8
SEQ = 2048
DEPTH = 2

D_MIX = 2 * D_MODEL
EPS = 1e-6
MLSTM_HEADS = 4
MLSTM_DV = D_MIX // 2
MLSTM_DV_HEAD = MLSTM_DV // MLSTM_HEADS
MLSTM_DK_HEAD = MLSTM_DV_HEAD // 2
MLSTM_DK = MLSTM_HEADS * MLSTM_DK_HEAD
MLSTM_CHUNK = 64
CONV_WIDTH = 4
S5_WIDTH = D_MIX // 2
S5_GROUP = 16
S5_GROUPS = S5_WIDTH // S5_GROUP
S5_STATE = 64
S5_DT_MIN = 1e-3
S5_DT_MAX = 1e-1
GLA_HEADS = 4
GLA_DV = D_MIX
GLA_DK = D_MIX // 2
GLA_DV_HEAD = GLA_DV // GLA_HEADS
GLA_DK_HEAD = GLA_DK // GLA_HEADS
GLA_GATE_RANK = 16
GLA_TAU = 16.0
GLA_CHUNK = 64
EVEN_SIZES = (MLSTM_DK, MLSTM_DK, MLSTM_DV, MLSTM_DV, MLSTM_HEADS, MLSTM_HEADS, S5_WIDTH, D_MIX)
EVEN_IN = sum(EVEN_SIZES)
ODD_SIZES = (GLA_DK, GLA_DK, GLA_DV, D_MIX, GLA_GATE_RANK)
ODD_IN = sum(ODD_SIZES)
N_EVEN = (DEPTH + 1) // 2
N_ODD = DEPTH // 2

kernel_name = "hybrid_mlstm_s5_gla_trunk"


def _split(t, sizes):
    idx = [int(v) for v in np.cumsum(sizes)[:-1]]
    return jnp.split(t, idx, axis=-1)


def rmsnorm(x, g):
    xf = x.astype(jnp.float32)
    y = xf * lax.rsqrt(jnp.mean(xf * xf, axis=-1, keepdims=True) + EPS)
    return (y * g.astype(jnp.float32)).astype(x.dtype)


def headwise_rmsnorm(h, g, n_heads):
    b, s, w = h.shape
    hf = h.astype(jnp.float32).reshape(b, s, n_heads, w // n_heads)
    hf = hf * lax.rsqrt(jnp.mean(hf * hf, axis=-1, keepdims=True) + EPS)
    return hf.reshape(b, s, w) * g.astype(jnp.float32)


def causal_dwconv(x, w, bias):
    c = x.shape[-1]
    y = lax.conv_general_dilated(
        x.astype(jnp.float32), w.astype(jnp.float32)[:, None, :],
        window_strides=(1,), padding=[(CONV_WIDTH - 1, 0)],
        dimension_numbers=('NWC', 'WIO', 'NWC'), feature_group_count=c)
    return y + bias.astype(jnp.float32)


def mlstm_chunkwise(q, k, v, i_pre, f_pre):
    b_, s_, h_, dk = q.shape
    dv = v.shape[-1]
    L = MLSTM_CHUNK
    nc = s_ // L
    q = q * (dk ** -0.5)
    logf = jax.nn.log_sigmoid(f_pre)
    qc = q.reshape(b_, nc, L, h_, dk)
    kc = k.reshape(b_, nc, L, h_, dk)
    vc = v.reshape(b_, nc, L, h_, dv)
    ic = i_pre.reshape(b_, nc, L, h_)
    bcum = jnp.cumsum(logf.reshape(b_, nc, L, h_), axis=2)
    g = bcum[:, :, -1]
    causal = jnp.tril(jnp.ones((L, L), dtype=bool))
    dmat = bcum[:, :, :, None, :] - bcum[:, :, None, :, :] + ic[:, :, None, :, :]
    dmat = jnp.where(causal[None, None, :, :, None], dmat, -jnp.inf)
    a = g[:, :, None, :] - bcum + ic
    m_loc = jnp.max(a, axis=2)
    w_loc = jnp.exp(a - m_loc[:, :, None, :])
    c_loc = jnp.einsum('bclh,bclhk,bclhv->bchkv', w_loc, kc, vc)
    n_loc = jnp.einsum('bclh,bclhk->bchk', w_loc, kc)

    def step(carry, inp):
        c_st, n_st, m_st = carry
        g_c, m_l, c_l, n_l = inp
        m_new = jnp.maximum(g_c + m_st, m_l)
        s_prev = jnp.exp(g_c + m_st - m_new)
        s_loc = jnp.exp(m_l - m_new)
        c_new = s_prev[..., None, None] * c_st + s_loc[..., None, None] * c_l
        n_new = s_prev[..., None] * n_st + s_loc[..., None] * n_l
        return (c_new, n_new, m_new), (c_st, n_st, m_st)

    init = (jnp.zeros((b_, h_, dk, dv), jnp.float32),
            jnp.zeros((b_, h_, dk), jnp.float32),
            jnp.zeros((b_, h_), jnp.float32))
    xs = (jnp.moveaxis(g, 1, 0), jnp.moveaxis(m_loc, 1, 0),
          jnp.moveaxis(c_loc, 1, 0), jnp.moveaxis(n_loc, 1, 0))
    _, (c_prev, n_prev, m_prev) = lax.scan(step, init, xs)
    c_prev = jnp.moveaxis(c_prev, 0, 1)
    n_prev = jnp.moveaxis(n_prev, 0, 1)
    m_prev = jnp.moveaxis(m_prev, 0, 1)
    inter_log = bcum + m_prev[:, :, None, :]
    m_t = jnp.maximum(inter_log, jnp.max(dmat, axis=3))
    w_intra = jnp.exp(dmat - m_t[:, :, :, None, :])
    w_inter = jnp.exp(inter_log - m_t)
    sc = w_intra * jnp.einsum('bcthk,bcshk->bctsh', qc, kc)
    num = (jnp.einsum('bctsh,bcshv->bcthv', sc, vc)
           + w_inter[..., None] * jnp.einsum('bcthk,bchkv->bcthv', qc, c_prev))
    den = jnp.sum(sc, axis=3) + w_inter * jnp.einsum('bcthk,bchk->bcth', qc, n_prev)
    h = num / jnp.maximum(jnp.abs(den), jnp.exp(-m_t))[..., None]
    return h.reshape(b_, s_, h_, dv)


def s5_branch(u, lam_re, lam_im, log_dt, b_re, b_im, c_re, c_im, d_skip, glu_w, glu_b):
    f32 = jnp.float32
    b_, s_, _ = u.shape
    uf = u.astype(f32)
    lam = lax.complex(lam_re.astype(f32), lam_im.astype(f32))
    dt = jnp.exp(log_dt.astype(f32))[:, None]
    a_bar = jnp.exp(lam * dt)
    b_bar = lax.complex(b_re.astype(f32), b_im.astype(f32)) * ((a_bar - 1.0) / lam)[..., None]
    ug = uf.reshape(b_, s_, S5_GROUPS, S5_GROUP)
    bu = jnp.einsum('bsgc,gpc->bsgp', ug, b_bar)
    a_seq = jnp.broadcast_to(a_bar, (1, s_, S5_GROUPS, S5_STATE))

    def combine(e1, e2):
        a1, x1 = e1
        a2, x2 = e2
        return a2 * a1, a2 * x1 + x2

    _, states = lax.associative_scan(combine, (a_seq, bu), axis=1)
    y = (jnp.einsum('bsgp,gcp->bsgc', jnp.real(states), c_re.astype(f32))
         - jnp.einsum('bsgp,gcp->bsgc', jnp.imag(states), c_im.astype(f32)))
    y = y.reshape(b_, s_, S5_WIDTH) + d_skip.astype(f32) * uf
    y = jax.nn.gelu(y)
    return y * jax.nn.sigmoid(y @ glu_w.astype(f32) + glu_b.astype(f32))


def gla_chunkwise(q, k, v, log_alpha):
    b_, s_, h_, dk = q.shape
    dv = v.shape[-1]
    L = GLA_CHUNK
    nc = s_ // L
    q = q * (dk ** -0.5)
    qc = q.reshape(b_, nc, L, h_, dk)
    kc = k.reshape(b_, nc, L, h_, dk)
    vc = v.reshape(b_, nc, L, h_, dv)
    bcum = jnp.cumsum(log_alpha.reshape(b_, nc, L, h_, dk), axis=2)
    g = bcum[:, :, -1]
    q_t = qc * jnp.exp(bcum)
    k_t = kc * jnp.exp(-bcum)
    causal = jnp.tril(jnp.ones((L, L), dtype=bool))
    attn = jnp.einsum('bcthk,bcshk->bchts', q_t, k_t)
    attn = jnp.where(causal[None, None, None], attn, 0.0)
    o_intra = jnp.einsum('bchts,bcshv->bcthv', attn, vc)
    k_end = kc * jnp.exp(g[:, :, None] - bcum)
    upd = jnp.einsum('bcshk,bcshv->bchkv', k_end, vc)

    def step(st, inp):
        g_c, u_c = inp
        return jnp.exp(g_c)[..., None] * st + u_c, st

    _, s_prev = lax.scan(step, jnp.zeros((b_, h_, dk, dv), jnp.float32),
                         (jnp.moveaxis(g, 1, 0), jnp.moveaxis(upd, 1, 0)))
    s_prev = jnp.moveaxis(s_prev, 0, 1)
    o_inter = jnp.einsum('bcthk,bchkv->bcthv', q_t, s_prev)
    return (o_intra + o_inter).reshape(b_, s_, h_, dv)


def even_layer(h, w_in, conv_w, conv_b, i_bias, f_bias, head_g,
               lam_re, lam_im, log_dt, b_re, b_im, c_re, c_im, d_skip, glu_w, glu_b, w_out):
    f32 = jnp.float32
    b_, s_, _ = h.shape
    proj = h @ w_in
    q, k, v, o, i_pre, f_pre, u, z = _split(proj, EVEN_SIZES)
    qk = jax.nn.silu(causal_dwconv(jnp.concatenate([q, k], axis=-1), conv_w, conv_b))
    q, k = qk[..., :MLSTM_DK], qk[..., MLSTM_DK:]
    hd = lambda t, dh: t.astype(f32).reshape(b_, s_, MLSTM_HEADS, dh)
    h_a = mlstm_chunkwise(hd(q, MLSTM_DK_HEAD), hd(k, MLSTM_DK_HEAD), hd(v, MLSTM_DV_HEAD),
                          i_pre.astype(f32) + i_bias.astype(f32),
                          f_pre.astype(f32) + f_bias.astype(f32))
    h_a = jax.nn.sigmoid(o.astype(f32)) * h_a.reshape(b_, s_, MLSTM_DV)
    h_a = headwise_rmsnorm(h_a, head_g, MLSTM_HEADS)
    h_b = s5_branch(u, lam_re, lam_im, log_dt, b_re, b_im, c_re, c_im, d_skip, glu_w, glu_b)
    y = jnp.concatenate([h_a, h_b], axis=-1) * jax.nn.silu(z.astype(f32))
    return y.astype(h.dtype) @ w_out


def odd_layer(h, w_in, w_alpha, b_alpha, head_g, w_out):
    f32 = jnp.float32
    b_, s_, _ = h.shape
    proj = h @ w_in
    q, k, v, z, r = _split(proj, ODD_SIZES)
    log_alpha = jax.nn.log_sigmoid(r.astype(f32) @ w_alpha.astype(f32)
                                   + b_alpha.astype(f32)) / GLA_TAU
    hd = lambda t, dh: t.astype(f32).reshape(b_, s_, GLA_HEADS, dh)
    o_c = gla_chunkwise(hd(q, GLA_DK_HEAD), hd(k, GLA_DK_HEAD), hd(v, GLA_DV_HEAD),
                        hd(log_alpha, GLA_DK_HEAD))
    o_c = headwise_rmsnorm(o_c.reshape(b_, s_, GLA_DV), head_g, GLA_HEADS)
    y = o_c * jax.nn.silu(z.astype(f32))
    return y.astype(h.dtype) @ w_out


def setup_inputs(seed: int = 0) -> dict:
    key = jax.random.key(seed)
    ks = jax.random.split(key, 26)
    f32 = jnp.float32

    def nrm(k, shape, scale):
        return scale * jax.random.normal(k, shape, f32)

    ne, no = N_EVEN, N_ODD
    gp = (ne, S5_GROUPS, S5_STATE)
    n_idx = jnp.arange(S5_STATE, dtype=f32)
    return {
        "x": nrm(ks[0], (BATCH, SEQ, D_MODEL), 1.0),
        "norm_g": 1.0 + nrm(ks[1], (DEPTH, D_MODEL), 0.01),
        "final_norm_g": 1.0 + nrm(ks[2], (D_MODEL,), 0.01),
        "ev_w_in": nrm(ks[3], (ne, D_MODEL, EVEN_IN), D_MODEL ** -0.5),
        "ev_conv_w": nrm(ks[4], (ne, CONV_WIDTH, 2 * MLSTM_DK), CONV_WIDTH ** -0.5),
        "ev_conv_b": nrm(ks[5], (ne, 2 * MLSTM_DK), 0.01),
        "ev_i_bias": nrm(ks[6], (ne, MLSTM_HEADS), 0.1),
        "ev_f_bias": jnp.linspace(3.0, 6.0, MLSTM_HEADS, dtype=f32) + nrm(ks[7], (ne, MLSTM_HEADS), 0.1),
        "ev_head_g": 1.0 + nrm(ks[8], (ne, MLSTM_DV), 0.01),
        "s5_lam_re": -0.5 + nrm(ks[9], gp, 0.01),
        "s5_lam_im": jnp.pi * n_idx + nrm(ks[10], gp, 0.01),
        "s5_log_dt": jax.random.uniform(ks[11], (ne, S5_GROUPS), f32,
                                        math.log(S5_DT_MIN), math.log(S5_DT_MAX)),
        "s5_b_re": nrm(ks[12], (ne, S5_GROUPS, S5_STATE, S5_GROUP), (2 * S5_GROUP) ** -0.5),
        "s5_b_im": nrm(ks[13], (ne, S5_GROUPS, S5_STATE, S5_GROUP), (2 * S5_GROUP) ** -0.5),
        "s5_c_re": nrm(ks[14], (ne, S5_GROUPS, S5_GROUP, S5_STATE), (2 * S5_STATE) ** -0.5),
        "s5_c_im": nrm(ks[15], (ne, S5_GROUPS, S5_GROUP, S5_STATE), (2 * S5_STATE) ** -0.5),
        "s5_d": nrm(ks[16], (ne, S5_WIDTH), 0.5),
        "s5_glu_w": nrm(ks[17], (ne, S5_WIDTH, S5_WIDTH), S5_WIDTH ** -0.5),
        "s5_glu_b": nrm(ks[18], (ne, S5_WIDTH), 0.01),
        "ev_w_out": nrm(ks[19], (ne, D_MIX, D_MODEL), D_MIX ** -0.5),
        "od_w_in": nrm(ks[20], (no, D_MODEL, ODD_IN), D_MODEL ** -0.5),
        "gla_w_alpha": nrm(ks[21], (no, GLA_GATE_RANK, GLA_DK), GLA_GATE_RANK ** -0.5),
        "gla_b_alpha": nrm(ks[22], (no, GLA_DK), 0.1),
        "gla_head_g": 1.0 + nrm(ks[23], (no, GLA_DV), 0.01),
        "od_w_out": nrm(ks[24], (no, D_MIX, D_MODEL), D_MIX ** -0.5),
    }


def reference(x, norm_g, final_norm_g, ev_w_in, ev_conv_w, ev_conv_b, ev_i_bias, ev_f_bias,
              ev_head_g, s5_lam_re, s5_lam_im, s5_log_dt, s5_b_re, s5_b_im, s5_c_re, s5_c_im,
              s5_d, s5_glu_w, s5_glu_b, ev_w_out, od_w_in, gla_w_alpha, gla_b_alpha,
              gla_head_g, od_w_out):
    for layer in range(DEPTH):
        hn = rmsnorm(x, norm_g[layer])
        j = layer // 2
        if layer % 2 == 0:
            y = even_layer(hn, ev_w_in[j], ev_conv_w[j], ev_conv_b[j], ev_i_bias[j], ev_f_bias[j],
                           ev_head_g[j], s5_lam_re[j], s5_lam_im[j], s5_log_dt[j], s5_b_re[j],
                           s5_b_im[j], s5_c_re[j], s5_c_im[j], s5_d[j], s5_glu_w[j], s5_glu_b[j],
                           ev_w_out[j])
        else:
            y = odd_layer(hn, od_w_in[j], gla_w_alpha[j], gla_b_alpha[j], gla_head_g[j], od_w_out[j])
        x = x + y.astype(x.dtype)
    return rmsnorm(x, final_norm_g)
```

```python
import math
from contextlib import ExitStack

import numpy as np
import ml_dtypes

import concourse.bass as bass
import concourse.mybir as mybir
from concourse.bass_utils import run_bass_kernel_spmd

F32 = mybir.dt.float32
BF16 = mybir.dt.bfloat16
ALU = mybir.AluOpType
AF = mybir.ActivationFunctionType
AX = mybir.AxisListType

D = 1024
S = 2048
EPS = 1e-6

STRICT = True
COMPUTE = ("pe", "act", "dve", "pool")
ENGS = ("pe", "act", "dve", "pool", "sp")


class Op:
    __slots__ = ("eng", "fn", "waits", "signal", "idx", "dsem", "dval", "cnt", "phase")

    def __init__(self, eng, fn):
        self.eng = eng
        self.fn = fn
        self.waits = []
        self.signal = False
        self.idx = -1
        self.dsem = None
        self.dval = 0
        self.cnt = 0
        self.phase = 0


class Prog:
    def __init__(self, nc, stack):
        self.nc = nc
        self.stack = stack
        self.streams = {e: [] for e in ENGS}
        self.esem = {e: stack.enter_context(nc.semaphore("sem_" + e)) for e in COMPUTE}
        self.base = {e: 0 for e in COMPUTE}
        self.nidx = {e: 0 for e in ENGS}
        self.waited = {e: {} for e in ENGS}
        self.state = {}
        self.dsems = {}
        self.phase = 0
        self.n_ops = 0

    def _add_wait(self, o, d, raw):
        if d is o or d.phase < self.phase:
            return
        if d.dsem is None and d.eng == o.eng and not raw and (not STRICT or o.eng == "pe"):
            return
        if d.dsem is not None:
            key, val = ("d", d.dsem), d.dval
        else:
            key, val = d.eng, d.idx
        w = self.waited[o.eng]
        if w.get(key, -1) >= val:
            return
        w[key] = val
        d.signal = True
        o.waits.append(d)

    def op(self, eng, fn, reads=(), writes=(), dma=None):
        o = Op(eng, fn)
        o.phase = self.phase
        o.idx = self.nidx[eng]
        self.nidx[eng] += 1
        self.streams[eng].append(o)
        self.n_ops += 1
        for k in reads:
            st = self.state.get(k)
            if st is not None:
                if st[0] is not None:
                    self._add_wait(o, st[0], True)
                if isinstance(k, tuple) and k[0] in ("psf", "psb"):
                    for r in st[1].values():
                        self._add_wait(o, r, False)
        for k in writes:
            st = self.state.get(k)
            if st is not None:
                if st[0] is not None:
                    self._add_wait(o, st[0], False)
                for r in st[1].values():
                    self._add_wait(o, r, False)
                for r in st[2]:
                    self._add_wait(o, r, False)
        if dma is not None:
            ds = self.dsems.get(dma)
            if ds is None:
                ds = [self.stack.enter_context(self.nc.semaphore("dsem_%d" % len(self.dsems))), 0]
                self.dsems[dma] = ds
            ds[1] += 16
            o.dsem = dma
            o.dval = ds[1]
            o.signal = True
        for k in reads:
            st = self.state.get(k)
            if st is None:
                st = [None, {}, []]
                self.state[k] = st
            if o.dsem is not None:
                st[2].append(o)
            else:
                st[1][eng] = o
        for k in writes:
            self.state[k] = [o, {}, []]
        return o

    def _emit_stream(self, name, eng):
        for o in self.streams[name]:
            for d in o.waits:
                if d.dsem is not None:
                    eng.wait_ge(self.dsems[d.dsem][0], d.dval)
                else:
                    eng.wait_ge(self.esem[d.eng], d.cnt)
            if o.fn is None:
                continue
            bi = o.fn(eng)
            if o.dsem is not None:
                bi.then_inc(self.dsems[o.dsem][0], 16)
            elif o.signal:
                bi.then_inc(self.esem[o.eng], 1)
        if name == "sp":
            for sem, tot in self.dsems.values():
                if tot > 0:
                    eng.wait_ge(sem, tot)

    def emit_phase(self):
        for e in COMPUTE:
            c = self.base[e]
            for o in self.streams[e]:
                if o.signal and o.dsem is None:
                    assert o.fn is not None
                    c += 1
                o.cnt = c
            self.base[e] = c
        with self.nc.Block() as block:
            @block.tensor
            def _(eng):
                self._emit_stream("pe", eng)

            @block.scalar
            def _(eng):
                self._emit_stream("act", eng)

            @block.vector
            def _(eng):
                self._emit_stream("dve", eng)

            @block.gpsimd
            def _(eng):
                self._emit_stream("pool", eng)

            @block.sync
            def _(eng):
                self._emit_stream("sp", eng)
        self.streams = {e: [] for e in ENGS}
        self.phase += 1


class Ctx:
    def __init__(self, nc, stack):
        self.nc = nc
        self.stack = stack
        self.P = Prog(nc, stack)

    def sb(self, name, shape, dt, st=None):
        self.nuniq = getattr(self, "nuniq", 0) + 1
        return (st or self.stack).enter_context(self.nc.sbuf_tensor("sb%d_%s" % (self.nuniq, name), list(shape), dt))

    def ps(self, name, shape, dt):
        return self.stack.enter_context(self.nc.psum_tensor(name, list(shape), dt))

    def dram_in(self, name, shape, dt):
        return self.nc.dram_tensor(name, list(shape), dt, kind="ExternalInput").ap()

    def dram_out(self, name, shape, dt):
        return self.nc.dram_tensor(name, list(shape), dt, kind="ExternalOutput").ap()

    def dram_scr(self, name, shape, dt):
        return self.nc.dram_tensor(name, list(shape), dt).ap()

    def mm(self, out, lhsT, rhs, start, stop, reads, pkey):
        self.P.op("pe", lambda e: e.matmul(out=out, lhsT=lhsT, rhs=rhs, start=start, stop=stop),
                  reads=reads, writes=[pkey])

    def tr(self, out, in_, ident, reads, pkey):
        self.P.op("pe", lambda e: e.transpose(out=out, in_=in_, identity=ident), reads=reads, writes=[pkey])

    def act(self, out, in_, func, reads, writes, bias=None, scale=None, accum_out=None, eng="act"):
        kw = {}
        if bias is not None:
            kw["bias"] = bias
        if scale is not None:
            kw["scale"] = scale
        if accum_out is not None:
            kw["accum_out"] = accum_out
        self.P.op(eng, lambda e: e.activation(out=out, in_=in_, func=func, **kw), reads=reads, writes=writes)

    def tt(self, eng, out, in0, in1, op, reads, writes):
        self.P.op(eng, lambda e: e.tensor_tensor(out=out, in0=in0, in1=in1, op=op), reads=reads, writes=writes)

    def ts(self, eng, out, in0, s1, s2, op0, op1, reads, writes):
        if s2 is None:
            self.P.op(eng, lambda e: e.tensor_scalar(out=out, in0=in0, scalar1=s1, scalar2=None, op0=op0),
                      reads=reads, writes=writes)
        else:
            self.P.op(eng, lambda e: e.tensor_scalar(out=out, in0=in0, scalar1=s1, scalar2=s2, op0=op0, op1=op1),
                      reads=reads, writes=writes)

    def stt(self, out, in0, scalar, in1, op0, op1, reads, writes):
        self.P.op("dve", lambda e: e.scalar_tensor_tensor(out=out, in0=in0, scalar=scalar, in1=in1,
                                                           op0=op0, op1=op1), reads=reads, writes=writes)

    def copy(self, eng, out, in_, reads, writes):
        if eng == "act":
            self.P.op("act", lambda e: e.activation(out=out, in_=in_, func=AF.Copy), reads=reads, writes=writes)
        else:
            self.P.op(eng, lambda e: e.tensor_copy(out=out, in_=in_), reads=reads, writes=writes)

    def memset(self, eng, ap, val, writes):
        self.P.op(eng, lambda e: e.memset(ap, val), writes=writes)

    def dma(self, q, out, in_, reads, writes, key):
        self.P.op(q, lambda e: e.dma_start(out=out, in_=in_), reads=reads, writes=writes, dma=key)


def setup_globals(C, cin):
    P = C.P
    C.x_sb = C.sb("x_sb", [128, 16, D], F32)
    C.hnT = C.sb("hnT", [128, 8, S], BF16)
    C.ident = C.sb("ident_bf", [128, 128], BF16)
    C.identf = C.sb("ident_f", [128, 128], F32)
    C.tri = C.sb("tri_f", [128, 128], F32)
    C.trin = C.sb("trin_f", [128, 128], F32)
    C.trin16 = C.sb("trin16_f", [128, 128], F32)
    C.ones = C.sb("ones_f", [128, 128], F32)
    C.cb = C.sb("cbias", [128, 4], F32)
    C.memset("dve", C.cb[:, 0:1], math.log(1.0 / 16.0), ["const"])
    C.memset("dve", C.cb[:, 1:2], 1.0, ["const"])
    C.psf = [C.ps("psf%d" % i, [128, 512], F32) for i in range(6)]
    C.psb = [C.ps("psb%d" % i, [128, 1024], BF16) for i in range(2)]
    for t, nm in ((C.ident, "ident_bf"), (C.identf, "ident_f"), (C.tri, "tri_f"), (C.trin, "trin_f"),
                  (C.trin16, "trin16_f"), (C.ones, "ones_f")):
        C.dma("sp", t[:], cin[nm], [], ["const"], "const")


def phase_load_x(C, x_d):
    xv = x_d.rearrange("(c s) d -> c s d", s=16)
    for q in range(4):
        C.dma("sp", C.x_sb[:, 4 * q:4 * q + 4, :], xv[:, 4 * q:4 * q + 4, :], [], [("x", s) for s in range(4 * q, 4 * q + 4)],
              ("x", q))


def rms_stats(C, st, tag):
    junk = C.sb("junk_" + tag, [128, D], BF16, st)
    ssq = C.sb("ssq_" + tag, [128, 16], F32, st)
    rstd = C.sb("rstd_" + tag, [128, 16], F32, st)
    for s in range(16):
        C.act(junk[:], C.x_sb[:, s, :], AF.Square, [("x", s)], ["junk", ("ssq", s)], accum_out=ssq[:, s:s + 1])
    C.ts("dve", rstd[:], ssq[:], 1.0 / D, EPS, ALU.mult, ALU.add, [("ssq", s) for s in range(16)], ["rstd"])
    C.act(rstd[:], rstd[:], AF.Sqrt, ["rstd"], ["rstd"])
    C.P.op("dve", lambda e: e.reciprocal(out=rstd[:], in_=rstd[:]), reads=["rstd"], writes=["rstd"])
    return rstd


def phase_norm(C, g_d, tag):
    st = ExitStack()
    with st:
        g_sb = C.sb("g_" + tag, [128, D], F32, st)
        hn = [C.sb("hn%d_%s" % (i, tag), [128, D], BF16, st) for i in range(3)]
        C.dma("sp", g_sb[:], g_d, [], ["g"], "g")
        rstd = rms_stats(C, st, tag)
        def mk_hn(s):
            C.stt(hn[s % 3][:], C.x_sb[:, s, :], rstd[:, s:s + 1], g_sb[:], ALU.mult, ALU.mult,
                  [("x", s), "rstd", "g"], [("hn", s % 3)])

        mk_hn(0)
        mk_hn(1)
        for s in range(16):
            h = hn[s % 3]
            hk = ("hn", s % 3)
            pt = C.psb[s % 2]
            pk = ("psb", s % 2)
            for j in range(8):
                C.tr(pt[:, 128 * j:128 * j + 128], h[:, 128 * j:128 * j + 128], C.ident[:], [hk, "const"], pk)
            if s + 2 < 16:
                mk_hn(s + 2)
            dst = C.hnT[:].rearrange("p j (c s) -> p j c s", s=16)[:, :, :, s]
            src = pt[:].rearrange("p (j c) -> p j c", j=8)
            C.copy("act" if s % 2 else "dve", dst, src, [pk], [("hnT", s)])
        C.P.emit_phase()


def phase_final(C, g_d, out_d):
    st = ExitStack()
    with st:
        g_sb = C.sb("g_fin", [128, D], F32, st)
        stage = [C.sb("ostage%d" % i, [128, 4, D], F32, st) for i in range(2)]
        C.dma("sp", g_sb[:], g_d, [], ["g"], "g")
        rstd = rms_stats(C, st, "fin")
        ov = out_d.rearrange("(c s) d -> c s d", s=16)
        for q in range(4):
            sg = stage[q % 2]
            for s4 in range(4):
                s = 4 * q + s4
                C.stt(sg[:, s4, :], C.x_sb[:, s, :], rstd[:, s:s + 1], g_sb[:], ALU.mult, ALU.mult,
                      [("x", s), "rstd", "g"], [("ostage", q % 2)])
            C.dma("sp", ov[:, 4 * q:4 * q + 4, :], sg[:], [("ostage", q % 2)], [], ("out", q % 2))
        C.P.emit_phase()


def run_interleaved(gens):
    gens = list(gens)
    while gens:
        for g in list(gens):
            try:
                next(g)
            except StopIteration:
                gens.remove(g)


def load_w(C, dst, w2d, f0, nf, key, rows0=0, nj=8):
    src = w2d[rows0:rows0 + 128 * nj, f0:f0 + nf].rearrange("(j p) f -> p j f", p=128)
    C.dma("pool", dst, src, [], [key], key)


def out_proj(C, yT, nfc, wo, ykey, wkey):
    n = 0
    for s in range(16):
        for dh in range(2):
            pm = C.psf[n % 2]
            pk = ("psf", n % 2)
            n += 1
            yv = yT[:, :, :].rearrange("p f (c s) -> p f c s", s=16)
            for fc in range(nfc):
                C.mm(pm[:], yv[:, fc, :, s], wo[:, fc, 512 * dh:512 * dh + 512], fc == 0, fc == nfc - 1,
                     [ykey, wkey], pk)
            xs = C.x_sb[:, s, 512 * dh:512 * dh + 512]
            C.tt("dve", xs, pm[:], xs, ALU.add, [pk, ("x", s)], [("x", s)])


GLA_Q0, GLA_K0, GLA_V0, GLA_Z0, GLA_R0 = 0, 1024, 2048, 4096, 6144


def phase_gla_prep(C, w_in, wab_d):
    st = ExitStack()
    with st:
        wr = C.sb("wr", [128, 8, 16], BF16, st)
        load_w(C, wr[:], w_in, GLA_R0, 16, "wr")
        C.memset("dve", C.rT1[:], 1.0, ["rT1"])
        C.dma("sp", C.wab[0:17, :], wab_d, [], ["wab"], "wab")
        for tg in range(4):
            pm = C.psf[tg % 2]
            pk = ("psf", tg % 2)
            for j in range(8):
                C.mm(pm[0:16, :], wr[:, j, :], C.hnT[:, j, 512 * tg:512 * tg + 512], j == 0, j == 7, ["wr", "hnT"], pk)
            C.copy("dve", C.rT1[0:16, 512 * tg:512 * tg + 512], pm[0:16, :], [pk, "rT1"], ["rT1"])
        C.P.emit_phase()


def phase_gla_head(C, h, w_in, hg_d, w_out):
    st = ExitStack()
    with st:
        try:
            _gla_head(C, st, h, w_in, hg_d, w_out)
        except StopPhase:
            pass
        C.P.emit_phase()


def _gla_head(C, st, h, w_in, hg_d, w_out):
    if True:
        wbuf = C.sb("g_wbuf", [128, 8, 512], BF16, st)
        wo = wbuf[:].rearrange("p (fc a) f -> p fc (a f)", a=2)
        wqk = C.wqk
        qT = C.sb("g_qT", [128, 2, S], BF16, st)
        kT = C.sb("g_kT", [128, 2, S], BF16, st)
        v_sb = C.sb("g_v", [128, 16, 512], BF16, st)
        zT = C.sb("g_zT", [128, 4, S], BF16, st)
        yT = C.sb("g_yT", [128, 4, S], BF16, st)
        hgc = C.sb("g_hgc", [128, 4], F32, st)
        Sf = C.sb("g_Sf", [128, 2, 512], F32, st)
        Sb = C.sb("g_Sb", [128, 2, 512], BF16, st)
        la = C.sb("g_la", [128, 256], F32, st)
        ep = C.sb("g_ep", [128, 128], F32, st)
        em = C.sb("g_em", [128, 128], F32, st)
        ee = C.sb("g_ee", [128, 128], F32, st)
        gcol = C.sb("g_gcol", [128, 2], F32, st)
        egc = C.sb("g_egc", [128, 2], F32, st)
        qs = C.sb("g_qs", [128, 2, 128], BF16, st)
        ks = C.sb("g_ks", [128, 2, 128], BF16, st)
        keT = C.sb("g_keT", [128, 2, 128], BF16, st)
        ke = C.sb("g_ke", [128, 256], BF16, st)
        attn = C.sb("g_attn", [128, 128], BF16, st)
        ssq = C.sb("g_ssq", [128, 2], F32, st)
        rstd = C.sb("g_rstd", [128, 2], F32, st)
        C.memset("dve", ssq[:], 1.0, ["g_ssq"])
        on = C.sb("g_on", [128, 512], BF16, st)
        junk = on

        C.dma("sp", hgc[:], hg_d[:, 4 * h:4 * h + 4], [], ["hgc"], "hgc")
        load_w(C, wbuf[:], w_in, GLA_V0 + 512 * h, 512, "g_wbuf")
        n = 0
        for which, dstT in ((0, qT), (1, kT)):
            for kt in range(2):
                for tg in range(4):
                    pm = C.psf[n % 4]
                    pk = ("psf", n % 4)
                    for j in range(8):
                        C.mm(pm[:], wqk[:, j, 256 * which + 128 * kt:256 * which + 128 * kt + 128],
                             C.hnT[:, j, 512 * tg:512 * tg + 512], j == 0, j == 7, ["g_wqk", "hnT"], pk)
                    C.copy("act" if n % 2 else "dve", dstT[:, kt, 512 * tg:512 * tg + 512], pm[:], [pk],
                           ["g_qT" if which == 0 else "g_kT"])
                    n += 1
        if h < 3:
            load_w(C, wqk[:, :, 0:256], w_in, GLA_Q0 + 256 * (h + 1), 256, "g_wqk")
            load_w(C, wqk[:, :, 256:512], w_in, GLA_K0 + 256 * (h + 1), 256, "g_wqk")
        for k in range(16):
            pm = C.psf[n % 4]
            pk = ("psf", n % 4)
            for j in range(8):
                C.mm(pm[:], C.hnT[:, j, 128 * k:128 * k + 128], wbuf[:, j, :], j == 0, j == 7, ["g_wbuf", "hnT"], pk)
            C.copy("act" if n % 2 else "dve", v_sb[:, k, :], pm[:], [pk], ["g_v"])
            n += 1
        load_w(C, wbuf[:], w_in, GLA_Z0 + 512 * h, 512, "g_wbuf")
        for fc in range(4):
            for tg in range(4):
                pm = C.psf[n % 4]
                pk = ("psf", n % 4)
                for j in range(8):
                    C.mm(pm[:], wbuf[:, j, 128 * fc:128 * fc + 128], C.hnT[:, j, 512 * tg:512 * tg + 512],
                         j == 0, j == 7, ["g_wbuf", "hnT"], pk)
                C.act(zT[:, fc, 512 * tg:512 * tg + 512], pm[:], AF.Silu, [pk], ["g_zT"])
                n += 1

        load_w(C, wo, w_out, 0, D, "g_wbuf", rows0=512 * h, nj=4)
        nchunk = {"proj": 0, "chunk1": 1, "chunk2": 2}.get(BISECT, 16)
        qs2 = [qs, C.sb("g_qs1", [128, 2, 128], BF16, st)]
        ke2 = [ke, C.sb("g_ke1", [128, 256], BF16, st)]
        attn2 = [attn, C.sb("g_attn1", [128, 128], BF16, st)]
        egc2 = [egc, C.sb("g_egc1", [128, 2], F32, st)]

        ep2 = [ep, ep]
        em2 = [em, C.sb("g_em1", [128, 128], F32, st)]
        ee2 = [ee, C.sb("g_ee1", [128, 128], F32, st)]

        def genA(k):
            b = k % 2
            tsl = slice(128 * k, 128 * k + 128)
            qsb, keb, attnb, egcb = qs2[b], ke2[b], attn2[b], egc2[b]
            p0, k0 = C.psf[0], ("psf", 0)
            C.mm(p0[:, 0:256], C.rT1[0:17, tsl], C.wab[0:17, 256 * h:256 * h + 256], True, True, ["rT1", "wab"], k0)
            yield
            C.act(la[:], p0[:, 0:256], AF.Exp, [k0], ["g_la"], scale=-1.0)
            yield
            C.act(la[:], la[:], AF.Ln, ["g_la", "const"], ["g_la"], bias=C.cb[:, 1:2])
            yield
            pk_ = [(C.psf[1], ("psf", 1)), (C.psf[2], ("psf", 2))]
            for kt in range(2):
                p1, k1 = pk_[kt]
                C.mm(p1[:, 0:128], la[:, 128 * kt:128 * kt + 128], C.trin16[:], True, True, ["g_la", "const"], k1)
            yield
            for kt in range(2):
                p1, k1 = pk_[kt]
                C.copy("dve", gcol[:, kt:kt + 1], p1[:, 127:128], [k1], [("g_gcol", kt)])
                C.act(em2[kt][:], p1[:, 0:128], AF.Exp, [k1], [("g_em", kt)], scale=-1.0)
            yield
            for kt in range(2):
                p1, k1 = pk_[kt]
                C.act(ep[:], p1[:, 0:128], AF.Exp, [k1, "const"], ["g_ep"], bias=C.cb[:, 0:1])
                C.tt("dve", qsb[:, kt, :], qT[:, kt, tsl], ep[:], ALU.mult, ["g_qT", "g_ep"], [("g_qs", kt, b)])
                yield
            for kt in range(2):
                p1, k1 = pk_[kt]
                C.act(ee2[kt][:], p1[:, 0:128], AF.Exp, [k1, ("g_gcol", kt)], [("g_ee", kt)], scale=-1.0,
                      bias=gcol[:, kt:kt + 1])
                C.tt("pool", ks[:, kt, :], kT[:, kt, tsl], em2[kt][:], ALU.mult, ["g_kT", ("g_em", kt)], [("g_ks", kt)])
            yield
            C.act(egcb[:, 0:2], gcol[:, 0:2], AF.Exp, [("g_gcol", 0), ("g_gcol", 1)], [("g_egc", 0, b), ("g_egc", 1, b)])
            for kt in range(2):
                C.tt("pool", keT[:, kt, :], kT[:, kt, tsl], ee2[kt][:], ALU.mult, ["g_kT", ("g_ee", kt)], [("g_keT", kt)])
            yield
            for kt in range(2):
                C.mm(p0[:, 0:128], ks[:, kt, :], qsb[:, kt, :], kt == 0, kt == 1, [("g_ks", kt), ("g_qs", kt, b)], k0)
            yield
            pb, kb = C.psb[0], ("psb", 0)
            for kt in range(2):
                C.tr(pb[:, 128 * kt:128 * kt + 128], keT[:, kt, :], C.ident[:], [("g_keT", kt), "const"], kb)
            C.tt("dve", attnb[:], p0[:, 0:128], C.tri[:], ALU.mult, [k0, "const"], [("g_attn", b)])
            yield
            C.copy("act", keb[:], pb[:, 0:256], [kb], [("g_ke", 0, b), ("g_ke", 1, b)])
            yield

        def genBs(k):
            b = k % 2
            keb, egcb = ke2[b], egc2[b]
            if k < 15:
                for kt in range(2):
                    p4, k4 = C.psf[4 + kt], ("psf", 4 + kt)
                    C.mm(p4[:], keb[:, 128 * kt:128 * kt + 128], v_sb[:, k, :], True, True, [("g_ke", kt, b), "g_v"], k4)
                yield
                for kt in range(2):
                    p4, k4 = C.psf[4 + kt], ("psf", 4 + kt)
                    if k == 0:
                        C.copy("dve", Sf[:, kt, :], p4[:], [k4], [("g_Sf", kt)])
                    else:
                        C.stt(Sf[:, kt, :], Sf[:, kt, :], egcb[:, kt:kt + 1], p4[:], ALU.mult, ALU.add,
                              [k4, ("g_egc", kt, b), ("g_Sf", kt)], [("g_Sf", kt)])
                    yield
                for kt in range(2):
                    C.copy("act", Sb2[(k + 1) % 2][:, kt, :], Sf[:, kt, :], [("g_Sf", kt)], [("g_Sb", kt, (k + 1) % 2)])
                    yield

        def genBo(k):
            b = k % 2
            tsl = slice(128 * k, 128 * k + 128)
            qsb, attnb = qs2[b], attn2[b]
            p3, k3 = C.psf[3], ("psf", 3)
            C.mm(p3[:], attnb[:], v_sb[:, k, :], True, k == 0, [("g_attn", b), "g_v"], k3)
            if k > 0:
                for kt in range(2):
                    C.mm(p3[:], qsb[:, kt, :], Sb2[b][:, kt, :], False, kt == 1, [("g_qs", kt, b), ("g_Sb", kt, b)], k3)
            yield
            C.act(junk[:], p3[:], AF.Square, [k3], ["g_on", "g_ssq"], accum_out=ssq[:, 0:1])
            yield
            C.ts("dve", rstd[:], ssq[:], 1.0 / 512, EPS, ALU.mult, ALU.add, ["g_ssq"], ["g_rstd"])
            yield
            C.act(rstd[:], rstd[:], AF.Sqrt, ["g_rstd"], ["g_rstd"])
            yield
            C.P.op("dve", lambda e: e.reciprocal(out=rstd[:], in_=rstd[:]), reads=["g_rstd"], writes=["g_rstd"])
            yield
            C.act(on[:], p3[:], AF.Copy, [k3, "g_rstd"], ["g_on"], scale=rstd[:, 0:1])
            yield
            pb, kb = C.psb[1], ("psb", 1)
            for fc in range(4):
                C.tr(pb[:, 128 * fc:128 * fc + 128], on[:, 128 * fc:128 * fc + 128], C.ident[:], ["g_on", "const"], kb)
            yield
            for fc in range(4):
                C.stt(yT[:, fc, tsl], pb[:, 128 * fc:128 * fc + 128], hgc[:, fc:fc + 1], zT[:, fc, tsl],
                      ALU.mult, ALU.mult, [kb, "hgc", "g_zT"], ["g_yT"])
                yield

        Sb2 = [Sb, C.sb("g_Sb1", [128, 2, 512], BF16, st)]
        if nchunk > 0:
            run_interleaved([genA(0)])
        for k in range(nchunk):
            gens = [genBs(k), genBo(k)]
            if k + 1 < nchunk:
                gens.insert(0, genA(k + 1))
            run_interleaved(gens)
        if BISECT in (None, "all1"):
            out_proj(C, yT, 4, wo, "g_yT", "g_wbuf")


BISECT = None


class StopPhase(Exception):
    pass


def cut(tag):
    if BISECT == tag:
        raise StopPhase()


def layer1(C, cin):
    C.rT1 = C.sb("rT1", [32, S], F32)
    C.wab = C.sb("wab", [32, 1024], F32)
    C.wqk = C.sb("g_wqk", [128, 8, 512], BF16)
    load_w(C, C.wqk[:, :, 0:256], cin["od_w_in"], GLA_Q0, 256, "g_wqk")
    load_w(C, C.wqk[:, :, 256:512], cin["od_w_in"], GLA_K0, 256, "g_wqk")
    phase_norm(C, cin["norm_g1"], "l1")
    if BISECT == "norm":
        return
    phase_gla_prep(C, cin["od_w_in"], cin["gla_wab"])
    if BISECT == "prep":
        return
    for h in range(4 if BISECT is None else 1):
        phase_gla_head(C, h, cin["od_w_in"], cin["gla_head_g"], cin["od_w_out"])


EV_Q0, EV_K0, EV_V0, EV_O0, EV_I0, EV_U0, EV_Z0 = 0, 512, 1024, 2048, 3072, 3080, 4104


def phase_ml_prep(C, w_in, bias_d):
    st = ExitStack()
    with st:
        wif = C.sb("wif", [128, 8, 8], BF16, st)
        bias = C.sb("ifbias", [128, 128], F32, st)
        G = C.sb("gatesG", [128, 16, 8], F32, st)
        load_w(C, wif[:], w_in, EV_I0, 8, "wif")
        C.dma("sp", bias[:], bias_d, [], ["ifbias"], "ifbias")
        pm, pk = C.psf[0], ("psf", 0)
        for k in range(16):
            for j in range(8):
                C.mm(pm[:, 8 * k:8 * k + 8], C.hnT[:, j, 128 * k:128 * k + 128], wif[:, j, :], j == 0, j == 7,
                     ["wif", "hnT"], pk)
        C.tt("dve", G[:].rearrange("p k g -> p (k g)"), pm[:, 0:128], bias[:], ALU.add, [pk, "ifbias"], ["gatesG"])
        lfv = C.lfn[:].rearrange("p (k h) -> p k h", h=4)
        C.act(lfv, G[:, :, 4:8], AF.Exp, ["gatesG"], ["lfn"], scale=-1.0)
        C.act(C.lfn[:], C.lfn[:], AF.Ln, ["lfn", "const"], ["lfn"], bias=C.cb[:, 1:2])
        p1, k1 = C.psf[1], ("psf", 1)
        C.mm(p1[:, 0:64], C.tri[:], C.lfn[:], True, True, ["lfn", "const"], k1)
        C.tt("dve", C.cbias[:].rearrange("p (k h) -> p k h", h=4), p1[:, 0:64].rearrange("p (k h) -> p k h", h=4),
             G[:, :, 0:4], ALU.add, [k1, "gatesG"], ["cbias"])
        C.P.emit_phase()


def phase_ml_head(C, h, w_in, w_out, cw_d, cb_d, hg_d):
    st = ExitStack()
    with st:
        try:
            _ml_head(C, st, h, w_in, w_out, cw_d, cb_d, hg_d)
        except StopPhase:
            pass
        C.P.emit_phase()


def _ml_head(C, st, h, w_in, w_out, cw_d, cb_d, hg_d):
    wq = C.sb("m_wq", [128, 8, 256], BF16, st)
    wvo = C.sb("m_wvo", [128, 8, 512], BF16, st)
    wz = C.sb("m_wz", [128, 8, 256], BF16, st)
    wo = C.sb("m_wo", [128, 2, D], BF16, st)
    cw = C.sb("m_cw", [128, 2, 4], F32, st)
    cbv = C.sb("m_cb", [128, 8], F32, st)
    hgc = C.sb("m_hgc", [128, 8], F32, st)
    pre = C.sb("m_pre", [128, S + 4], F32, st)
    acc = C.sb("m_acc", [128, S], F32, st)
    qT = C.sb("m_qT", [128, S], BF16, st)
    kT = C.sb("m_kT", [128, S], BF16, st)
    v_sb = C.sb("m_v", [128, 16, 258], BF16, st)
    o_sb = C.sb("m_o", [128, 16, 256], BF16, st)
    zT = C.sb("m_zT", [128, 2, S], BF16, st)
    yT = C.sb("m_yT", [128, 2, S], BF16, st)
    Cf = C.sb("m_Cf", [128, 258], F32, st)
    Cb = C.sb("m_Cb", [128, 258], BF16, st)
    lfbc = C.sb("m_lfbc", [128, 128], F32, st)
    eB = C.sb("m_eB", [128, 128], F32, st)
    w = C.sb("m_w", [128, 128], F32, st)
    wm = C.sb("m_wm", [128, 128], F32, st)
    sc = C.sb("m_sc", [128, 128], BF16, st)
    qs = C.sb("m_qs", [128, 128], BF16, st)
    vt = C.sb("m_vt", [128, 258], BF16, st)
    kTok = C.sb("m_kTok", [128, 128], BF16, st)
    hg = C.sb("m_hg", [128, 256], F32, st)
    hgn = C.sb("m_hgn", [128, 256], BF16, st)
    junk = C.sb("m_junk", [128, 256], BF16, st)
    ssq = C.sb("m_ssq", [128, 2], F32, st)
    rstd = C.sb("m_rstd", [128, 2], F32, st)
    r = C.sb("m_r", [128, 2], F32, st)
    gB = C.sb("m_gB", [128, 2], F32, st)
    wk4 = C.sb("m_wk4", [128, 4], F32, st)

    C.dma("sp", cw[:, 0, :], cw_d[128 * h:128 * h + 128, :], [], ["m_cw"], "m_cw")
    C.dma("sp", cw[:, 1, :], cw_d[512 + 128 * h:512 + 128 * h + 128, :], [], ["m_cw"], "m_cw")
    C.dma("sp", cbv[:], cb_d, [], ["m_cb"], "m_cb")
    C.dma("sp", hgc[:], hg_d, [], ["m_hgc"], "m_hgc")
    load_w(C, wq[:, :, 0:128], w_in, EV_Q0 + 128 * h, 128, "m_wq")
    load_w(C, wq[:, :, 128:256], w_in, EV_K0 + 128 * h, 128, "m_wq")
    load_w(C, wvo[:, :, 0:256], w_in, EV_V0 + 256 * h, 256, "m_wvo")
    load_w(C, wvo[:, :, 256:512], w_in, EV_O0 + 256 * h, 256, "m_wvo")
    load_w(C, wz[:], w_in, EV_Z0 + 256 * h, 256, "m_wz")
    load_w(C, wo[:], w_out, 0, D, "m_wo", rows0=256 * h, nj=2)
    C.memset("dve", ssq[:], 1.0, ["m_ssq"])
    C.memset("dve", pre[:, 0:4], 0.0, ["m_pre0"])
    C.memset("pool", v_sb[:, :, 256:258], 1.0, ["m_vones"])
    cut("mc_dma")

    n = 0
    for which in range(2):
        for tg in range(4):
            pm, pk = C.psf[n % 4], ("psf", n % 4)
            n += 1
            for j in range(8):
                C.mm(pm[:], wq[:, j, 128 * which:128 * which + 128], C.hnT[:, j, 512 * tg:512 * tg + 512],
                     j == 0, j == 7, ["m_wq", "hnT"], pk)
            C.copy("act", pre[:, 4 + 512 * tg:4 + 512 * tg + 512], pm[:], [pk], ["m_pre"])
        cut("mc_proj")
        C.ts("dve", acc[:], pre[:, 1:1 + S], cw[:, which, 0:1], None, ALU.mult, None, ["m_pre", "m_pre0", "m_cw"], ["m_acc"])
        for j in range(1, 4):
            C.stt(acc[:], pre[:, 1 + j:1 + j + S], cw[:, which, j:j + 1], acc[:], ALU.mult, ALU.add,
                  ["m_pre", "m_pre0", "m_cw", "m_acc"], ["m_acc"])
        cut("mc_conv")
        bcol = cbv[:, 4 * which + h:4 * which + h + 1]
        if which == 0:
            C.act(acc[:], acc[:], AF.Silu, ["m_acc", "m_cb"], ["m_acc"], bias=bcol)
            C.ts("dve", qT[:], acc[:], 128.0 ** -0.5, None, ALU.mult, None, ["m_acc"], ["m_qT"])
        else:
            C.act(kT[:], acc[:], AF.Silu, ["m_acc", "m_cb"], ["m_kT"], bias=bcol)
    cut("mc_qk")
    for k in range(16):
        pm, pk = C.psf[n % 4], ("psf", n % 4)
        n += 1
        for j in range(8):
            C.mm(pm[:], C.hnT[:, j, 128 * k:128 * k + 128], wvo[:, j, :], j == 0, j == 7, ["m_wvo", "hnT"], pk)
        C.copy("dve", v_sb[:, k, 0:256], pm[:, 0:256], [pk], [("m_v", k)])
        C.act(o_sb[:, k, :], pm[:, 256:512], AF.Sigmoid, [pk], [("m_o", k)])
    cut("mc_vo")
    for fc in range(2):
        for tg in range(4):
            pm, pk = C.psf[n % 4], ("psf", n % 4)
            n += 1
            for j in range(8):
                C.mm(pm[:], wz[:, j, 128 * fc:128 * fc + 128], C.hnT[:, j, 512 * tg:512 * tg + 512], j == 0, j == 7,
                     ["m_wz", "hnT"], pk)
            C.act(zT[:, fc, 512 * tg:512 * tg + 512], pm[:], AF.Silu, [pk], ["m_zT"])

    nchunk = 16
    if BISECT and BISECT.startswith("m_"):
        nchunk = int(BISECT[2:])
    sc2 = [sc, C.sb("m_sc1", [128, 128], BF16, st)]
    qs2 = [qs, C.sb("m_qs1", [128, 128], BF16, st)]
    eg2 = [C.sb("m_eg%d" % i, [128, 2], F32, st) for i in range(2)]

    Cb2 = [Cb, C.sb("m_Cb1", [128, 258], BF16, st)]

    def genA(k):
        b = k % 2
        tsl = slice(128 * k, 128 * k + 128)
        col = 4 * k + h
        p0, k0 = C.psf[0], ("psf", 0)
        p1, k1 = C.psf[1], ("psf", 1)
        p3, k3 = C.psf[3 + b], ("psf", 3 + b)
        C.ts("pool", lfbc[:], C.ones[:], C.lfn[:, col:col + 1], 0.0, ALU.mult, ALU.add, ["const", "lfn"], ["m_lfbc"])
        C.mm(p0[:, 0:128], kT[:, tsl], qT[:, tsl], True, True, ["m_kT", "m_qT"], k0)
        yield
        C.mm(p1[:, 0:128], lfbc[:], C.trin[:], True, True, ["m_lfbc", "const"], k1)
        pb0, kb0 = C.psb[0], ("psb", 0)
        if k < 15:
            C.tr(pb0[:, 0:128], kT[:, tsl], C.ident[:], ["m_kT", "const"], kb0)
        yield
        C.copy("dve", gB[:, 0:1], p1[:, 127:128], [k1], ["m_gB"])
        C.act(w[:], p1[:, 0:128], AF.Exp, [k1, "cbias"], ["m_w"], bias=C.cbias[:, col:col + 1])
        yield
        C.act(eB[:], p1[:, 0:128], AF.Exp, [k1], ["m_eB"])
        C.tt("pool", wm[:], w[:], C.tri[:], ALU.mult, ["m_w", "const"], ["m_wm"])
        yield
        if k < 15:
            C.act(wk4[:], C.cbias[:, 4 * k:4 * k + 4], AF.Exp, ["cbias", "m_gB"], ["m_wk4"], bias=gB[:, 0:1])
        C.tt("dve", sc2[b][:], p0[:, 0:128], wm[:], ALU.mult, [k0, "m_wm"], [("m_sc", b)])
        yield
        C.tt("pool", qs2[b][:], qT[:, tsl], eB[:], ALU.mult, ["m_qT", "m_eB"], [("m_qs", b)])
        C.copy("dve", eg2[b][:, 0:2], eB[:, 126:128], ["m_eB"], [("m_eg", b)])
        yield
        if k < 15:
            C.copy("act", kTok[:], pb0[:, 0:128], [kb0], ["m_kTok"])
            C.ts("dve", vt[:, 0:257], v_sb[:, k, 0:257], wk4[:, h:h + 1], None, ALU.mult, None,
                 [("m_v", k), "m_vones", "m_wk4"], ["m_vt"])
            yield
            C.mm(p3[:, 0:257], kTok[:], vt[:, 0:257], True, True, ["m_kTok", "m_vt"], k3)
            yield

    def genBs(k):
        b = k % 2
        p3, k3 = C.psf[3 + b], ("psf", 3 + b)
        if k < 15:
            if k == 0:
                C.copy("dve", Cf[:, 0:257], p3[:, 0:257], [k3], ["m_Cf"])
            else:
                C.stt(Cf[:, 0:257], Cf[:, 0:257], eg2[b][:, 1:2], p3[:, 0:257], ALU.mult, ALU.add,
                      [k3, ("m_eg", b), "m_Cf"], ["m_Cf"])
            yield
            C.copy("act", Cb2[(k + 1) % 2][:, 0:257], Cf[:, 0:257], ["m_Cf"], [("m_Cb", (k + 1) % 2)])
            yield

    def genBo(k):
        b = k % 2
        tsl = slice(128 * k, 128 * k + 128)
        p2, k2 = C.psf[2], ("psf", 2)
        C.mm(p2[:, 0:257], sc2[b][:], v_sb[:, k, 0:257], True, k == 0, [("m_sc", b), ("m_v", k), "m_vones"], k2)
        if k > 0:
            C.mm(p2[:, 0:257], qs2[b][:], Cb2[b][:, 0:257], False, True, [("m_qs", b), ("m_Cb", b)], k2)
        yield
        C.ts("dve", r[:, 0:1], p2[:, 256:257], -1.0, 1.0, ALU.mult, ALU.max, [k2], ["m_r"])
        C.ts("dve", r[:, 1:2], p2[:, 256:257], 1.0, None, ALU.max, None, [k2], ["m_r1"])
        yield
        C.tt("dve", r[:, 0:1], r[:, 0:1], r[:, 1:2], ALU.max, ["m_r", "m_r1"], ["m_r"])
        yield
        C.P.op("dve", lambda e: e.reciprocal(out=r[:, 0:1], in_=r[:, 0:1]), reads=["m_r"], writes=["m_r"])
        yield
        C.stt(hg[:], p2[:, 0:256], r[:, 0:1], o_sb[:, k, :], ALU.mult, ALU.mult, [k2, "m_r", ("m_o", k)], ["m_hg"])
        yield
        C.act(junk[:], hg[:], AF.Square, ["m_hg"], ["m_junk", "m_ssq"], accum_out=ssq[:, 0:1])
        yield
        C.ts("dve", rstd[:], ssq[:], 1.0 / 256, EPS, ALU.mult, ALU.add, ["m_ssq"], ["m_rstd"])
        yield
        C.act(rstd[:], rstd[:], AF.Sqrt, ["m_rstd"], ["m_rstd"])
        yield
        C.P.op("dve", lambda e: e.reciprocal(out=rstd[:], in_=rstd[:]), reads=["m_rstd"], writes=["m_rstd"])
        yield
        C.act(hgn[:], hg[:], AF.Copy, ["m_hg", "m_rstd"], ["m_hgn"], scale=rstd[:, 0:1])
        yield
        pb, kb = C.psb[1], ("psb", 1)
        for fc in range(2):
            C.tr(pb[:, 128 * fc:128 * fc + 128], hgn[:, 128 * fc:128 * fc + 128], C.ident[:], ["m_hgn", "const"], kb)
        yield
        for fc in range(2):
            C.stt(yT[:, fc, tsl], pb[:, 128 * fc:128 * fc + 128], hgc[:, 2 * h + fc:2 * h + fc + 1], zT[:, fc, tsl],
                  ALU.mult, ALU.mult, [kb, "m_hgc", "m_zT"], ["m_yT"])
            yield

    if nchunk > 0:
        run_interleaved([genA(0)])
    for k in range(nchunk):
        gens = [genBs(k), genBo(k)]
        if k + 1 < nchunk:
            gens.insert(0, genA(k + 1))
        run_interleaved(gens)
    out_proj(C, yT, 2, wo, "m_yT", "m_wo")


def cmul(C, eng, outR, outI, aR, aI, bR, bI, t1, t2, rk, wk, coarse=False):
    k1, k2, kR, kI = (wk, wk, wk, wk) if coarse else (wk + "_t1", wk + "_t2", wk + "_R", wk + "_I")
    C.tt(eng, t1, aR, bR, ALU.mult, rk, [k1])
    C.tt(eng, t2, aI, bI, ALU.mult, rk, [k2])
    C.tt(eng, outR, t1, t2, ALU.subtract, [k1, k2], [kR])
    C.tt(eng, t1, aR, bI, ALU.mult, rk, [k1])
    C.tt(eng, t2, aI, bR, ALU.mult, rk, [k2])
    C.tt(eng, outI, t1, t2, ALU.add, [k1, k2], [kI])


def phase_s5_setup(C, cin, scr):
    st = ExitStack()
    with st:
        LR = C.sb("s_LR", [128, 32], F32, st)
        LI = C.sb("s_LI", [128, 32], F32, st)
        DT = C.sb("s_DT", [128, 32], F32, st)
        TH = C.sb("s_TH", [128, 32], F32, st)
        LD = C.sb("s_LD", [128, 32], F32, st)
        cs = C.sb("s_cs", [128, 32], F32, st)
        sn = C.sb("s_sn", [128, 32], F32, st)
        rho = C.sb("s_rho", [128, 32], F32, st)
        rhi = C.sb("s_rhi", [128, 32], F32, st)
        aiR = C.sb("s_aiR", [128, 32], F32, st)
        aiI = C.sb("s_aiI", [128, 32], F32, st)
        t1 = C.sb("s_t1", [128, 32], F32, st)
        t2 = C.sb("s_t2", [128, 32], F32, st)
        t3 = C.sb("s_t3", [128, 32], F32, st)
        fR = C.sb("s_fR", [128, 32], F32, st)
        fI = C.sb("s_fI", [128, 32], F32, st)
        pi2 = C.sb("s_pi2", [128, 1], F32, st)
        mD = C.sb("s_mD", [128, 128], F32, st)
        P0 = C.sb("s_P0", [128, 128], F32, st)
        dcol = C.sb("s_dcol", [128, 64], F32, st)
        ER, EI = C.ER, C.EI
        C.dma("sp", LR[:], cin["s5_lr"], [], ["s_in"], "s_in")
        C.dma("sp", LI[:], cin["s5_li"], [], ["s_in"], "s_in")
        C.dma("sp", DT[:], cin["s5_ldt"], [], ["s_in"], "s_in")
        C.dma("sp", mD[:], cin["s5_maskD"], [], ["s_in"], "s_in")
        C.dma("sp", P0[:], cin["s5_P0"], [], ["s_in"], "s_in")
        C.dma("sp", dcol[:], cin["s5_dcol"], [], ["s_in"], "s_in")
        C.memset("dve", pi2[:], math.pi / 2.0, ["s_pi2"])
        K = ["s_in", "s_k"]
        C.act(DT[:], DT[:], AF.Exp, ["s_in"], ["s_k"])
        C.tt("dve", TH[:], LI[:], DT[:], ALU.mult, K, ["s_k"])
        C.tt("dve", LD[:], LR[:], DT[:], ALU.mult, K, ["s_k"])
        C.act(rho[:], LD[:], AF.Exp, K, ["s_k"])
        C.act(rhi[:], LD[:], AF.Exp, K, ["s_k"], scale=-1.0)
        C.act(sn[:], TH[:], AF.Sin, K, ["s_k"], scale=1.0 / 16.0)
        C.act(cs[:], TH[:], AF.Sin, K + ["s_pi2"], ["s_k"], scale=-1.0 / 16.0, bias=pi2[:, 0:1])
        for _ in range(4):
            C.tt("dve", t1[:], cs[:], cs[:], ALU.mult, K, ["s_k"])
            C.tt("dve", t2[:], sn[:], sn[:], ALU.mult, K, ["s_k"])
            C.tt("dve", t3[:], cs[:], sn[:], ALU.mult, K, ["s_k"])
            C.tt("dve", cs[:], t1[:], t2[:], ALU.subtract, K, ["s_k"])
            C.ts("dve", sn[:], t3[:], 2.0, None, ALU.mult, None, K, ["s_k"])
        C.memset("dve", ER[:, :, 7:8], 1.0, ["s_k"])
        C.memset("dve", EI[:, :, 7:8], 0.0, ["s_k"])
        C.tt("dve", ER[:, :, 8], rho[:], cs[:], ALU.mult, K, ["s_k"])
        C.tt("dve", EI[:, :, 8], rho[:], sn[:], ALU.mult, K, ["s_k"])
        C.tt("dve", aiR[:], rhi[:], cs[:], ALU.mult, K, ["s_k"])
        C.tt("dve", aiI[:], rhi[:], sn[:], ALU.mult, K, ["s_k"])
        C.ts("dve", aiI[:], aiI[:], -1.0, None, ALU.mult, None, K, ["s_k"])
        for e in range(1, 16):
            cmul(C, "dve", ER[:, :, 8 + e], EI[:, :, 8 + e], ER[:, :, 7 + e], EI[:, :, 7 + e], ER[:, :, 8], EI[:, :, 8],
                 t1[:], t2[:], K, "s_k", coarse=True)
        C.copy("dve", ER[:, :, 6], aiR[:], K, ["s_k"])
        C.copy("dve", EI[:, :, 6], aiI[:], K, ["s_k"])
        for e in range(1, 7):
            cmul(C, "dve", ER[:, :, 6 - e], EI[:, :, 6 - e], ER[:, :, 7 - e], EI[:, :, 7 - e], aiR[:], aiI[:],
                 t1[:], t2[:], K, "s_k", coarse=True)
        C.tt("dve", t1[:], LR[:], LR[:], ALU.mult, K, ["s_k"])
        C.tt("dve", t2[:], LI[:], LI[:], ALU.mult, K, ["s_k"])
        C.tt("dve", t1[:], t1[:], t2[:], ALU.add, K, ["s_k"])
        C.P.op("dve", lambda e: e.reciprocal(out=t3[:], in_=t1[:]), reads=K, writes=["s_k"])
        C.ts("dve", t1[:], ER[:, :, 8], -1.0, None, ALU.add, None, K, ["s_k"])
        C.tt("dve", fR[:], t1[:], LR[:], ALU.mult, K, ["s_k"])
        C.tt("dve", t2[:], EI[:, :, 8], LI[:], ALU.mult, K, ["s_k"])
        C.tt("dve", fR[:], fR[:], t2[:], ALU.add, K, ["s_k"])
        C.tt("dve", fR[:], fR[:], t3[:], ALU.mult, K, ["s_k"])
        C.tt("dve", fI[:], EI[:, :, 8], LR[:], ALU.mult, K, ["s_k"])
        C.tt("dve", t2[:], t1[:], LI[:], ALU.mult, K, ["s_k"])
        C.tt("dve", fI[:], fI[:], t2[:], ALU.subtract, K, ["s_k"])
        C.tt("dve", fI[:], fI[:], t3[:], ALU.mult, K, ["s_k"])

        bR = C.sb("s_bR", [128, 4, 16], F32, st)
        bI = C.sb("s_bI", [128, 4, 16], F32, st)
        cR = C.sb("s_cR", [128, 4, 16], F32, st)
        cI = C.sb("s_cI", [128, 4, 16], F32, st)
        BbR = C.sb("s_BbR", [128, 4, 16], F32, st)
        BbI = C.sb("s_BbI", [128, 4, 16], F32, st)
        u1 = C.sb("s_u1", [128, 4, 16], F32, st)
        u2 = C.sb("s_u2", [128, 4, 16], F32, st)
        KWR = C.sb("s_KWR", [128, 4, 16, 16], F32, st)
        KWI = C.sb("s_KWI", [128, 4, 16, 16], F32, st)
        QR = C.sb("s_QR", [128, 4, 24, 16], F32, st)
        QI = C.sb("s_QI", [128, 4, 24, 16], F32, st)
        v1 = C.sb("s_v1", [128, 4, 24, 16], F32, st)
        v2 = C.sb("s_v2", [128, 4, 24, 16], F32, st)
        v3 = C.sb("s_v3", [128, 4, 24, 16], F32, st)
        v4 = C.sb("s_v4", [128, 4, 24, 16], F32, st)
        w3 = C.sb("s_w3", [128, 4, 16, 16], F32, st)
        w4 = C.sb("s_w4", [128, 4, 16, 16], F32, st)
        w1 = C.sb("s_w1", [128, 4, 16, 16], F32, st)
        w2 = C.sb("s_w2", [128, 4, 16, 16], F32, st)
        Tsb = C.sb("s_Tsb", [128, 8, 256], BF16, st)
        Wsb = C.sb("s_Wsb", [128, 8, 256], BF16, st)
        OQb = C.sb("s_OQb", [128, 4, 2, 256], BF16, st)
        tmpT = C.sb("s_tmpT", [128, 128], F32, st)
        for b in range(8):
            g2s = slice(4 * b, 4 * b + 4)
            for t, nm in ((bR, "s5_br"), (bI, "s5_bi"), (cR, "s5_cr"), (cI, "s5_ci")):
                C.dma("sp", t[:], cin[nm][:, g2s, :], [], ["s_bc"], "s_bc")
            KB = ["s_bc", "s_k", "s_b"]
            fRb = fR[:, g2s].unsqueeze(2).to_broadcast([128, 4, 16])
            fIb = fI[:, g2s].unsqueeze(2).to_broadcast([128, 4, 16])
            cmul(C, "dve", BbR[:], BbI[:], bR[:], bI[:], fRb, fIb, u1[:], u2[:], KB, "s_b")
            KBB = KB + ["s_b_R", "s_b_I"]
            eR = ER[:, g2s, :].unsqueeze(3).to_broadcast([128, 4, 24, 16])
            eI = EI[:, g2s, :].unsqueeze(3).to_broadcast([128, 4, 24, 16])
            ccR = cR[:].unsqueeze(2).to_broadcast([128, 4, 24, 16])
            ccI = cI[:].unsqueeze(2).to_broadcast([128, 4, 24, 16])
            KQ0 = ["s_bc", "s_k"]
            C.tt("pool", v3[:], eR, ccI, ALU.mult, KQ0, ["s_q_t3"])
            C.tt("pool", v4[:], eI, ccR, ALU.mult, KQ0, ["s_q_t4"])
            C.tt("pool", v1[:], eR, ccR, ALU.mult, KQ0, ["s_q_t1"])
            C.tt("pool", v2[:], eI, ccI, ALU.mult, KQ0, ["s_q_t2"])
            C.tt("dve", QR[:], v1[:], v2[:], ALU.subtract, ["s_q_t1", "s_q_t2"], ["s_q_R"])
            C.stt(QI[:], v3[:], -1.0, v4[:], ALU.mult, ALU.subtract, ["s_q_t3", "s_q_t4"], ["s_q_I"])
            eR = ER[:, g2s, 7:23].unsqueeze(3).to_broadcast([128, 4, 16, 16])
            eI = EI[:, g2s, 7:23].unsqueeze(3).to_broadcast([128, 4, 16, 16])
            bbR = BbR[:].unsqueeze(2).to_broadcast([128, 4, 16, 16])
            bbI = BbI[:].unsqueeze(2).to_broadcast([128, 4, 16, 16])
            C.tt("pool", w3[:], eR, bbI, ALU.mult, KBB, ["s_kw_t3"])
            C.tt("pool", w4[:], eI, bbR, ALU.mult, KBB, ["s_kw_t4"])
            C.tt("pool", KWI[:], w3[:], w4[:], ALU.add, ["s_kw_t3", "s_kw_t4"], ["s_kw_I"])
            C.tt("dve", w1[:], eR, bbR, ALU.mult, KBB, ["s_kw_t1"])
            C.tt("dve", w2[:], eI, bbI, ALU.mult, KBB, ["s_kw_t2"])
            C.tt("dve", KWR[:], w1[:], w2[:], ALU.subtract, ["s_kw_t1", "s_kw_t2"], ["s_kw_R"])
            KQ = ["s_kw_R", "s_kw_I", "s_q_R", "s_q_I", "const", "s_in"]
            C.copy("act", OQb[:, :, 0, :], QR[:, :, 8:24, :].rearrange("p g e o -> p g (e o)"), KQ, ["s_OQb"])
            C.copy("act", OQb[:, :, 1, :], QI[:, :, 8:24, :].rearrange("p g e o -> p g (e o)"), KQ, ["s_OQb"])
            C.dma("sp", scr["OQ"][4 * b:4 * b + 4].rearrange("g p r c -> p g r c"), OQb[:], ["s_OQb"], [], "s_oq_out")
            for g2l in range(4):
                for gp in range(2):
                    gl = 2 * g2l + gp
                    ps_ = slice(64 * gp, 64 * gp + 64)
                    pm, pk = C.psf[gl % 2], ("psf", gl % 2)
                    C.mm(pm[:, 0:256], KWR[ps_, g2l, 0:8, :].rearrange("p s i -> p (s i)"),
                         QR[ps_, g2l, 0:16, :].rearrange("p e o -> p (e o)"), True, False, KQ, pk)
                    C.mm(pm[:, 0:256], KWI[ps_, g2l, 0:8, :].rearrange("p s i -> p (s i)"),
                         QI[ps_, g2l, 0:16, :].rearrange("p e o -> p (e o)"), False, True, KQ, pk)
                    C.tt("dve", tmpT[:], pm[:, 0:128], mD[:], ALU.mult, [pk, "s_in"], ["s_tmpT"])
                    g = 8 * b + gl
                    C.stt(Tsb[:, gl, 0:128], P0[:], dcol[:, g:g + 1], tmpT[:], ALU.mult, ALU.add,
                          ["s_tmpT", "s_in"], ["s_Tsb"])
                    C.copy("act", Tsb[:, gl, 128:256], pm[:, 128:256], [pk], ["s_Tsb"])
                    pw, pkw = C.psf[2 + gl % 2], ("psf", 2 + gl % 2)
                    idn = C.identf[ps_, 64 * gp:64 * gp + 64]
                    for n, (src, half) in enumerate(((KWR, 0), (KWI, 0), (KWR, 1), (KWI, 1))):
                        C.tr(pw[:, 64 * n:64 * n + 64], src[ps_, g2l, 8 * half:8 * half + 8, :].rearrange("p s i -> p (s i)"),
                             idn, KQ, pkw)
                    C.copy("dve", Wsb[:, gl, :], pw[:, 0:256], [pkw], ["s_Wsb"])
            C.dma("sp", scr["T"][8 * b:8 * b + 8].rearrange("g p c -> p g c"), Tsb[:], ["s_Tsb"], [], "s_t_out")
            C.dma("sp", scr["W"][8 * b:8 * b + 8].rearrange("g p c -> p g c"), Wsb[:], ["s_Wsb"], [], "s_w_out")
        C.P.emit_phase()


def phase_s5_in(C, cin, scr, XR, XI, u_cm):
    st = ExitStack()
    with st:
        wu = C.sb("s_wu", [128, 8, 512], BF16, st)
        Wsb = C.sb("s1_Wsb", [128, 8, 256], BF16, st)
        Usb = [C.sb("s1_Usb%d" % i, [128, 256], BF16, st) for i in range(2)]
        n = 0
        for half in range(2):
            load_w(C, wu[:], cin["ev_w_in"], EV_U0 + 512 * half, 512, "s_wu")
            for s in range(16):
                pm, pk = C.psf[n % 2], ("psf", n % 2)
                n += 1
                hv = C.hnT[:].rearrange("p j (c s) -> p j c s", s=16)
                for j in range(8):
                    C.mm(pm[:], hv[:, j, :, s], wu[:, j, :], j == 0, j == 7, ["s_wu", "hnT"], pk)
                C.copy("act" if n % 2 else "dve", u_cm[:, 32 * half:32 * half + 32, 15 - s, :],
                       pm[:].rearrange("p (g i) -> p g i", i=16), [pk], ["s_ucm"])
        for b in range(8):
            C.dma("sp", Wsb[:], scr["W"][8 * b:8 * b + 8].rearrange("g p c -> p g c"), [], ["s1_Wsb"], "s1_Wsb")
            def gen_in(gl):
                g = 8 * b + gl
                gp, g2 = g % 2, g // 2
                U = Usb[g % 2]
                uk = ("s1_U", g % 2)
                pb, kb = C.psb[g % 2], ("psb", g % 2)
                for hf in range(2):
                    C.tr(pb[:, 128 * hf:128 * hf + 128], u_cm[:, g, 8 * hf:8 * hf + 8, :].rearrange("p s i -> p (s i)"),
                         C.ident[:], ["s_ucm", "const"], kb)
                yield
                C.copy("act" if g % 2 else "dve", U[:], pb[:, 0:256], [kb], [uk])
                yield
                C.dma("sp", scr["U"][g], U[:], [uk], [], ("s1_uo", g % 2))
                px, kx = C.psf[2 + g2 % 2], ("psf", 2 + g2 % 2)
                ps_ = slice(64 * gp, 64 * gp + 64)
                for ri in range(2):
                    C.mm(px[ps_, 128 * ri:128 * ri + 128], Wsb[:, gl, 64 * ri:64 * ri + 64], U[:, 0:128], True, False,
                         ["s1_Wsb", uk], kx)
                    C.mm(px[ps_, 128 * ri:128 * ri + 128], Wsb[:, gl, 128 + 64 * ri:128 + 64 * ri + 64], U[:, 128:256],
                         False, True, ["s1_Wsb", uk], kx)
                yield
                if gp == 1:
                    C.copy("dve", XR[:, g2, :], px[:, 0:128], [kx], ["s_XR"])
                    C.copy("act", XI[:, g2, :], px[:, 128:256], [kx], ["s_XI"])
                yield

            for gl in range(0, 8, 2):
                run_interleaved([gen_in(gl), gen_in(gl + 1)])
        C.P.emit_phase()


def phase_s5_scan(C, XR, XI, XRb, XIb):
    st = ExitStack()
    with st:
        t1 = C.sb("sc_t1", [128, 32], F32, st)
        t2 = C.sb("sc_t2", [128, 32], F32, st)
        t3 = C.sb("sc_t3", [128, 32], F32, st)
        t4 = C.sb("sc_t4", [128, 32], F32, st)
        AR, AI = C.ER[:, :, 23], C.EI[:, :, 23]
        for c in range(1, 128):
            C.tt("dve", t1[:], AR, XR[:, :, c - 1], ALU.mult, ["s_XR"], ["sc_t1"])
            C.tt("dve", t2[:], AI, XI[:, :, c - 1], ALU.mult, ["s_XI"], ["sc_t2"])
            C.tt("dve", t3[:], AR, XI[:, :, c - 1], ALU.mult, ["s_XI"], ["sc_t3"])
            C.tt("dve", t4[:], AI, XR[:, :, c - 1], ALU.mult, ["s_XR"], ["sc_t4"])
            C.tt("dve", t1[:], t1[:], t2[:], ALU.subtract, ["sc_t1", "sc_t2"], ["sc_t1"])
            C.tt("dve", t3[:], t3[:], t4[:], ALU.add, ["sc_t3", "sc_t4"], ["sc_t3"])
            C.tt("dve", XR[:, :, c], XR[:, :, c], t1[:], ALU.add, ["s_XR", "sc_t1"], ["s_XR"])
            C.tt("dve", XI[:, :, c], XI[:, :, c], t3[:], ALU.add, ["s_XI", "sc_t3"], ["s_XI"])
        C.memset("dve", XRb[:, :, 0:1], 0.0, ["s_XRb0"])
        C.memset("dve", XIb[:, :, 0:1], 0.0, ["s_XIb0"])
        C.copy("dve", XRb[:, :, 1:128], XR[:, :, 0:127], ["s_XR"], ["s_XRb"])
        C.copy("act", XIb[:, :, 1:128], XI[:, :, 0:127], ["s_XI"], ["s_XIb"])
        C.P.emit_phase()


GELU_C = 0.7978845608028654


def phase_s5_out(C, cin, scr, XRb, XIb, yg_cm):
    st = ExitStack()
    with st:
        Tsb = C.sb("s3_Tsb", [128, 8, 256], BF16, st)
        OQb = C.sb("s3_OQb", [128, 4, 2, 256], BF16, st)
        Ub = C.sb("s3_Ub", [128, 8, 256], BF16, st)
        Ysb = [C.sb("s3_Y%d" % i, [128, 256], F32, st) for i in range(2)]
        xs2 = [C.sb("s3_xs%d" % i, [128, 256], F32, st) for i in range(2)]
        x22 = [C.sb("s3_x2%d" % i, [128, 256], F32, st) for i in range(2)]
        sg2 = [C.sb("s3_sg%d" % i, [128, 256], F32, st) for i in range(2)]
        XK = ["s_XRb", "s_XIb", "s_XRb0", "s_XIb0"]
        for b in range(8):
            C.dma("sp", Tsb[:], scr["T"][8 * b:8 * b + 8].rearrange("g p c -> p g c"), [], ["s3_Tsb"], "s3_Tsb")
            C.dma("sp", OQb[:], scr["OQ"][4 * b:4 * b + 4].rearrange("g p r c -> p g r c"), [], ["s3_OQb"], "s3_OQb")
            C.dma("sp", Ub[:], scr["U"][8 * b:8 * b + 8].rearrange("g p c -> p g c"), [], ["s3_Ub"], "s3_Ub")
            def gen_out(gl):
                g = 8 * b + gl
                par = g % 2
                gp, g2, g2l = g % 2, g // 2, gl // 2
                ps_ = slice(64 * gp, 64 * gp + 64)
                W = ["s3_Tsb", "s3_OQb", "s3_Ub"] + XK
                pa, ka = C.psf[3 * par], ("psf", 3 * par)
                pbk, kbk = C.psf[3 * par + 1], ("psf", 3 * par + 1)
                pt, kt = C.psf[3 * par + 2], ("psf", 3 * par + 2)
                xs, x2, sg = xs2[par], x22[par], sg2[par]
                kxs, kx2, ksg = ("s3_xs", par), ("s3_x2", par), ("s3_sg", par)
                UA, UB = Ub[:, gl, 0:128], Ub[:, gl, 128:256]
                TD, TO = Tsb[:, gl, 0:128], Tsb[:, gl, 128:256]
                C.mm(pa[:, 0:128], TD, UB, True, False, W, ka)
                C.mm(pa[:, 0:128], OQb[ps_, g2l, 0, 0:128], XRb[ps_, g2, :], False, False, W, ka)
                C.mm(pa[:, 0:128], OQb[ps_, g2l, 1, 0:128], XIb[ps_, g2, :], False, True, W, ka)
                C.mm(pbk[:, 0:128], TD, UA, True, False, W, kbk)
                C.mm(pbk[:, 0:128], TO, UB, False, False, W, kbk)
                C.mm(pbk[:, 0:128], OQb[ps_, g2l, 0, 128:256], XRb[ps_, g2, :], False, False, W, kbk)
                C.mm(pbk[:, 0:128], OQb[ps_, g2l, 1, 128:256], XIb[ps_, g2, :], False, True, W, kbk)
                yield
                Y = Ysb[par]
                yk = ("s3_Y", par)
                C.copy("act", Y[:, 0:128], pa[:, 0:128], [ka], [yk])
                C.copy("dve", Y[:, 128:256], pbk[:, 0:128], [kbk], [yk])
                yield
                for hf in range(2):
                    C.tr(pt[:, 128 * hf:128 * hf + 128], Y[:, 128 * hf:128 * hf + 128], C.identf[:], [yk, "const"], kt)
                yield
                C.copy("act", xs[:], pt[:, 0:256], [kt], [kxs])
                yield
                C.tt("pool", x2[:], xs[:], xs[:], ALU.mult, [kxs], [kx2])
                yield
                C.ts("dve", x2[:], x2[:], 0.044715, 1.0, ALU.mult, ALU.add, [kx2], [kx2])
                yield
                C.tt("pool", x2[:], x2[:], xs[:], ALU.mult, [kx2, kxs], [kx2])
                yield
                C.act(sg[:], x2[:], AF.Sigmoid, [kx2], [ksg], scale=2.0 * GELU_C)
                yield
                C.tt("dve", yg_cm[:, :, 16 * g:16 * g + 16], xs[:].rearrange("p (t o) -> p t o", o=16),
                     sg[:].rearrange("p (t o) -> p t o", o=16), ALU.mult, [kxs, ksg], ["s_ygcm"])
                yield

            for gl in range(0, 8, 2):
                run_interleaved([gen_out(gl), gen_out(gl + 1)])
        C.P.emit_phase()


def phase_s5_glu(C, cin, yg_cm, ygT, yT):
    for s in range(16):
        pb, kb = C.psb[s % 2], ("psb", s % 2)
        for j in range(8):
            C.tr(pb[:, 128 * j:128 * j + 128], yg_cm[:, s, 128 * j:128 * j + 128], C.ident[:], ["s_ygcm", "const"], kb)
        dst = ygT.rearrange("p j (c s) -> p j c s", s=16)[:, :, :, s]
        C.copy("act" if s % 2 else "dve", dst, pb[:].rearrange("p (j c) -> p j c", j=8), [kb], ["s4_ygT"])
    C.P.emit_phase()
    sC = ExitStack()
    with sC:
        gw = C.sb("s4_gw", [128, 8, 1024], BF16, sC)
        wz = C.sb("s4_wz", [128, 8, 128], BF16, sC)
        gb = C.sb("s4_gb", [128, 8], F32, sC)
        sgt = [C.sb("s4_sg%d" % i, [128, 512], BF16, sC) for i in range(2)]
        zs = [C.sb("s4_zs%d" % i, [128, 512], BF16, sC) for i in range(2)]
        load_w(C, gw[:, :, 0:512], cin["s5_glu_w"], 0, 512, "s4_gw")
        load_w(C, gw[:, :, 512:1024], cin["s5_glu_w"], 512, 512, "s4_gw")
        C.dma("sp", gb[:], cin["s5_glu_bc"], [], ["s4_gb"], "s4_gb")
        n = 0
        for fo in range(8):
            load_w(C, wz[:], cin["ev_w_in"], EV_Z0 + 1024 + 128 * fo, 128, "s4_wz")
            for tg in range(4):
                tsl = slice(512 * tg, 512 * tg + 512)
                pm, pk = C.psf[n % 2], ("psf", n % 2)
                pz, kz = C.psf[2 + n % 2], ("psf", 2 + n % 2)
                sgk, zsk = ("s4_sg", n % 2), ("s4_zs", n % 2)
                for j in range(8):
                    C.mm(pm[:], gw[:, j, 128 * fo:128 * fo + 128], ygT[:, j, tsl], j == 0, j == 7,
                         ["s4_gw", "s4_ygT"], pk)
                for j in range(8):
                    C.mm(pz[:], wz[:, j, :], C.hnT[:, j, tsl], j == 0, j == 7, ["s4_wz", "hnT"], kz)
                C.act(sgt[n % 2][:], pm[:], AF.Sigmoid, [pk, "s4_gb"], [sgk], bias=gb[:, fo:fo + 1])
                C.act(zs[n % 2][:], pz[:], AF.Silu, [kz], [zsk])
                C.tt("dve", sgt[n % 2][:], sgt[n % 2][:], ygT[:, fo, tsl], ALU.mult, [sgk, "s4_ygT"], [sgk])
                C.tt("pool", yT[:, fo, tsl], sgt[n % 2][:], zs[n % 2][:], ALU.mult, [sgk, zsk], ["s4_yT"])
                n += 1
        C.P.emit_phase()
    sD = ExitStack()
    with sD:
        wo = C.sb("s4_wo", [128, 8, D], BF16, sD)
        load_w(C, wo[:], cin["ev_w_out"], 0, D, "s4_wo", rows0=1024, nj=8)
        out_proj(C, yT, 8, wo, "s4_yT", "s4_wo")
        C.P.emit_phase()


def layer0_s5(C, cin):
    scr = {
        "T": C.dram_scr("scr_T", [64, 128, 256], BF16),
        "W": C.dram_scr("scr_W", [64, 128, 256], BF16),
        "OQ": C.dram_scr("scr_OQ", [32, 128, 2, 256], BF16),
        "U": C.dram_scr("scr_U", [64, 128, 256], BF16),
    }
    sE = ExitStack()
    with sE:
        C.ER = C.sb("s_ER", [128, 32, 24], F32, sE)
        C.EI = C.sb("s_EI", [128, 32, 24], F32, sE)
        phase_s5_setup(C, cin, scr)
        bufA = C.sb("s_bufA", [128, 16384], BF16, sE)
        bufB = C.sb("s_bufB", [128, 16384], BF16, sE)
        u_cm = bufA[:].rearrange("p (g s i) -> p g s i", s=16, i=16)
        yg_cm = bufA[:].rearrange("p (s c) -> p s c", c=1024)
        yT = bufA[:].rearrange("p (j t) -> p j t", t=S)
        XR = bufB[:, 0:8192].bitcast(F32).rearrange("p (g c) -> p g c", c=128)
        XI = bufB[:, 8192:16384].bitcast(F32).rearrange("p (g c) -> p g c", c=128)
        ygT = bufB[:].rearrange("p (j t) -> p j t", t=S)
        sXb = ExitStack()
        with sXb:
            XRb = C.sb("s_XRb", [128, 32, 128], BF16, sXb)
            XIb = C.sb("s_XIb", [128, 32, 128], BF16, sXb)
            phase_s5_in(C, cin, scr, XR, XI, u_cm)
            phase_s5_scan(C, XR, XI, XRb, XIb)
            phase_s5_out(C, cin, scr, XRb, XIb, yg_cm)
        phase_s5_glu(C, cin, yg_cm, ygT, yT)


def layer0(C, cin):
    phase_norm(C, cin["norm_g0"], "l0")
    if BISECT == "l0norm":
        return
    if "ml" not in SKIP:
        phase_ml_prep(C, cin["ev_w_in"], cin["ev_if_bias"])
        if BISECT == "ml_prep":
            return
        for h in range(1 if BISECT else 4):
            phase_ml_head(C, h, cin["ev_w_in"], cin["ev_w_out"], cin["ev_conv_wT"], cin["ev_conv_bc"], cin["ev_head_gc"])
    if "s5" not in SKIP:
        layer0_s5(C, cin)


SKIP = set()


CONST_SPECS = {
    "ident_bf": ([128, 128], BF16), "ident_f": ([128, 128], F32), "tri_f": ([128, 128], F32),
    "trin_f": ([128, 128], F32), "trin16_f": ([128, 128], F32), "ones_f": ([128, 128], F32),
}

IN_SPECS = {
    "x": ([S, D], F32),
    "norm_g0": ([128, D], F32), "norm_g1": ([128, D], F32), "norm_gf": ([128, D], F32),
    "od_w_in": ([D, 6160], F32), "gla_wab": ([17, 1024], F32), "gla_head_g": ([128, 16], F32),
    "od_w_out": ([2048, D], F32),
    "ev_w_in": ([D, 6152], F32), "ev_w_out": ([2048, D], F32), "ev_if_bias": ([128, 128], F32),
    "ev_conv_wT": ([1024, 4], F32), "ev_conv_bc": ([128, 8], F32), "ev_head_gc": ([128, 8], F32),
    "s5_lr": ([128, 32], F32), "s5_li": ([128, 32], F32), "s5_ldt": ([128, 32], F32),
    "s5_br": ([128, 32, 16], F32), "s5_bi": ([128, 32, 16], F32), "s5_cr": ([128, 32, 16], F32),
    "s5_ci": ([128, 32, 16], F32), "s5_maskD": ([128, 128], F32), "s5_P0": ([128, 128], F32),
    "s5_dcol": ([128, 64], F32), "s5_glu_w": ([1024, 1024], F32), "s5_glu_bc": ([128, 8], F32),
}


def host_consts():
    tri = np.triu(np.ones((128, 128), np.float32))
    return {
        "ident_bf": np.eye(128, dtype=ml_dtypes.bfloat16), "ident_f": np.eye(128, dtype=np.float32),
        "tri_f": tri, "trin_f": -tri, "trin16_f": -tri / 16.0, "ones_f": np.ones((128, 128), np.float32),
    }


def build_program(layers=(0, 1), final=True):
    nc = bass.Bass("TRN2", target_bir_lowering=False)
    st = ExitStack()
    with st:
        C = Ctx(nc, st)
        cin = {}
        for nm, (shp, dt) in list(CONST_SPECS.items()) + list(IN_SPECS.items()):
            cin[nm] = C.dram_in(nm, shp, dt)
        out_d = C.dram_out("out", [S, D], F32)
        setup_globals(C, cin)
        C.lfn = C.sb("lfn", [128, 64], F32)
        C.cbias = C.sb("cbias", [128, 64], F32)
        phase_load_x(C, cin["x"])
        if 0 in layers:
            layer0(C, cin)
        if 1 in layers:
            layer1(C, cin)
        if final:
            phase_final(C, cin["norm_gf"], out_d)
        else:
            ov = out_d.rearrange("(c s) d -> c s d", s=16)
            for q in range(4):
                C.dma("sp", ov[:, 4 * q:4 * q + 4, :], C.x_sb[:, 4 * q:4 * q + 4, :],
                      [("x", s) for s in range(4 * q, 4 * q + 4)], [], ("out", q % 2))
            C.P.emit_phase()
    return nc


def host_inputs(inp, b):
    f32 = np.float32
    d = dict(host_consts())
    d["x"] = np.ascontiguousarray(inp["x"][b], dtype=f32)
    d["norm_g0"] = np.ascontiguousarray(np.broadcast_to(inp["norm_g"][0], (128, D)), dtype=f32)
    d["norm_g1"] = np.ascontiguousarray(np.broadcast_to(inp["norm_g"][1], (128, D)), dtype=f32)
    d["norm_gf"] = np.ascontiguousarray(np.broadcast_to(inp["final_norm_g"], (128, D)), dtype=f32)
    d["od_w_in"] = np.ascontiguousarray(inp["od_w_in"][0], dtype=f32)
    d["gla_wab"] = np.ascontiguousarray(np.concatenate([inp["gla_w_alpha"][0], inp["gla_b_alpha"][0][None, :]], 0), dtype=f32)
    d["gla_head_g"] = np.ascontiguousarray(inp["gla_head_g"][0].reshape(16, 128).T, dtype=f32)
    d["od_w_out"] = np.ascontiguousarray(inp["od_w_out"][0], dtype=f32)
    d["ev_w_in"] = np.ascontiguousarray(inp["ev_w_in"][0], dtype=f32)
    d["ev_w_out"] = np.ascontiguousarray(inp["ev_w_out"][0], dtype=f32)
    ifb = np.concatenate([inp["ev_i_bias"][0], inp["ev_f_bias"][0]])
    d["ev_if_bias"] = np.ascontiguousarray(np.broadcast_to(np.tile(ifb, 16), (128, 128)), dtype=f32)
    d["ev_conv_wT"] = np.ascontiguousarray(inp["ev_conv_w"][0].T, dtype=f32)
    d["ev_conv_bc"] = np.ascontiguousarray(inp["ev_conv_b"][0].reshape(8, 128).T, dtype=f32)
    d["ev_head_gc"] = np.ascontiguousarray(inp["ev_head_g"][0].reshape(8, 128).T, dtype=f32)

    def pair(a):
        a = a.reshape((32, 2, 64) + a.shape[2:])
        return np.ascontiguousarray(np.moveaxis(a, 0, 2).reshape((128, 32) + a.shape[3:]), dtype=f32)

    d["s5_lr"] = pair(inp["s5_lam_re"][0])
    d["s5_li"] = pair(inp["s5_lam_im"][0])
    d["s5_ldt"] = pair(np.broadcast_to(inp["s5_log_dt"][0][:, None], (64, 64)))
    d["s5_br"] = pair(inp["s5_b_re"][0])
    d["s5_bi"] = pair(inp["s5_b_im"][0])
    d["s5_cr"] = pair(np.swapaxes(inp["s5_c_re"][0], 1, 2))
    d["s5_ci"] = pair(np.swapaxes(inp["s5_c_im"][0], 1, 2))
    sig = np.arange(128) // 16
    ii = np.arange(128) % 16
    d["s5_maskD"] = (sig[:, None] + sig[None, :] >= 7).astype(f32)
    d["s5_P0"] = ((sig[:, None] + sig[None, :] == 7) & (ii[:, None] == ii[None, :])).astype(f32)
    d["s5_dcol"] = np.ascontiguousarray(np.tile(inp["s5_d"][0].reshape(64, 16).T, (8, 1)), dtype=f32)
    d["s5_glu_w"] = np.ascontiguousarray(inp["s5_glu_w"][0], dtype=f32)
    d["s5_glu_bc"] = np.ascontiguousarray(inp["s5_glu_b"][0].reshape(8, 128).T, dtype=f32)
    return d


def kernel(**inputs):
    inp = {k: np.asarray(v) for k, v in inputs.items()}
    nc = build_program()
    in_maps = [host_inputs(inp, b) for b in range(8)]
    res = run_bass_kernel_spmd(nc, in_maps, core_ids=list(range(8)))
    return np.stack([np.asarray(r["out"], dtype=np.float32) for r in res.results], 0)
```

```python
import math
from contextlib import ExitStack

import numpy as np
import ml_dtypes

import concourse.bass as bass
import concourse.mybir as mybir
from concourse.bass_utils import run_bass_kernel_spmd

F32 = mybir.dt.float32
BF16 = mybir.dt.bfloat16
ALU = mybir.AluOpType
AF = mybir.ActivationFunctionType
AX = mybir.AxisListType

D = 1024
S = 2048
EPS = 1e-6

STRICT = True
COMPUTE = ("pe", "act", "dve", "pool")
ENGS = ("pe", "act", "dve", "pool", "sp")


class Op:
    __slots__ = ("eng", "fn", "waits", "signal", "idx", "dsem", "dval", "cnt", "phase")

    def __init__(self, eng, fn):
        self.eng = eng
        self.fn = fn
        self.waits = []
        self.signal = False
        self.idx = -1
        self.dsem = None
        self.dval = 0
        self.cnt = 0
        self.phase = 0


class Prog:
    def __init__(self, nc, stack):
        self.nc = nc
        self.stack = stack
        self.streams = {e: [] for e in ENGS}
        self.esem = {e: stack.enter_context(nc.semaphore("sem_" + e)) for e in COMPUTE}
        self.base = {e: 0 for e in COMPUTE}
        self.nidx = {e: 0 for e in ENGS}
        self.waited = {e: {} for e in ENGS}
        self.state = {}
        self.dsems = {}
        self.phase = 0
        self.n_ops = 0

    def _add_wait(self, o, d, raw):
        if d is o or d.phase < self.phase:
            return
        if d.dsem is None and d.eng == o.eng and not raw and (not STRICT or o.eng == "pe"):
            return
        if d.dsem is not None:
            key, val = ("d", d.dsem), d.dval
        else:
            key, val = d.eng, d.idx
        w = self.waited[o.eng]
        if w.get(key, -1) >= val:
            return
        w[key] = val
        d.signal = True
        o.waits.append(d)

    def op(self, eng, fn, reads=(), writes=(), dma=None):
        o = Op(eng, fn)
        o.phase = self.phase
        o.idx = self.nidx[eng]
        self.nidx[eng] += 1
        self.streams[eng].append(o)
        self.n_ops += 1
        for k in reads:
            st = self.state.get(k)
            if st is not None:
                if st[0] is not None:
                    self._add_wait(o, st[0], True)
                if isinstance(k, tuple) and k[0] in ("psf", "psb"):
                    for r in st[1].values():
                        self._add_wait(o, r, False)
        for k in writes:
            st = self.state.get(k)
            if st is not None:
                if st[0] is not None:
                    self._add_wait(o, st[0], False)
                for r in st[1].values():
                    self._add_wait(o, r, False)
                for r in st[2]:
                    self._add_wait(o, r, False)
        if dma is not None:
            ds = self.dsems.get(dma)
            if ds is None:
                ds = [self.stack.enter_context(self.nc.semaphore("dsem_%d" % len(self.dsems))), 0]
                self.dsems[dma] = ds
            ds[1] += 16
            o.dsem = dma
            o.dval = ds[1]
            o.signal = True
        for k in reads:
            st = self.state.get(k)
            if st is None:
                st = [None, {}, []]
                self.state[k] = st
            if o.dsem is not None:
                st[2].append(o)
            else:
                st[1][eng] = o
        for k in writes:
            self.state[k] = [o, {}, []]
        return o

    def _emit_stream(self, name, eng):
        for o in self.streams[name]:
            for d in o.waits:
                if d.dsem is not None:
                    eng.wait_ge(self.dsems[d.dsem][0], d.dval)
                else:
                    eng.wait_ge(self.esem[d.eng], d.cnt)
            if o.fn is None:
                continue
            bi = o.fn(eng)
            if o.dsem is not None:
                bi.then_inc(self.dsems[o.dsem][0], 16)
            elif o.signal:
                bi.then_inc(self.esem[o.eng], 1)
        if name == "sp":
            for sem, tot in self.dsems.values():
                if tot > 0:
                    eng.wait_ge(sem, tot)

    def emit_phase(self):
        for e in COMPUTE:
            c = self.base[e]
            for o in self.streams[e]:
                if o.signal and o.dsem is None:
                    assert o.fn is not None
                    c += 1
                o.cnt = c
            self.base[e] = c
        with self.nc.Block() as block:
            @block.tensor
            def _(eng):
                self._emit_stream("pe", eng)

            @block.scalar
            def _(eng):
                self._emit_stream("act", eng)

            @block.vector
            def _(eng):
                self._emit_stream("dve", eng)

            @block.gpsimd
            def _(eng):
                self._emit_stream("pool", eng)

            @block.sync
            def _(eng):
                self._emit_stream("sp", eng)
        self.streams = {e: [] for e in ENGS}
        self.phase += 1


class Ctx:
    def __init__(self, nc, stack):
        self.nc = nc
        self.stack = stack
        self.P = Prog(nc, stack)

    def sb(self, name, shape, dt, st=None):
        self.nuniq = getattr(self, "nuniq", 0) + 1
        return (st or self.stack).enter_context(self.nc.sbuf_tensor("sb%d_%s" % (self.nuniq, name), list(shape), dt))

    def ps(self, name, shape, dt):
        return self.stack.enter_context(self.nc.psum_tensor(name, list(shape), dt))

    def dram_in(self, name, shape, dt):
        return self.nc.dram_tensor(name, list(shape), dt, kind="ExternalInput").ap()

    def dram_out(self, name, shape, dt):
        return self.nc.dram_tensor(name, list(shape), dt, kind="ExternalOutput").ap()

    def dram_scr(self, name, shape, dt):
        return self.nc.dram_tensor(name, list(shape), dt).ap()

    def mm(self, out, lhsT, rhs, start, stop, reads, pkey):
        self.P.op("pe", lambda e: e.matmul(out=out, lhsT=lhsT, rhs=rhs, start=start, stop=stop),
                  reads=reads, writes=[pkey])

    def tr(self, out, in_, ident, reads, pkey):
        self.P.op("pe", lambda e: e.transpose(out=out, in_=in_, identity=ident), reads=reads, writes=[pkey])

    def act(self, out, in_, func, reads, writes, bias=None, scale=None, accum_out=None, eng="act"):
        kw = {}
        if bias is not None:
            kw["bias"] = bias
        if scale is not None:
            kw["scale"] = scale
        if accum_out is not None:
            kw["accum_out"] = accum_out
        self.P.op(eng, lambda e: e.activation(out=out, in_=in_, func=func, **kw), reads=reads, writes=writes)

    def tt(self, eng, out, in0, in1, op, reads, writes):
        self.P.op(eng, lambda e: e.tensor_tensor(out=out, in0=in0, in1=in1, op=op), reads=reads, writes=writes)

    def ts(self, eng, out, in0, s1, s2, op0, op1, reads, writes):
        if s2 is None:
            self.P.op(eng, lambda e: e.tensor_scalar(out=out, in0=in0, scalar1=s1, scalar2=None, op0=op0),
                      reads=reads, writes=writes)
        else:
            self.P.op(eng, lambda e: e.tensor_scalar(out=out, in0=in0, scalar1=s1, scalar2=s2, op0=op0, op1=op1),
                      reads=reads, writes=writes)

    def stt(self, out, in0, scalar, in1, op0, op1, reads, writes):
        self.P.op("dve", lambda e: e.scalar_tensor_tensor(out=out, in0=in0, scalar=scalar, in1=in1,
                                                           op0=op0, op1=op1), reads=reads, writes=writes)

    def copy(self, eng, out, in_, reads, writes):
        if eng == "act":
            self.P.op("act", lambda e: e.activation(out=out, in_=in_, func=AF.Copy), reads=reads, writes=writes)
        else:
            self.P.op(eng, lambda e: e.tensor_copy(out=out, in_=in_), reads=reads, writes=writes)

    def memset(self, eng, ap, val, writes):
        self.P.op(eng, lambda e: e.memset(ap, val), writes=writes)

    def dma(self, q, out, in_, reads, writes, key):
        self.P.op(q, lambda e: e.dma_start(out=out, in_=in_), reads=reads, writes=writes, dma=key)


def setup_globals(C, cin):
    P = C.P
    C.x_sb = C.sb("x_sb", [128, 16, D], F32)
    C.hnT = C.sb("hnT", [128, 8, S], BF16)
    C.ident = C.sb("ident_bf", [128, 128], BF16)
    C.identf = C.sb("ident_f", [128, 128], F32)
    C.tri = C.sb("tri_f", [128, 128], F32)
    C.trin = C.sb("trin_f", [128, 128], F32)
    C.trin16 = C.sb("trin16_f", [128, 128], F32)
    C.ones = C.sb("ones_f", [128, 128], F32)
    C.cb = C.sb("cbias", [128, 4], F32)
    C.memset("dve", C.cb[:, 0:1], math.log(1.0 / 16.0), ["const"])
    C.memset("dve", C.cb[:, 1:2], 1.0, ["const"])
    C.psf = [C.ps("psf%d" % i, [128, 512], F32) for i in range(6)]
    C.psb = [C.ps("psb%d" % i, [128, 1024], BF16) for i in range(2)]
    for t, nm in ((C.ident, "ident_bf"), (C.identf, "ident_f"), (C.tri, "tri_f"), (C.trin, "trin_f"),
                  (C.trin16, "trin16_f"), (C.ones, "ones_f")):
        C.dma("sp", t[:], cin[nm], [], ["const"], "const")


def phase_load_x(C, x_d):
    xv = x_d.rearrange("(c s) d -> c s d", s=16)
    for q in range(4):
        C.dma("sp", C.x_sb[:, 4 * q:4 * q + 4, :], xv[:, 4 * q:4 * q + 4, :], [], [("x", s) for s in range(4 * q, 4 * q + 4)],
              ("x", q))


def rms_stats(C, st, tag):
    junk = C.sb("junk_" + tag, [128, D], BF16, st)
    ssq = C.sb("ssq_" + tag, [128, 16], F32, st)
    rstd = C.sb("rstd_" + tag, [128, 16], F32, st)
    for s in range(16):
        C.act(junk[:], C.x_sb[:, s, :], AF.Square, [("x", s)], ["junk", ("ssq", s)], accum_out=ssq[:, s:s + 1])
    C.ts("dve", rstd[:], ssq[:], 1.0 / D, EPS, ALU.mult, ALU.add, [("ssq", s) for s in range(16)], ["rstd"])
    C.act(rstd[:], rstd[:], AF.Sqrt, ["rstd"], ["rstd"])
    C.P.op("dve", lambda e: e.reciprocal(out=rstd[:], in_=rstd[:]), reads=["rstd"], writes=["rstd"])
    return rstd


def phase_norm(C, g_d, tag):
    st = ExitStack()
    with st:
        g_sb = C.sb("g_" + tag, [128, D], F32, st)
        hn = [C.sb("hn%d_%s" % (i, tag), [128, D], BF16, st) for i in range(3)]
        C.dma("sp", g_sb[:], g_d, [], ["g"], "g")
        rstd = rms_stats(C, st, tag)
        def mk_hn(s):
            C.stt(hn[s % 3][:], C.x_sb[:, s, :], rstd[:, s:s + 1], g_sb[:], ALU.mult, ALU.mult,
                  [("x", s), "rstd", "g"], [("hn", s % 3)])

        mk_hn(0)
        mk_hn(1)
        for s in range(16):
            h = hn[s % 3]
            hk = ("hn", s % 3)
            pt = C.psb[s % 2]
            pk = ("psb", s % 2)
            for j in range(8):
                C.tr(pt[:, 128 * j:128 * j + 128], h[:, 128 * j:128 * j + 128], C.ident[:], [hk, "const"], pk)
            if s + 2 < 16:
                mk_hn(s + 2)
            dst = C.hnT[:].rearrange("p j (c s) -> p j c s", s=16)[:, :, :, s]
            src = pt[:].rearrange("p (j c) -> p j c", j=8)
            C.copy("act" if s % 2 else "dve", dst, src, [pk], [("hnT", s)])
        C.P.emit_phase()


def phase_final(C, g_d, out_d):
    st = ExitStack()
    with st:
        g_sb = C.sb("g_fin", [128, D], F32, st)
        stage = [C.sb("ostage%d" % i, [128, 4, D], F32, st) for i in range(2)]
        C.dma("sp", g_sb[:], g_d, [], ["g"], "g")
        rstd = rms_stats(C, st, "fin")
        ov = out_d.rearrange("(c s) d -> c s d", s=16)
        for q in range(4):
            sg = stage[q % 2]
            for s4 in range(4):
                s = 4 * q + s4
                C.stt(sg[:, s4, :], C.x_sb[:, s, :], rstd[:, s:s + 1], g_sb[:], ALU.mult, ALU.mult,
                      [("x", s), "rstd", "g"], [("ostage", q % 2)])
            C.dma("sp", ov[:, 4 * q:4 * q + 4, :], sg[:], [("ostage", q % 2)], [], ("out", q % 2))
        C.P.emit_phase()


def run_interleaved(gens):
    gens = list(gens)
    while gens:
        for g in list(gens):
            try:
                next(g)
            except StopIteration:
                gens.remove(g)


def load_w(C, dst, w2d, f0, nf, key, rows0=0, nj=8):
    src = w2d[rows0:rows0 + 128 * nj, f0:f0 + nf].rearrange("(j p) f -> p j f", p=128)
    C.dma("pool", dst, src, [], [key], key)


def out_proj(C, yT, nfc, wo, ykey, wkey):
    n = 0
    for s in range(16):
        for dh in range(2):
            pm = C.psf[n % 2]
            pk = ("psf", n % 2)
            n += 1
            yv = yT[:, :, :].rearrange("p f (c s) -> p f c s", s=16)
            for fc in range(nfc):
                C.mm(pm[:], yv[:, fc, :, s], wo[:, fc, 512 * dh:512 * dh + 512], fc == 0, fc == nfc - 1,
                     [ykey, wkey], pk)
            xs = C.x_sb[:, s, 512 * dh:512 * dh + 512]
            C.tt("dve", xs, pm[:], xs, ALU.add, [pk, ("x", s)], [("x", s)])


GLA_Q0, GLA_K0, GLA_V0, GLA_Z0, GLA_R0 = 0, 1024, 2048, 4096, 6144


def phase_gla_prep(C, w_in, wab_d):
    st = ExitStack()
    with st:
        wr = C.sb("wr", [128, 8, 16], BF16, st)
        load_w(C, wr[:], w_in, GLA_R0, 16, "wr")
        C.memset("dve", C.rT1[:], 1.0, ["rT1"])
        C.dma("sp", C.wab[0:17, :], wab_d, [], ["wab"], "wab")
        for tg in range(4):
            pm = C.psf[tg % 2]
            pk = ("psf", tg % 2)
            for j in range(8):
                C.mm(pm[0:16, :], wr[:, j, :], C.hnT[:, j, 512 * tg:512 * tg + 512], j == 0, j == 7, ["wr", "hnT"], pk)
            C.copy("dve", C.rT1[0:16, 512 * tg:512 * tg + 512], pm[0:16, :], [pk, "rT1"], ["rT1"])
        C.P.emit_phase()


def phase_gla_head(C, h, w_in, hg_d, w_out):
    st = ExitStack()
    with st:
        try:
            _gla_head(C, st, h, w_in, hg_d, w_out)
        except StopPhase:
            pass
        C.P.emit_phase()


def _gla_head(C, st, h, w_in, hg_d, w_out):
    if True:
        wbuf = C.sb("g_wbuf", [128, 8, 512], BF16, st)
        wo = wbuf[:].rearrange("p (fc a) f -> p fc (a f)", a=2)
        wqk = C.wqk
        qT = C.sb("g_qT", [128, 2, S], BF16, st)
        kT = C.sb("g_kT", [128, 2, S], BF16, st)
        v_sb = C.sb("g_v", [128, 16, 512], BF16, st)
        zT = C.sb("g_zT", [128, 4, S], BF16, st)
        yT = C.sb("g_yT", [128, 4, S], BF16, st)
        hgc = C.sb("g_hgc", [128, 4], F32, st)
        Sf = C.sb("g_Sf", [128, 2, 512], F32, st)
        Sb = C.sb("g_Sb", [128, 2, 512], BF16, st)
        la = C.sb("g_la", [128, 256], F32, st)
        ep = C.sb("g_ep", [128, 128], F32, st)
        em = C.sb("g_em", [128, 128], F32, st)
        ee = C.sb("g_ee", [128, 128], F32, st)
        gcol = C.sb("g_gcol", [128, 2], F32, st)
        egc = C.sb("g_egc", [128, 2], F32, st)
        qs = C.sb("g_qs", [128, 2, 128], BF16, st)
        ks = C.sb("g_ks", [128, 2, 128], BF16, st)
        keT = C.sb("g_keT", [128, 2, 128], BF16, st)
        ke = C.sb("g_ke", [128, 256], BF16, st)
        attn = C.sb("g_attn", [128, 128], BF16, st)
        ssq = C.sb("g_ssq", [128, 2], F32, st)
        rstd = C.sb("g_rstd", [128, 2], F32, st)
        C.memset("dve", ssq[:], 1.0, ["g_ssq"])
        on = C.sb("g_on", [128, 512], BF16, st)
        junk = on

        C.dma("sp", hgc[:], hg_d[:, 4 * h:4 * h + 4], [], ["hgc"], "hgc")
        load_w(C, wbuf[:], w_in, GLA_V0 + 512 * h, 512, "g_wbuf")
        n = 0
        for which, dstT in ((0, qT), (1, kT)):
            for kt in range(2):
                for tg in range(4):
                    pm = C.psf[n % 4]
                    pk = ("psf", n % 4)
                    for j in range(8):
                        C.mm(pm[:], wqk[:, j, 256 * which + 128 * kt:256 * which + 128 * kt + 128],
                             C.hnT[:, j, 512 * tg:512 * tg + 512], j == 0, j == 7, ["g_wqk", "hnT"], pk)
                    C.copy("act" if n % 2 else "dve", dstT[:, kt, 512 * tg:512 * tg + 512], pm[:], [pk],
                           ["g_qT" if which == 0 else "g_kT"])
                    n += 1
        if h < 3:
            load_w(C, wqk[:, :, 0:256], w_in, GLA_Q0 + 256 * (h + 1), 256, "g_wqk")
            load_w(C, wqk[:, :, 256:512], w_in, GLA_K0 + 256 * (h + 1), 256, "g_wqk")
        for k in range(16):
            pm = C.psf[n % 4]
            pk = ("psf", n % 4)
            for j in range(8):
                C.mm(pm[:], C.hnT[:, j, 128 * k:128 * k + 128], wbuf[:, j, :], j == 0, j == 7, ["g_wbuf", "hnT"], pk)
            C.copy("act" if n % 2 else "dve", v_sb[:, k, :], pm[:], [pk], ["g_v"])
            n += 1
        load_w(C, wbuf[:], w_in, GLA_Z0 + 512 * h, 512, "g_wbuf")
        for fc in range(4):
            for tg in range(4):
                pm = C.psf[n % 4]
                pk = ("psf", n % 4)
                for j in range(8):
                    C.mm(pm[:], wbuf[:, j, 128 * fc:128 * fc + 128], C.hnT[:, j, 512 * tg:512 * tg + 512],
                         j == 0, j == 7, ["g_wbuf", "hnT"], pk)
                C.act(zT[:, fc, 512 * tg:512 * tg + 512], pm[:], AF.Silu, [pk], ["g_zT"])
                n += 1

        load_w(C, wo, w_out, 0, D, "g_wbuf", rows0=512 * h, nj=4)
        nchunk = {"proj": 0, "chunk1": 1, "chunk2": 2}.get(BISECT, 16)
        qs2 = [qs, C.sb("g_qs1", [128, 2, 128], BF16, st)]
        ke2 = [ke, C.sb("g_ke1", [128, 256], BF16, st)]
        attn2 = [attn, C.sb("g_attn1", [128, 128], BF16, st)]
        egc2 = [egc, C.sb("g_egc1", [128, 2], F32, st)]

        ep2 = [ep, ep]
        em2 = [em, C.sb("g_em1", [128, 128], F32, st)]
        ee2 = [ee, C.sb("g_ee1", [128, 128], F32, st)]

        def genA(k):
            b = k % 2
            tsl = slice(128 * k, 128 * k + 128)
            qsb, keb, attnb, egcb = qs2[b], ke2[b], attn2[b], egc2[b]
            p0, k0 = C.psf[0], ("psf", 0)
            C.mm(p0[:, 0:256], C.rT1[0:17, tsl], C.wab[0:17, 256 * h:256 * h + 256], True, True, ["rT1", "wab"], k0)
            yield
            C.act(la[:], p0[:, 0:256], AF.Exp, [k0], ["g_la"], scale=-1.0)
            yield
            C.act(la[:], la[:], AF.Ln, ["g_la", "const"], ["g_la"], bias=C.cb[:, 1:2])
            yield
            pk_ = [(C.psf[1], ("psf", 1)), (C.psf[2], ("psf", 2))]
            for kt in range(2):
                p1, k1 = pk_[kt]
                C.mm(p1[:, 0:128], la[:, 128 * kt:128 * kt + 128], C.trin16[:], True, True, ["g_la", "const"], k1)
            yield
            for kt in range(2):
                p1, k1 = pk_[kt]
                C.copy("dve", gcol[:, kt:kt + 1], p1[:, 127:128], [k1], [("g_gcol", kt)])
                C.act(em2[kt][:], p1[:, 0:128], AF.Exp, [k1], [("g_em", kt)], scale=-1.0)
            yield
            for kt in range(2):
                p1, k1 = pk_[kt]
                C.act(ep[:], p1[:, 0:128], AF.Exp, [k1, "const"], ["g_ep"], bias=C.cb[:, 0:1])
                C.tt("dve", qsb[:, kt, :], qT[:, kt, tsl], ep[:], ALU.mult, ["g_qT", "g_ep"], [("g_qs", kt, b)])
                yield
            for kt in range(2):
                p1, k1 = pk_[kt]
                C.act(ee2[kt][:], p1[:, 0:128], AF.Exp, [k1, ("g_gcol", kt)], [("g_ee", kt)], scale=-1.0,
                      bias=gcol[:, kt:kt + 1])
                C.tt("pool", ks[:, kt, :], kT[:, kt, tsl], em2[kt][:], ALU.mult, ["g_kT", ("g_em", kt)], [("g_ks", kt)])
            yield
            C.act(egcb[:, 0:2], gcol[:, 0:2], AF.Exp, [("g_gcol", 0), ("g_gcol", 1)], [("g_egc", 0, b), ("g_egc", 1, b)])
            for kt in range(2):
                C.tt("pool", keT[:, kt, :], kT[:, kt, tsl], ee2[kt][:], ALU.mult, ["g_kT", ("g_ee", kt)], [("g_keT", kt)])
            yield
            for kt in range(2):
                C.mm(p0[:, 0:128], ks[:, kt, :], qsb[:, kt, :], kt == 0, kt == 1, [("g_ks", kt), ("g_qs", kt, b)], k0)
            yield
            pb, kb = C.psb[0], ("psb", 0)
            for kt in range(2):
                C.tr(pb[:, 128 * kt:128 * kt + 128], keT[:, kt, :], C.ident[:], [("g_keT", kt), "const"], kb)
            C.tt("dve", attnb[:], p0[:, 0:128], C.tri[:], ALU.mult, [k0, "const"], [("g_attn", b)])
            yield
            C.copy("act", keb[:], pb[:, 0:256], [kb], [("g_ke", 0, b), ("g_ke", 1, b)])
            yield

        def genBs(k):
            b = k % 2
            keb, egcb = ke2[b], egc2[b]
            if k < 15:
                for kt in range(2):
                    p4, k4 = C.psf[4 + kt], ("psf", 4 + kt)
                    C.mm(p4[:], keb[:, 128 * kt:128 * kt + 128], v_sb[:, k, :], True, True, [("g_ke", kt, b), "g_v"], k4)
                yield
                for kt in range(2):
                    p4, k4 = C.psf[4 + kt], ("psf", 4 + kt)
                    if k == 0:
                        C.copy("dve", Sf[:, kt, :], p4[:], [k4], [("g_Sf", kt)])
                    else:
                        C.stt(Sf[:, kt, :], Sf[:, kt, :], egcb[:, kt:kt + 1], p4[:], ALU.mult, ALU.add,
                              [k4, ("g_egc", kt, b), ("g_Sf", kt)], [("g_Sf", kt)])
                    yield
                for kt in range(2):
                    C.copy("act", Sb2[(k + 1) % 2][:, kt, :], Sf[:, kt, :], [("g_Sf", kt)], [("g_Sb", kt, (k + 1) % 2)])
                    yield

        def genBo(k):
            b = k % 2
            tsl = slice(128 * k, 128 * k + 128)
            qsb, attnb = qs2[b], attn2[b]
            p3, k3 = C.psf[3], ("psf", 3)
            C.mm(p3[:], attnb[:], v_sb[:, k, :], True, k == 0, [("g_attn", b), "g_v"], k3)
            if k > 0:
                for kt in range(2):
                    C.mm(p3[:], qsb[:, kt, :], Sb2[b][:, kt, :], False, kt == 1, [("g_qs", kt, b), ("g_Sb", kt, b)], k3)
            yield
            C.act(junk[:], p3[:], AF.Square, [k3], ["g_on", "g_ssq"], accum_out=ssq[:, 0:1])
            yield
            C.ts("dve", rstd[:], ssq[:], 1.0 / 512, EPS, ALU.mult, ALU.add, ["g_ssq"], ["g_rstd"])
            yield
            C.act(rstd[:], rstd[:], AF.Sqrt, ["g_rstd"], ["g_rstd"])
            yield
            C.P.op("dve", lambda e: e.reciprocal(out=rstd[:], in_=rstd[:]), reads=["g_rstd"], writes=["g_rstd"])
            yield
            C.act(on[:], p3[:], AF.Copy, [k3, "g_rstd"], ["g_on"], scale=rstd[:, 0:1])
            yield
            pb, kb = C.psb[1], ("psb", 1)
            for fc in range(4):
                C.tr(pb[:, 128 * fc:128 * fc + 128], on[:, 128 * fc:128 * fc + 128], C.ident[:], ["g_on", "const"], kb)
            yield
            for fc in range(4):
                C.stt(yT[:, fc, tsl], pb[:, 128 * fc:128 * fc + 128], hgc[:, fc:fc + 1], zT[:, fc, tsl],
                      ALU.mult, ALU.mult, [kb, "hgc", "g_zT"], ["g_yT"])
                yield

        Sb2 = [Sb, C.sb("g_Sb1", [128, 2, 512], BF16, st)]
        if nchunk > 0:
            run_interleaved([genA(0)])
        for k in range(nchunk):
            gens = [genBs(k), genBo(k)]
            if k + 1 < nchunk:
                gens.insert(0, genA(k + 1))
            run_interleaved(gens)
        if BISECT in (None, "all1"):
            out_proj(C, yT, 4, wo, "g_yT", "g_wbuf")


BISECT = None


class StopPhase(Exception):
    pass


def cut(tag):
    if BISECT == tag:
        raise StopPhase()


def layer1(C, cin):
    C.rT1 = C.sb("rT1", [32, S], F32)
    C.wab = C.sb("wab", [32, 1024], F32)
    C.wqk = C.sb("g_wqk", [128, 8, 512], BF16)
    load_w(C, C.wqk[:, :, 0:256], cin["od_w_in"], GLA_Q0, 256, "g_wqk")
    load_w(C, C.wqk[:, :, 256:512], cin["od_w_in"], GLA_K0, 256, "g_wqk")
    phase_norm(C, cin["norm_g1"], "l1")
    if BISECT == "norm":
        return
    phase_gla_prep(C, cin["od_w_in"], cin["gla_wab"])
    if BISECT == "prep":
        return
    for h in range(4 if BISECT is None else 1):
        phase_gla_head(C, h, cin["od_w_in"], cin["gla_head_g"], cin["od_w_out"])


EV_Q0, EV_K0, EV_V0, EV_O0, EV_I0, EV_U0, EV_Z0 = 0, 512, 1024, 2048, 3072, 3080, 4104


def phase_ml_prep(C, w_in, bias_d):
    st = ExitStack()
    with st:
        wif = C.sb("wif", [128, 8, 8], BF16, st)
        bias = C.sb("ifbias", [128, 128], F32, st)
        G = C.sb("gatesG", [128, 16, 8], F32, st)
        load_w(C, wif[:], w_in, EV_I0, 8, "wif")
        C.dma("sp", bias[:], bias_d, [], ["ifbias"], "ifbias")
        pm, pk = C.psf[0], ("psf", 0)
        for k in range(16):
            for j in range(8):
                C.mm(pm[:, 8 * k:8 * k + 8], C.hnT[:, j, 128 * k:128 * k + 128], wif[:, j, :], j == 0, j == 7,
                     ["wif", "hnT"], pk)
        C.tt("dve", G[:].rearrange("p k g -> p (k g)"), pm[:, 0:128], bias[:], ALU.add, [pk, "ifbias"], ["gatesG"])
        lfv = C.lfn[:].rearrange("p (k h) -> p k h", h=4)
        C.act(lfv, G[:, :, 4:8], AF.Exp, ["gatesG"], ["lfn"], scale=-1.0)
        C.act(C.lfn[:], C.lfn[:], AF.Ln, ["lfn", "const"], ["lfn"], bias=C.cb[:, 1:2])
        p1, k1 = C.psf[1], ("psf", 1)
        C.mm(p1[:, 0:64], C.tri[:], C.lfn[:], True, True, ["lfn", "const"], k1)
        C.tt("dve", C.cbias[:].rearrange("p (k h) -> p k h", h=4), p1[:, 0:64].rearrange("p (k h) -> p k h", h=4),
             G[:, :, 0:4], ALU.add, [k1, "gatesG"], ["cbias"])
        C.P.emit_phase()


def phase_ml_head(C, h, w_in, w_out, cw_d, cb_d, hg_d):
    st = ExitStack()
    with st:
        try:
            _ml_head(C, st, h, w_in, w_out, cw_d, cb_d, hg_d)
        except StopPhase:
            pass
        C.P.emit_phase()


def _ml_head(C, st, h, w_in, w_out, cw_d, cb_d, hg_d):
    wq = C.sb("m_wq", [128, 8, 256], BF16, st)
    wvo = C.sb("m_wvo", [128, 8, 512], BF16, st)
    wz = C.sb("m_wz", [128, 8, 256], BF16, st)
    wo = C.sb("m_wo", [128, 2, D], BF16, st)
    cw = C.sb("m_cw", [128, 2, 4], F32, st)
    cbv = C.sb("m_cb", [128, 8], F32, st)
    hgc = C.sb("m_hgc", [128, 8], F32, st)
    pre = C.sb("m_pre", [128, S + 4], F32, st)
    acc = C.sb("m_acc", [128, S], F32, st)
    qT = C.sb("m_qT", [128, S], BF16, st)
    kT = C.sb("m_kT", [128, S], BF16, st)
    v_sb = C.sb("m_v", [128, 16, 258], BF16, st)
    o_sb = C.sb("m_o", [128, 16, 256], BF16, st)
    zT = C.sb("m_zT", [128, 2, S], BF16, st)
    yT = C.sb("m_yT", [128, 2, S], BF16, st)
    Cf = C.sb("m_Cf", [128, 258], F32, st)
    Cb = C.sb("m_Cb", [128, 258], BF16, st)
    lfbc = C.sb("m_lfbc", [128, 128], F32, st)
    eB = C.sb("m_eB", [128, 128], F32, st)
    w = C.sb("m_w", [128, 128], F32, st)
    wm = C.sb("m_wm", [128, 128], F32, st)
    sc = C.sb("m_sc", [128, 128], BF16, st)
    qs = C.sb("m_qs", [128, 128], BF16, st)
    vt = C.sb("m_vt", [128, 258], BF16, st)
    kTok = C.sb("m_kTok", [128, 128], BF16, st)
    hg = C.sb("m_hg", [128, 256], F32, st)
    hgn = C.sb("m_hgn", [128, 256], BF16, st)
    junk = C.sb("m_junk", [128, 256], BF16, st)
    ssq = C.sb("m_ssq", [128, 2], F32, st)
    rstd = C.sb("m_rstd", [128, 2], F32, st)
    r = C.sb("m_r", [128, 2], F32, st)
    gB = C.sb("m_gB", [128, 2], F32, st)
    wk4 = C.sb("m_wk4", [128, 4], F32, st)

    C.dma("sp", cw[:, 0, :], cw_d[128 * h:128 * h + 128, :], [], ["m_cw"], "m_cw")
    C.dma("sp", cw[:, 1, :], cw_d[512 + 128 * h:512 + 128 * h + 128, :], [], ["m_cw"], "m_cw")
    C.dma("sp", cbv[:], cb_d, [], ["m_cb"], "m_cb")
    C.dma("sp", hgc[:], hg_d, [], ["m_hgc"], "m_hgc")
    load_w(C, wq[:, :, 0:128], w_in, EV_Q0 + 128 * h, 128, "m_wq")
    load_w(C, wq[:, :, 128:256], w_in, EV_K0 + 128 * h, 128, "m_wq")
    load_w(C, wvo[:, :, 0:256], w_in, EV_V0 + 256 * h, 256, "m_wvo")
    load_w(C, wvo[:, :, 256:512], w_in, EV_O0 + 256 * h, 256, "m_wvo")
    load_w(C, wz[:], w_in, EV_Z0 + 256 * h, 256, "m_wz")
    load_w(C, wo[:], w_out, 0, D, "m_wo", rows0=256 * h, nj=2)
    C.memset("dve", ssq[:], 1.0, ["m_ssq"])
    C.memset("dve", pre[:, 0:4], 0.0, ["m_pre0"])
    C.memset("pool", v_sb[:, :, 256:258], 1.0, ["m_vones"])
    cut("mc_dma")

    n = 0
    for which in range(2):
        for tg in range(4):
            pm, pk = C.psf[n % 4], ("psf", n % 4)
            n += 1
            for j in range(8):
                C.mm(pm[:], wq[:, j, 128 * which:128 * which + 128], C.hnT[:, j, 512 * tg:512 * tg + 512],
                     j == 0, j == 7, ["m_wq", "hnT"], pk)
            C.copy("act", pre[:, 4 + 512 * tg:4 + 512 * tg + 512], pm[:], [pk], ["m_pre"])
        cut("mc_proj")
        C.ts("dve", acc[:], pre[:, 1:1 + S], cw[:, which, 0:1], None, ALU.mult, None, ["m_pre", "m_pre0", "m_cw"], ["m_acc"])
        for j in range(1, 4):
            C.stt(acc[:], pre[:, 1 + j:1 + j + S], cw[:, which, j:j + 1], acc[:], ALU.mult, ALU.add,
                  ["m_pre", "m_pre0", "m_cw", "m_acc"], ["m_acc"])
        cut("mc_conv")
        bcol = cbv[:, 4 * which + h:4 * which + h + 1]
        if which == 0:
            C.act(acc[:], acc[:], AF.Silu, ["m_acc", "m_cb"], ["m_acc"], bias=bcol)
            C.ts("dve", qT[:], acc[:], 128.0 ** -0.5, None, ALU.mult, None, ["m_acc"], ["m_qT"])
        else:
            C.act(kT[:], acc[:], AF.Silu, ["m_acc", "m_cb"], ["m_kT"], bias=bcol)
    cut("mc_qk")
    for k in range(16):
        pm, pk = C.psf[n % 4], ("psf", n % 4)
        n += 1
        for j in range(8):
            C.mm(pm[:], C.hnT[:, j, 128 * k:128 * k + 128], wvo[:, j, :], j == 0, j == 7, ["m_wvo", "hnT"], pk)
        C.copy("dve", v_sb[:, k, 0:256], pm[:, 0:256], [pk], [("m_v", k)])
        C.act(o_sb[:, k, :], pm[:, 256:512], AF.Sigmoid, [pk], [("m_o", k)])
    cut("mc_vo")
    for fc in range(2):
        for tg in range(4):
            pm, pk = C.psf[n % 4], ("psf", n % 4)
            n += 1
            for j in range(8):
                C.mm(pm[:], wz[:, j, 128 * fc:128 * fc + 128], C.hnT[:, j, 512 * tg:512 * tg + 512], j == 0, j == 7,
                     ["m_wz", "hnT"], pk)
            C.act(zT[:, fc, 512 * tg:512 * tg + 512], pm[:], AF.Silu, [pk], ["m_zT"])

    nchunk = 16
    if BISECT and BISECT.startswith("m_"):
        nchunk = int(BISECT[2:])
    sc2 = [sc, C.sb("m_sc1", [128, 128], BF16, st)]
    qs2 = [qs, C.sb("m_qs1", [128, 128], BF16, st)]
    eg2 = [C.sb("m_eg%d" % i, [128, 2], F32, st) for i in range(2)]

    Cb2 = [Cb, C.sb("m_Cb1", [128, 258], BF16, st)]

    def genA(k):
        b = k % 2
        tsl = slice(128 * k, 128 * k + 128)
        col = 4 * k + h
        p0, k0 = C.psf[0], ("psf", 0)
        p1, k1 = C.psf[1], ("psf", 1)
        p3, k3 = C.psf[3 + b], ("psf", 3 + b)
        C.ts("pool", lfbc[:], C.ones[:], C.lfn[:, col:col + 1], 0.0, ALU.mult, ALU.add, ["const", "lfn"], ["m_lfbc"])
        C.mm(p0[:, 0:128], kT[:, tsl], qT[:, tsl], True, True, ["m_kT", "m_qT"], k0)
        yield
        C.mm(p1[:, 0:128], lfbc[:], C.trin[:], True, True, ["m_lfbc", "const"], k1)
        pb0, kb0 = C.psb[0], ("psb", 0)
        if k < 15:
            C.tr(pb0[:, 0:128], kT[:, tsl], C.ident[:], ["m_kT", "const"], kb0)
        yield
        C.copy("dve", gB[:, 0:1], p1[:, 127:128], [k1], ["m_gB"])
        C.act(w[:], p1[:, 0:128], AF.Exp, [k1, "cbias"], ["m_w"], bias=C.cbias[:, col:col + 1])
        yield
        C.act(eB[:], p1[:, 0:128], AF.Exp, [k1], ["m_eB"])
        C.tt("pool", wm[:], w[:], C.tri[:], ALU.mult, ["m_w", "const"], ["m_wm"])
        yield
        if k < 15:
            C.act(wk4[:], C.cbias[:, 4 * k:4 * k + 4], AF.Exp, ["cbias", "m_gB"], ["m_wk4"], bias=gB[:, 0:1])
        C.tt("dve", sc2[b][:], p0[:, 0:128], wm[:], ALU.mult, [k0, "m_wm"], [("m_sc", b)])
        yield
        C.tt("pool", qs2[b][:], qT[:, tsl], eB[:], ALU.mult, ["m_qT", "m_eB"], [("m_qs", b)])
        C.copy("dve", eg2[b][:, 0:2], eB[:, 126:128], ["m_eB"], [("m_eg", b)])
        yield
        if k < 15:
            C.copy("act", kTok[:], pb0[:, 0:128], [kb0], ["m_kTok"])
            C.ts("dve", vt[:, 0:257], v_sb[:, k, 0:257], wk4[:, h:h + 1], None, ALU.mult, None,
                 [("m_v", k), "m_vones", "m_wk4"], ["m_vt"])
            yield
            C.mm(p3[:, 0:257], kTok[:], vt[:, 0:257], True, True, ["m_kTok", "m_vt"], k3)
            yield

    def genBs(k):
        b = k % 2
        p3, k3 = C.psf[3 + b], ("psf", 3 + b)
        if k < 15:
            if k == 0:
                C.copy("dve", Cf[:, 0:257], p3[:, 0:257], [k3], ["m_Cf"])
            else:
                C.stt(Cf[:, 0:257], Cf[:, 0:257], eg2[b][:, 1:2], p3[:, 0:257], ALU.mult, ALU.add,
                      [k3, ("m_eg", b), "m_Cf"], ["m_Cf"])
            yield
            C.copy("act", Cb2[(k + 1) % 2][:, 0:257], Cf[:, 0:257], ["m_Cf"], [("m_Cb", (k + 1) % 2)])
            yield

    r2 = [r, C.sb("m_r1", [128, 2], F32, st)]
    hg2 = [hg, C.sb("m_hg1", [128, 256], F32, st)]
    hgn2 = [hgn, C.sb("m_hgn1", [128, 256], BF16, st)]
    junk2 = [junk, C.sb("m_junk1", [128, 256], BF16, st)]
    ssq2 = [ssq, C.sb("m_ssq1", [128, 2], F32, st)]
    rstd2 = [rstd, C.sb("m_rstd1", [128, 2], F32, st)]
    C.memset("dve", ssq2[1][:], 1.0, [("m_ssq", 1)])

    def genBo(k):
        b = k % 2
        tsl = slice(128 * k, 128 * k + 128)
        p2, k2 = (C.psf[2], ("psf", 2)) if b == 0 else (C.psf[5], ("psf", 5))
        r_, hg_, hgn_, junk_, ssq_, rstd_ = r2[b], hg2[b], hgn2[b], junk2[b], ssq2[b], rstd2[b]
        kr, kr1, khg, kjk, kss, krs, khn = (("m_r", b), ("m_r1", b), ("m_hg", b), ("m_junk", b), ("m_ssq", b),
                                            ("m_rstd", b), ("m_hgn", b))
        C.mm(p2[:, 0:257], sc2[b][:], v_sb[:, k, 0:257], True, k == 0, [("m_sc", b), ("m_v", k), "m_vones"], k2)
        if k > 0:
            C.mm(p2[:, 0:257], qs2[b][:], Cb2[b][:, 0:257], False, True, [("m_qs", b), ("m_Cb", b)], k2)
        yield
        C.ts("dve", r_[:, 0:1], p2[:, 256:257], -1.0, 1.0, ALU.mult, ALU.max, [k2], [kr])
        C.ts("dve", r_[:, 1:2], p2[:, 256:257], 1.0, None, ALU.max, None, [k2], [kr1])
        yield
        C.tt("dve", r_[:, 0:1], r_[:, 0:1], r_[:, 1:2], ALU.max, [kr, kr1], [kr])
        yield
        C.P.op("dve", lambda e: e.reciprocal(out=r_[:, 0:1], in_=r_[:, 0:1]), reads=[kr], writes=[kr])
        yield
        C.stt(hg_[:], p2[:, 0:256], r_[:, 0:1], o_sb[:, k, :], ALU.mult, ALU.mult, [k2, kr, ("m_o", k)], [khg])
        yield
        C.act(junk_[:], hg_[:], AF.Square, [khg], [kjk, kss], accum_out=ssq_[:, 0:1])
        yield
        C.ts("dve", rstd_[:], ssq_[:], 1.0 / 256, EPS, ALU.mult, ALU.add, [kss], [krs])
        yield
        C.act(rstd_[:], rstd_[:], AF.Sqrt, [krs], [krs])
        yield
        C.P.op("dve", lambda e: e.reciprocal(out=rstd_[:], in_=rstd_[:]), reads=[krs], writes=[krs])
        yield
        C.act(hgn_[:], hg_[:], AF.Copy, [khg, krs], [khn], scale=rstd_[:, 0:1])
        yield
        pb, kb = C.psb[1], ("psb", 1)
        for fc in range(2):
            C.tr(pb[:, 128 * fc:128 * fc + 128], hgn_[:, 128 * fc:128 * fc + 128], C.ident[:], [khn, "const"], kb)
        yield
        for fc in range(2):
            C.stt(yT[:, fc, tsl], pb[:, 128 * fc:128 * fc + 128], hgc[:, 2 * h + fc:2 * h + fc + 1], zT[:, fc, tsl],
                  ALU.mult, ALU.mult, [kb, "m_hgc", "m_zT"], ["m_yT"])
            yield

    if nchunk > 0:
        run_interleaved([genA(0)])
    carry = []
    for k in range(nchunk):
        must = [genBs(k)]
        if k + 1 < nchunk:
            must.insert(0, genA(k + 1))
        bo = genBo(k)
        active = must + carry + [bo]
        must_left = list(must)
        while must_left:
            for g in list(active):
                try:
                    next(g)
                except StopIteration:
                    active.remove(g)
                    if g in must_left:
                        must_left.remove(g)
        for g in carry:
            if g in active:
                for _ in g:
                    pass
                active.remove(g)
        carry = [g for g in active]
    for g in carry:
        for _ in g:
            pass
    out_proj(C, yT, 2, wo, "m_yT", "m_wo")


def cmul(C, eng, outR, outI, aR, aI, bR, bI, t1, t2, rk, wk, coarse=False):
    k1, k2, kR, kI = (wk, wk, wk, wk) if coarse else (wk + "_t1", wk + "_t2", wk + "_R", wk + "_I")
    C.tt(eng, t1, aR, bR, ALU.mult, rk, [k1])
    C.tt(eng, t2, aI, bI, ALU.mult, rk, [k2])
    C.tt(eng, outR, t1, t2, ALU.subtract, [k1, k2], [kR])
    C.tt(eng, t1, aR, bI, ALU.mult, rk, [k1])
    C.tt(eng, t2, aI, bR, ALU.mult, rk, [k2])
    C.tt(eng, outI, t1, t2, ALU.add, [k1, k2], [kI])


def phase_s5_setup(C, cin, scr):
    st = ExitStack()
    with st:
        LR = C.sb("s_LR", [128, 32], F32, st)
        LI = C.sb("s_LI", [128, 32], F32, st)
        DT = C.sb("s_DT", [128, 32], F32, st)
        TH = C.sb("s_TH", [128, 32], F32, st)
        LD = C.sb("s_LD", [128, 32], F32, st)
        cs = C.sb("s_cs", [128, 32], F32, st)
        sn = C.sb("s_sn", [128, 32], F32, st)
        rho = C.sb("s_rho", [128, 32], F32, st)
        rhi = C.sb("s_rhi", [128, 32], F32, st)
        aiR = C.sb("s_aiR", [128, 32], F32, st)
        aiI = C.sb("s_aiI", [128, 32], F32, st)
        t1 = C.sb("s_t1", [128, 32], F32, st)
        t2 = C.sb("s_t2", [128, 32], F32, st)
        t3 = C.sb("s_t3", [128, 32], F32, st)
        fR = C.sb("s_fR", [128, 32], F32, st)
        fI = C.sb("s_fI", [128, 32], F32, st)
        pi2 = C.sb("s_pi2", [128, 1], F32, st)
        mD = C.sb("s_mD", [128, 128], F32, st)
        P0 = C.sb("s_P0", [128, 128], F32, st)
        dcol = C.sb("s_dcol", [128, 64], F32, st)
        ER, EI = C.ER, C.EI
        C.dma("sp", LR[:], cin["s5_lr"], [], ["s_in"], "s_in")
        C.dma("sp", LI[:], cin["s5_li"], [], ["s_in"], "s_in")
        C.dma("sp", DT[:], cin["s5_ldt"], [], ["s_in"], "s_in")
        C.dma("sp", mD[:], cin["s5_maskD"], [], ["s_in"], "s_in")
        C.dma("sp", P0[:], cin["s5_P0"], [], ["s_in"], "s_in")
        C.dma("sp", dcol[:], cin["s5_dcol"], [], ["s_in"], "s_in")
        C.memset("dve", pi2[:], math.pi / 2.0, ["s_pi2"])
        K = ["s_in", "s_k"]
        C.act(DT[:], DT[:], AF.Exp, ["s_in"], ["s_k"])
        C.tt("dve", TH[:], LI[:], DT[:], ALU.mult, K, ["s_k"])
        C.tt("dve", LD[:], LR[:], DT[:], ALU.mult, K, ["s_k"])
        C.act(rho[:], LD[:], AF.Exp, K, ["s_k"])
        C.act(rhi[:], LD[:], AF.Exp, K, ["s_k"], scale=-1.0)
        C.act(sn[:], TH[:], AF.Sin, K, ["s_k"], scale=1.0 / 16.0)
        C.act(cs[:], TH[:], AF.Sin, K + ["s_pi2"], ["s_k"], scale=-1.0 / 16.0, bias=pi2[:, 0:1])
        for _ in range(4):
            C.tt("dve", t1[:], cs[:], cs[:], ALU.mult, K, ["s_k"])
            C.tt("dve", t2[:], sn[:], sn[:], ALU.mult, K, ["s_k"])
            C.tt("dve", t3[:], cs[:], sn[:], ALU.mult, K, ["s_k"])
            C.tt("dve", cs[:], t1[:], t2[:], ALU.subtract, K, ["s_k"])
            C.ts("dve", sn[:], t3[:], 2.0, None, ALU.mult, None, K, ["s_k"])
        C.memset("dve", ER[:, :, 7:8], 1.0, ["s_k"])
        C.memset("dve", EI[:, :, 7:8], 0.0, ["s_k"])
        C.tt("dve", ER[:, :, 8], rho[:], cs[:], ALU.mult, K, ["s_k"])
        C.tt("dve", EI[:, :, 8], rho[:], sn[:], ALU.mult, K, ["s_k"])
        C.tt("dve", aiR[:], rhi[:], cs[:], ALU.mult, K, ["s_k"])
        C.tt("dve", aiI[:], rhi[:], sn[:], ALU.mult, K, ["s_k"])
        C.ts("dve", aiI[:], aiI[:], -1.0, None, ALU.mult, None, K, ["s_k"])
        for e in range(1, 16):
            cmul(C, "dve", ER[:, :, 8 + e], EI[:, :, 8 + e], ER[:, :, 7 + e], EI[:, :, 7 + e], ER[:, :, 8], EI[:, :, 8],
                 t1[:], t2[:], K, "s_k", coarse=True)
        C.copy("dve", ER[:, :, 6], aiR[:], K, ["s_k"])
        C.copy("dve", EI[:, :, 6], aiI[:], K, ["s_k"])
        for e in range(1, 7):
            cmul(C, "dve", ER[:, :, 6 - e], EI[:, :, 6 - e], ER[:, :, 7 - e], EI[:, :, 7 - e], aiR[:], aiI[:],
                 t1[:], t2[:], K, "s_k", coarse=True)
        C.tt("dve", t1[:], LR[:], LR[:], ALU.mult, K, ["s_k"])
        C.tt("dve", t2[:], LI[:], LI[:], ALU.mult, K, ["s_k"])
        C.tt("dve", t1[:], t1[:], t2[:], ALU.add, K, ["s_k"])
        C.P.op("dve", lambda e: e.reciprocal(out=t3[:], in_=t1[:]), reads=K, writes=["s_k"])
        C.ts("dve", t1[:], ER[:, :, 8], -1.0, None, ALU.add, None, K, ["s_k"])
        C.tt("dve", fR[:], t1[:], LR[:], ALU.mult, K, ["s_k"])
        C.tt("dve", t2[:], EI[:, :, 8], LI[:], ALU.mult, K, ["s_k"])
        C.tt("dve", fR[:], fR[:], t2[:], ALU.add, K, ["s_k"])
        C.tt("dve", fR[:], fR[:], t3[:], ALU.mult, K, ["s_k"])
        C.tt("dve", fI[:], EI[:, :, 8], LR[:], ALU.mult, K, ["s_k"])
        C.tt("dve", t2[:], t1[:], LI[:], ALU.mult, K, ["s_k"])
        C.tt("dve", fI[:], fI[:], t2[:], ALU.subtract, K, ["s_k"])
        C.tt("dve", fI[:], fI[:], t3[:], ALU.mult, K, ["s_k"])

        bR = C.sb("s_bR", [128, 4, 16], F32, st)
        bI = C.sb("s_bI", [128, 4, 16], F32, st)
        cR = C.sb("s_cR", [128, 4, 16], F32, st)
        cI = C.sb("s_cI", [128, 4, 16], F32, st)
        BbR = C.sb("s_BbR", [128, 4, 16], F32, st)
        BbI = C.sb("s_BbI", [128, 4, 16], F32, st)
        u1 = C.sb("s_u1", [128, 4, 16], F32, st)
        u2 = C.sb("s_u2", [128, 4, 16], F32, st)
        KWR = C.sb("s_KWR", [128, 4, 16, 16], F32, st)
        KWI = C.sb("s_KWI", [128, 4, 16, 16], F32, st)
        QR = C.sb("s_QR", [128, 4, 24, 16], F32, st)
        QI = C.sb("s_QI", [128, 4, 24, 16], F32, st)
        v1 = C.sb("s_v1", [128, 4, 24, 16], F32, st)
        v2 = C.sb("s_v2", [128, 4, 24, 16], F32, st)
        v3 = C.sb("s_v3", [128, 4, 24, 16], F32, st)
        v4 = C.sb("s_v4", [128, 4, 24, 16], F32, st)
        w3 = C.sb("s_w3", [128, 4, 16, 16], F32, st)
        w4 = C.sb("s_w4", [128, 4, 16, 16], F32, st)
        w1 = C.sb("s_w1", [128, 4, 16, 16], F32, st)
        w2 = C.sb("s_w2", [128, 4, 16, 16], F32, st)
        Tsb = C.sb("s_Tsb", [128, 8, 256], BF16, st)
        Wsb = C.sb("s_Wsb", [128, 8, 256], BF16, st)
        OQb = C.sb("s_OQb", [128, 4, 2, 256], BF16, st)
        tmpT = C.sb("s_tmpT", [128, 128], F32, st)
        for b in range(8):
            g2s = slice(4 * b, 4 * b + 4)
            for t, nm in ((bR, "s5_br"), (bI, "s5_bi"), (cR, "s5_cr"), (cI, "s5_ci")):
                C.dma("sp", t[:], cin[nm][:, g2s, :], [], ["s_bc"], "s_bc")
            KB = ["s_bc", "s_k", "s_b"]
            fRb = fR[:, g2s].unsqueeze(2).to_broadcast([128, 4, 16])
            fIb = fI[:, g2s].unsqueeze(2).to_broadcast([128, 4, 16])
            cmul(C, "dve", BbR[:], BbI[:], bR[:], bI[:], fRb, fIb, u1[:], u2[:], KB, "s_b")
            KBB = KB + ["s_b_R", "s_b_I"]
            eR = ER[:, g2s, :].unsqueeze(3).to_broadcast([128, 4, 24, 16])
            eI = EI[:, g2s, :].unsqueeze(3).to_broadcast([128, 4, 24, 16])
            ccR = cR[:].unsqueeze(2).to_broadcast([128, 4, 24, 16])
            ccI = cI[:].unsqueeze(2).to_broadcast([128, 4, 24, 16])
            KQ0 = ["s_bc", "s_k"]
            C.tt("pool", v3[:], eR, ccI, ALU.mult, KQ0, ["s_q_t3"])
            C.tt("pool", v4[:], eI, ccR, ALU.mult, KQ0, ["s_q_t4"])
            C.tt("dve", v1[:], eR, ccR, ALU.mult, KQ0, ["s_q_t1"])
            C.tt("dve", v2[:], eI, ccI, ALU.mult, KQ0, ["s_q_t2"])
            C.tt("dve", QR[:], v1[:], v2[:], ALU.subtract, ["s_q_t1", "s_q_t2"], ["s_q_R"])
            C.stt(QI[:], v3[:], -1.0, v4[:], ALU.mult, ALU.subtract, ["s_q_t3", "s_q_t4"], ["s_q_I"])
            eR = ER[:, g2s, 7:23].unsqueeze(3).to_broadcast([128, 4, 16, 16])
            eI = EI[:, g2s, 7:23].unsqueeze(3).to_broadcast([128, 4, 16, 16])
            bbR = BbR[:].unsqueeze(2).to_broadcast([128, 4, 16, 16])
            bbI = BbI[:].unsqueeze(2).to_broadcast([128, 4, 16, 16])
            C.tt("pool", w3[:], eR, bbI, ALU.mult, KBB, ["s_kw_t3"])
            C.tt("pool", w4[:], eI, bbR, ALU.mult, KBB, ["s_kw_t4"])
            C.tt("pool", KWI[:], w3[:], w4[:], ALU.add, ["s_kw_t3", "s_kw_t4"], ["s_kw_I"])
            C.tt("dve", w1[:], eR, bbR, ALU.mult, KBB, ["s_kw_t1"])
            C.tt("dve", w2[:], eI, bbI, ALU.mult, KBB, ["s_kw_t2"])
            C.tt("dve", KWR[:], w1[:], w2[:], ALU.subtract, ["s_kw_t1", "s_kw_t2"], ["s_kw_R"])
            KQ = ["s_kw_R", "s_kw_I", "s_q_R", "s_q_I", "const", "s_in"]
            C.copy("act", OQb[:, :, 0, :], QR[:, :, 8:24, :].rearrange("p g e o -> p g (e o)"), KQ, ["s_OQb"])
            C.copy("act", OQb[:, :, 1, :], QI[:, :, 8:24, :].rearrange("p g e o -> p g (e o)"), KQ, ["s_OQb"])
            C.dma("sp", scr["OQ"][4 * b:4 * b + 4].rearrange("g p r c -> p g r c"), OQb[:], ["s_OQb"], [], "s_oq_out")
            for g2l in range(4):
                for gp in range(2):
                    gl = 2 * g2l + gp
                    ps_ = slice(64 * gp, 64 * gp + 64)
                    pm, pk = C.psf[gl % 2], ("psf", gl % 2)
                    C.mm(pm[:, 0:256], KWR[ps_, g2l, 0:8, :].rearrange("p s i -> p (s i)"),
                         QR[ps_, g2l, 0:16, :].rearrange("p e o -> p (e o)"), True, False, KQ, pk)
                    C.mm(pm[:, 0:256], KWI[ps_, g2l, 0:8, :].rearrange("p s i -> p (s i)"),
                         QI[ps_, g2l, 0:16, :].rearrange("p e o -> p (e o)"), False, True, KQ, pk)
                    C.tt("dve", tmpT[:], pm[:, 0:128], mD[:], ALU.mult, [pk, "s_in"], ["s_tmpT"])
                    g = 8 * b + gl
                    C.stt(Tsb[:, gl, 0:128], P0[:], dcol[:, g:g + 1], tmpT[:], ALU.mult, ALU.add,
                          ["s_tmpT", "s_in"], ["s_Tsb"])
                    C.copy("act", Tsb[:, gl, 128:256], pm[:, 128:256], [pk], ["s_Tsb"])
                    pw, pkw = C.psf[2 + gl % 2], ("psf", 2 + gl % 2)
                    idn = C.identf[ps_, 64 * gp:64 * gp + 64]
                    for n, (src, half) in enumerate(((KWR, 0), (KWI, 0), (KWR, 1), (KWI, 1))):
                        C.tr(pw[:, 64 * n:64 * n + 64], src[ps_, g2l, 8 * half:8 * half + 8, :].rearrange("p s i -> p (s i)"),
                             idn, KQ, pkw)
                    C.copy("dve", Wsb[:, gl, :], pw[:, 0:256], [pkw], ["s_Wsb"])
            C.dma("sp", scr["T"][8 * b:8 * b + 8].rearrange("g p c -> p g c"), Tsb[:], ["s_Tsb"], [], "s_t_out")
            C.dma("sp", scr["W"][8 * b:8 * b + 8].rearrange("g p c -> p g c"), Wsb[:], ["s_Wsb"], [], "s_w_out")
        C.P.emit_phase()


def phase_s5_in(C, cin, scr, XR, XI, u_cm):
    st = ExitStack()
    with st:
        wu = C.sb("s_wu", [128, 8, 512], BF16, st)
        Wsb = C.sb("s1_Wsb", [128, 8, 256], BF16, st)
        Usb = [C.sb("s1_Usb%d" % i, [128, 256], BF16, st) for i in range(2)]
        n = 0
        for half in range(2):
            load_w(C, wu[:], cin["ev_w_in"], EV_U0 + 512 * half, 512, "s_wu")
            for s in range(16):
                pm, pk = C.psf[n % 2], ("psf", n % 2)
                n += 1
                hv = C.hnT[:].rearrange("p j (c s) -> p j c s", s=16)
                for j in range(8):
                    C.mm(pm[:], hv[:, j, :, s], wu[:, j, :], j == 0, j == 7, ["s_wu", "hnT"], pk)
                C.copy("act" if n % 2 else "dve", u_cm[:, 32 * half:32 * half + 32, 15 - s, :],
                       pm[:].rearrange("p (g i) -> p g i", i=16), [pk], ["s_ucm"])
        for b in range(8):
            C.dma("sp", Wsb[:], scr["W"][8 * b:8 * b + 8].rearrange("g p c -> p g c"), [], ["s1_Wsb"], "s1_Wsb")
            def gen_in(gl):
                g = 8 * b + gl
                gp, g2 = g % 2, g // 2
                U = Usb[g % 2]
                uk = ("s1_U", g % 2)
                pb, kb = C.psb[g % 2], ("psb", g % 2)
                for hf in range(2):
                    C.tr(pb[:, 128 * hf:128 * hf + 128], u_cm[:, g, 8 * hf:8 * hf + 8, :].rearrange("p s i -> p (s i)"),
                         C.ident[:], ["s_ucm", "const"], kb)
                yield
                C.copy("act" if g % 2 else "dve", U[:], pb[:, 0:256], [kb], [uk])
                yield
                C.dma("sp", scr["U"][g], U[:], [uk], [], ("s1_uo", g % 2))
                px, kx = C.psf[2 + g2 % 2], ("psf", 2 + g2 % 2)
                ps_ = slice(64 * gp, 64 * gp + 64)
                for ri in range(2):
                    C.mm(px[ps_, 128 * ri:128 * ri + 128], Wsb[:, gl, 64 * ri:64 * ri + 64], U[:, 0:128], True, False,
                         ["s1_Wsb", uk], kx)
                    C.mm(px[ps_, 128 * ri:128 * ri + 128], Wsb[:, gl, 128 + 64 * ri:128 + 64 * ri + 64], U[:, 128:256],
                         False, True, ["s1_Wsb", uk], kx)
                yield
                if gp == 1:
                    C.copy("dve", XR[:, g2, :], px[:, 0:128], [kx], ["s_XR"])
                    C.copy("act", XI[:, g2, :], px[:, 128:256], [kx], ["s_XI"])
                yield

            for gl in range(0, 8, 2):
                run_interleaved([gen_in(gl), gen_in(gl + 1)])
        C.P.emit_phase()


def phase_s5_scan(C, XR, XI, XRb, XIb):
    st = ExitStack()
    with st:
        t1 = C.sb("sc_t1", [128, 32], F32, st)
        t2 = C.sb("sc_t2", [128, 32], F32, st)
        t3 = C.sb("sc_t3", [128, 32], F32, st)
        t4 = C.sb("sc_t4", [128, 32], F32, st)
        AR, AI = C.ER[:, :, 23], C.EI[:, :, 23]
        for c in range(1, 128):
            C.tt("dve", t1[:], AR, XR[:, :, c - 1], ALU.mult, ["s_XR"], ["sc_t1"])
            C.tt("dve", t2[:], AI, XI[:, :, c - 1], ALU.mult, ["s_XI"], ["sc_t2"])
            C.tt("dve", t3[:], AR, XI[:, :, c - 1], ALU.mult, ["s_XI"], ["sc_t3"])
            C.tt("dve", t4[:], AI, XR[:, :, c - 1], ALU.mult, ["s_XR"], ["sc_t4"])
            C.tt("dve", t1[:], t1[:], t2[:], ALU.subtract, ["sc_t1", "sc_t2"], ["sc_t1"])
            C.tt("dve", t3[:], t3[:], t4[:], ALU.add, ["sc_t3", "sc_t4"], ["sc_t3"])
            C.tt("dve", XR[:, :, c], XR[:, :, c], t1[:], ALU.add, ["s_XR", "sc_t1"], ["s_XR"])
            C.tt("dve", XI[:, :, c], XI[:, :, c], t3[:], ALU.add, ["s_XI", "sc_t3"], ["s_XI"])
        C.memset("dve", XRb[:, :, 0:1], 0.0, ["s_XRb0"])
        C.memset("dve", XIb[:, :, 0:1], 0.0, ["s_XIb0"])
        C.copy("dve", XRb[:, :, 1:128], XR[:, :, 0:127], ["s_XR"], ["s_XRb"])
        C.copy("act", XIb[:, :, 1:128], XI[:, :, 0:127], ["s_XI"], ["s_XIb"])
        C.P.emit_phase()


GELU_C = 0.7978845608028654


def phase_s5_out(C, cin, scr, XRb, XIb, yg_cm):
    st = ExitStack()
    with st:
        Tsb = C.sb("s3_Tsb", [128, 8, 256], BF16, st)
        OQb = C.sb("s3_OQb", [128, 4, 2, 256], BF16, st)
        Ub = C.sb("s3_Ub", [128, 8, 256], BF16, st)
        Ysb = [C.sb("s3_Y%d" % i, [128, 256], F32, st) for i in range(2)]
        xs2 = [C.sb("s3_xs%d" % i, [128, 256], F32, st) for i in range(2)]
        x22 = [C.sb("s3_x2%d" % i, [128, 256], F32, st) for i in range(2)]
        sg2 = [C.sb("s3_sg%d" % i, [128, 256], F32, st) for i in range(2)]
        XK = ["s_XRb", "s_XIb", "s_XRb0", "s_XIb0"]
        for b in range(8):
            C.dma("sp", Tsb[:], scr["T"][8 * b:8 * b + 8].rearrange("g p c -> p g c"), [], ["s3_Tsb"], "s3_Tsb")
            C.dma("sp", OQb[:], scr["OQ"][4 * b:4 * b + 4].rearrange("g p r c -> p g r c"), [], ["s3_OQb"], "s3_OQb")
            C.dma("sp", Ub[:], scr["U"][8 * b:8 * b + 8].rearrange("g p c -> p g c"), [], ["s3_Ub"], "s3_Ub")
            def gen_out(gl):
                g = 8 * b + gl
                par = g % 2
                gp, g2, g2l = g % 2, g // 2, gl // 2
                ps_ = slice(64 * gp, 64 * gp + 64)
                W = ["s3_Tsb", "s3_OQb", "s3_Ub"] + XK
                pa, ka = C.psf[3 * par], ("psf", 3 * par)
                pbk, kbk = C.psf[3 * par + 1], ("psf", 3 * par + 1)
                pt, kt = C.psf[3 * par + 2], ("psf", 3 * par + 2)
                xs, x2, sg = xs2[par], x22[par], sg2[par]
                kxs, kx2, ksg = ("s3_xs", par), ("s3_x2", par), ("s3_sg", par)
                UA, UB = Ub[:, gl, 0:128], Ub[:, gl, 128:256]
                TD, TO = Tsb[:, gl, 0:128], Tsb[:, gl, 128:256]
                C.mm(pa[:, 0:128], TD, UB, True, False, W, ka)
                C.mm(pa[:, 0:128], OQb[ps_, g2l, 0, 0:128], XRb[ps_, g2, :], False, False, W, ka)
                C.mm(pa[:, 0:128], OQb[ps_, g2l, 1, 0:128], XIb[ps_, g2, :], False, True, W, ka)
                C.mm(pbk[:, 0:128], TD, UA, True, False, W, kbk)
                C.mm(pbk[:, 0:128], TO, UB, False, False, W, kbk)
                C.mm(pbk[:, 0:128], OQb[ps_, g2l, 0, 128:256], XRb[ps_, g2, :], False, False, W, kbk)
                C.mm(pbk[:, 0:128], OQb[ps_, g2l, 1, 128:256], XIb[ps_, g2, :], False, True, W, kbk)
                yield
                Y = Ysb[par]
                yk = ("s3_Y", par)
                C.copy("act", Y[:, 0:128], pa[:, 0:128], [ka], [yk])
                C.copy("dve", Y[:, 128:256], pbk[:, 0:128], [kbk], [yk])
                yield
                for hf in range(2):
                    C.tr(pt[:, 128 * hf:128 * hf + 128], Y[:, 128 * hf:128 * hf + 128], C.identf[:], [yk, "const"], kt)
                yield
                C.copy("act", xs[:], pt[:, 0:256], [kt], [kxs])
                yield
                C.tt("pool", x2[:], xs[:], xs[:], ALU.mult, [kxs], [kx2])
                yield
                C.ts("dve", x2[:], x2[:], 0.044715, 1.0, ALU.mult, ALU.add, [kx2], [kx2])
                yield
                C.tt("pool", x2[:], x2[:], xs[:], ALU.mult, [kx2, kxs], [kx2])
                yield
                C.act(sg[:], x2[:], AF.Sigmoid, [kx2], [ksg], scale=2.0 * GELU_C)
                yield
                C.tt("dve", yg_cm[:, :, 16 * g:16 * g + 16], xs[:].rearrange("p (t o) -> p t o", o=16),
                     sg[:].rearrange("p (t o) -> p t o", o=16), ALU.mult, [kxs, ksg], ["s_ygcm"])
                yield

            for gl in range(0, 8, 2):
                run_interleaved([gen_out(gl), gen_out(gl + 1)])
        C.P.emit_phase()


def phase_s5_glu(C, cin, yg_cm, ygT, yT):
    for s in range(16):
        pb, kb = C.psb[s % 2], ("psb", s % 2)
        for j in range(8):
            C.tr(pb[:, 128 * j:128 * j + 128], yg_cm[:, s, 128 * j:128 * j + 128], C.ident[:], ["s_ygcm", "const"], kb)
        dst = ygT.rearrange("p j (c s) -> p j c s", s=16)[:, :, :, s]
        C.copy("act" if s % 2 else "dve", dst, pb[:].rearrange("p (j c) -> p j c", j=8), [kb], ["s4_ygT"])
    C.P.emit_phase()
    sC = ExitStack()
    with sC:
        gw = C.sb("s4_gw", [128, 8, 1024], BF16, sC)
        wz = C.sb("s4_wz", [128, 8, 128], BF16, sC)
        gb = C.sb("s4_gb", [128, 8], F32, sC)
        sgt = [C.sb("s4_sg%d" % i, [128, 512], BF16, sC) for i in range(2)]
        zs = [C.sb("s4_zs%d" % i, [128, 512], BF16, sC) for i in range(2)]
        load_w(C, gw[:, :, 0:512], cin["s5_glu_w"], 0, 512, "s4_gw")
        load_w(C, gw[:, :, 512:1024], cin["s5_glu_w"], 512, 512, "s4_gw")
        C.dma("sp", gb[:], cin["s5_glu_bc"], [], ["s4_gb"], "s4_gb")
        n = 0
        for fo in range(8):
            load_w(C, wz[:], cin["ev_w_in"], EV_Z0 + 1024 + 128 * fo, 128, "s4_wz")
            for tg in range(4):
                tsl = slice(512 * tg, 512 * tg + 512)
                pm, pk = C.psf[n % 2], ("psf", n % 2)
                pz, kz = C.psf[2 + n % 2], ("psf", 2 + n % 2)
                sgk, zsk = ("s4_sg", n % 2), ("s4_zs", n % 2)
                for j in range(8):
                    C.mm(pm[:], gw[:, j, 128 * fo:128 * fo + 128], ygT[:, j, tsl], j == 0, j == 7,
                         ["s4_gw", "s4_ygT"], pk)
                for j in range(8):
                    C.mm(pz[:], wz[:, j, :], C.hnT[:, j, tsl], j == 0, j == 7, ["s4_wz", "hnT"], kz)
                C.act(sgt[n % 2][:], pm[:], AF.Sigmoid, [pk, "s4_gb"], [sgk], bias=gb[:, fo:fo + 1])
                C.act(zs[n % 2][:], pz[:], AF.Silu, [kz], [zsk])
                C.tt("dve", sgt[n % 2][:], sgt[n % 2][:], ygT[:, fo, tsl], ALU.mult, [sgk, "s4_ygT"], [sgk])
                C.tt("pool", yT[:, fo, tsl], sgt[n % 2][:], zs[n % 2][:], ALU.mult, [sgk, zsk], ["s4_yT"])
                n += 1
        C.P.emit_phase()
    sD = ExitStack()
    with sD:
        wo = C.sb("s4_wo", [128, 8, D], BF16, sD)
        load_w(C, wo[:], cin["ev_w_out"], 0, D, "s4_wo", rows0=1024, nj=8)
        out_proj(C, yT, 8, wo, "s4_yT", "s4_wo")
        C.P.emit_phase()


def layer0_s5(C, cin):
    scr = {
        "T": C.dram_scr("scr_T", [64, 128, 256], BF16),
        "W": C.dram_scr("scr_W", [64, 128, 256], BF16),
        "OQ": C.dram_scr("scr_OQ", [32, 128, 2, 256], BF16),
        "U": C.dram_scr("scr_U", [64, 128, 256], BF16),
    }
    sE = ExitStack()
    with sE:
        C.ER = C.sb("s_ER", [128, 32, 24], F32, sE)
        C.EI = C.sb("s_EI", [128, 32, 24], F32, sE)
        phase_s5_setup(C, cin, scr)
        bufA = C.sb("s_bufA", [128, 16384], BF16, sE)
        bufB = C.sb("s_bufB", [128, 16384], BF16, sE)
        u_cm = bufA[:].rearrange("p (g s i) -> p g s i", s=16, i=16)
        yg_cm = bufA[:].rearrange("p (s c) -> p s c", c=1024)
        yT = bufA[:].rearrange("p (j t) -> p j t", t=S)
        XR = bufB[:, 0:8192].bitcast(F32).rearrange("p (g c) -> p g c", c=128)
        XI = bufB[:, 8192:16384].bitcast(F32).rearrange("p (g c) -> p g c", c=128)
        ygT = bufB[:].rearrange("p (j t) -> p j t", t=S)
        sXb = ExitStack()
        with sXb:
            XRb = C.sb("s_XRb", [128, 32, 128], BF16, sXb)
            XIb = C.sb("s_XIb", [128, 32, 128], BF16, sXb)
            phase_s5_in(C, cin, scr, XR, XI, u_cm)
            phase_s5_scan(C, XR, XI, XRb, XIb)
            phase_s5_out(C, cin, scr, XRb, XIb, yg_cm)
        phase_s5_glu(C, cin, yg_cm, ygT, yT)


def layer0(C, cin):
    phase_norm(C, cin["norm_g0"], "l0")
    if BISECT == "l0norm":
        return
    if "ml" not in SKIP:
        phase_ml_prep(C, cin["ev_w_in"], cin["ev_if_bias"])
        if BISECT == "ml_prep":
            return
        for h in range(1 if BISECT else 4):
            phase_ml_head(C, h, cin["ev_w_in"], cin["ev_w_out"], cin["ev_conv_wT"], cin["ev_conv_bc"], cin["ev_head_gc"])
    if "s5" not in SKIP:
        layer0_s5(C, cin)


SKIP = set()


CONST_SPECS = {
    "ident_bf": ([128, 128], BF16), "ident_f": ([128, 128], F32), "tri_f": ([128, 128], F32),
    "trin_f": ([128, 128], F32), "trin16_f": ([128, 128], F32), "ones_f": ([128, 128], F32),
}

IN_SPECS = {
    "x": ([S, D], F32),
    "norm_g0": ([128, D], F32), "norm_g1": ([128, D], F32), "norm_gf": ([128, D], F32),
    "od_w_in": ([D, 6160], F32), "gla_wab": ([17, 1024], F32), "gla_head_g": ([128, 16], F32),
    "od_w_out": ([2048, D], F32),
    "ev_w_in": ([D, 6152], F32), "ev_w_out": ([2048, D], F32), "ev_if_bias": ([128, 128], F32),
    "ev_conv_wT": ([1024, 4], F32), "ev_conv_bc": ([128, 8], F32), "ev_head_gc": ([128, 8], F32),
    "s5_lr": ([128, 32], F32), "s5_li": ([128, 32], F32), "s5_ldt": ([128, 32], F32),
    "s5_br": ([128, 32, 16], F32), "s5_bi": ([128, 32, 16], F32), "s5_cr": ([128, 32, 16], F32),
    "s5_ci": ([128, 32, 16], F32), "s5_maskD": ([128, 128], F32), "s5_P0": ([128, 128], F32),
    "s5_dcol": ([128, 64], F32), "s5_glu_w": ([1024, 1024], F32), "s5_glu_bc": ([128, 8], F32),
}


def host_consts():
    tri = np.triu(np.ones((128, 128), np.float32))
    return {
        "ident_bf": np.eye(128, dtype=ml_dtypes.bfloat16), "ident_f": np.eye(128, dtype=np.float32),
        "tri_f": tri, "trin_f": -tri, "trin16_f": -tri / 16.0, "ones_f": np.ones((128, 128), np.float32),
    }


def build_program(layers=(0, 1), final=True):
    nc = bass.Bass("TRN2", target_bir_lowering=False)
    st = ExitStack()
    with st:
        C = Ctx(nc, st)
        cin = {}
        for nm, (shp, dt) in list(CONST_SPECS.items()) + list(IN_SPECS.items()):
            cin[nm] = C.dram_in(nm, shp, dt)
        out_d = C.dram_out("out", [S, D], F32)
        setup_globals(C, cin)
        C.lfn = C.sb("lfn", [128, 64], F32)
        C.cbias = C.sb("cbias", [128, 64], F32)
        phase_load_x(C, cin["x"])
        if 0 in layers:
            layer0(C, cin)
        if 1 in layers:
            layer1(C, cin)
        if final:
            phase_final(C, cin["norm_gf"], out_d)
        else:
            ov = out_d.rearrange("(c s) d -> c s d", s=16)
            for q in range(4):
                C.dma("sp", ov[:, 4 * q:4 * q + 4, :], C.x_sb[:, 4 * q:4 * q + 4, :],
                      [("x", s) for s in range(4 * q, 4 * q + 4)], [], ("out", q % 2))
            C.P.emit_phase()
    return nc


def host_inputs(inp, b):
    f32 = np.float32
    d = dict(host_consts())
    d["x"] = np.ascontiguousarray(inp["x"][b], dtype=f32)
    d["norm_g0"] = np.ascontiguousarray(np.broadcast_to(inp["norm_g"][0], (128, D)), dtype=f32)
    d["norm_g1"] = np.ascontiguousarray(np.broadcast_to(inp["norm_g"][1], (128, D)), dtype=f32)
    d["norm_gf"] = np.ascontiguousarray(np.broadcast_to(inp["final_norm_g"], (128, D)), dtype=f32)
    d["od_w_in"] = np.ascontiguousarray(inp["od_w_in"][0], dtype=f32)
    d["gla_wab"] = np.ascontiguousarray(np.concatenate([inp["gla_w_alpha"][0], inp["gla_b_alpha"][0][None, :]], 0), dtype=f32)
    d["gla_head_g"] = np.ascontiguousarray(inp["gla_head_g"][0].reshape(16, 128).T, dtype=f32)
    d["od_w_out"] = np.ascontiguousarray(inp["od_w_out"][0], dtype=f32)
    d["ev_w_in"] = np.ascontiguousarray(inp["ev_w_in"][0], dtype=f32)
    d["ev_w_out"] = np.ascontiguousarray(inp["ev_w_out"][0], dtype=f32)
    ifb = np.concatenate([inp["ev_i_bias"][0], inp["ev_f_bias"][0]])
    d["ev_if_bias"] = np.ascontiguousarray(np.broadcast_to(np.tile(ifb, 16), (128, 128)), dtype=f32)
    d["ev_conv_wT"] = np.ascontiguousarray(inp["ev_conv_w"][0].T, dtype=f32)
    d["ev_conv_bc"] = np.ascontiguousarray(inp["ev_conv_b"][0].reshape(8, 128).T, dtype=f32)
    d["ev_head_gc"] = np.ascontiguousarray(inp["ev_head_g"][0].reshape(8, 128).T, dtype=f32)

    def pair(a):
        a = a.reshape((32, 2, 64) + a.shape[2:])
        return np.ascontiguousarray(np.moveaxis(a, 0, 2).reshape((128, 32) + a.shape[3:]), dtype=f32)

    d["s5_lr"] = pair(inp["s5_lam_re"][0])
    d["s5_li"] = pair(inp["s5_lam_im"][0])
    d["s5_ldt"] = pair(np.broadcast_to(inp["s5_log_dt"][0][:, None], (64, 64)))
    d["s5_br"] = pair(inp["s5_b_re"][0])
    d["s5_bi"] = pair(inp["s5_b_im"][0])
    d["s5_cr"] = pair(np.swapaxes(inp["s5_c_re"][0], 1, 2))
    d["s5_ci"] = pair(np.swapaxes(inp["s5_c_im"][0], 1, 2))
    sig = np.arange(128) // 16
    ii = np.arange(128) % 16
    d["s5_maskD"] = (sig[:, None] + sig[None, :] >= 7).astype(f32)
    d["s5_P0"] = ((sig[:, None] + sig[None, :] == 7) & (ii[:, None] == ii[None, :])).astype(f32)
    d["s5_dcol"] = np.ascontiguousarray(np.tile(inp["s5_d"][0].reshape(64, 16).T, (8, 1)), dtype=f32)
    d["s5_glu_w"] = np.ascontiguousarray(inp["s5_glu_w"][0], dtype=f32)
    d["s5_glu_bc"] = np.ascontiguousarray(inp["s5_glu_b"][0].reshape(8, 128).T, dtype=f32)
    return d


def kernel(**inputs):
    inp = {k: np.asarray(v) for k, v in inputs.items()}
    nc = build_program()
    in_maps = [host_inputs(inp, b) for b in range(8)]
    res = run_bass_kernel_spmd(nc, in_maps, core_ids=list(range(8)))
    return np.stack([np.asarray(r["out"], dtype=np.float32) for r in res.results], 0)
```

```python
import math
from contextlib import ExitStack

import numpy as np
import ml_dtypes

import concourse.bass as bass
import concourse.mybir as mybir
from concourse.bass_utils import run_bass_kernel_spmd

F32 = mybir.dt.float32
BF16 = mybir.dt.bfloat16
ALU = mybir.AluOpType
AF = mybir.ActivationFunctionType
AX = mybir.AxisListType

D = 1024
S = 2048
EPS = 1e-6

STRICT = False
COMPUTE = ("pe", "act", "dve", "pool")
ENGS = ("pe", "act", "dve", "pool", "sp")


class Op:
    __slots__ = ("eng", "fn", "waits", "signal", "idx", "dsem", "dval", "cnt", "phase")

    def __init__(self, eng, fn):
        self.eng = eng
        self.fn = fn
        self.waits = []
        self.signal = False
        self.idx = -1
        self.dsem = None
        self.dval = 0
        self.cnt = 0
        self.phase = 0


class Prog:
    def __init__(self, nc, stack):
        self.nc = nc
        self.stack = stack
        self.streams = {e: [] for e in ENGS}
        self.esem = {e: stack.enter_context(nc.semaphore("sem_" + e)) for e in COMPUTE}
        self.base = {e: 0 for e in COMPUTE}
        self.nidx = {e: 0 for e in ENGS}
        self.waited = {e: {} for e in ENGS}
        self.state = {}
        self.dsems = {}
        self.phase = 0
        self.n_ops = 0

    def _add_wait(self, o, d, raw):
        if d is o or d.phase < self.phase:
            return
        if d.dsem is None and d.eng == o.eng and not raw and (not STRICT or o.eng == "pe"):
            return
        if d.dsem is not None:
            key, val = ("d", d.dsem), d.dval
        else:
            key, val = d.eng, d.idx
        w = self.waited[o.eng]
        if w.get(key, -1) >= val:
            return
        w[key] = val
        d.signal = True
        o.waits.append(d)

    def op(self, eng, fn, reads=(), writes=(), dma=None):
        o = Op(eng, fn)
        o.phase = self.phase
        o.idx = self.nidx[eng]
        self.nidx[eng] += 1
        self.streams[eng].append(o)
        self.n_ops += 1
        for k in reads:
            st = self.state.get(k)
            if st is not None:
                if st[0] is not None:
                    self._add_wait(o, st[0], True)
                if isinstance(k, tuple) and k[0] in ("psf", "psb"):
                    for r in st[1].values():
                        self._add_wait(o, r, False)
        for k in writes:
            st = self.state.get(k)
            if st is not None:
                if st[0] is not None:
                    self._add_wait(o, st[0], False)
                for r in st[1].values():
                    self._add_wait(o, r, False)
                for r in st[2]:
                    self._add_wait(o, r, False)
        if dma is not None:
            ds = self.dsems.get(dma)
            if ds is None:
                ds = [self.stack.enter_context(self.nc.semaphore("dsem_%d" % len(self.dsems))), 0]
                self.dsems[dma] = ds
            ds[1] += 16
            o.dsem = dma
            o.dval = ds[1]
            o.signal = True
        for k in reads:
            st = self.state.get(k)
            if st is None:
                st = [None, {}, []]
                self.state[k] = st
            if o.dsem is not None:
                st[2].append(o)
            else:
                st[1][eng] = o
        for k in writes:
            self.state[k] = [o, {}, []]
        return o

    def _emit_stream(self, name, eng):
        for o in self.streams[name]:
            for d in o.waits:
                if d.dsem is not None:
                    eng.wait_ge(self.dsems[d.dsem][0], d.dval)
                else:
                    eng.wait_ge(self.esem[d.eng], d.cnt)
            if o.fn is None:
                continue
            bi = o.fn(eng)
            if o.dsem is not None:
                bi.then_inc(self.dsems[o.dsem][0], 16)
            elif o.signal:
                bi.then_inc(self.esem[o.eng], 1)
        if name == "sp":
            for sem, tot in self.dsems.values():
                if tot > 0:
                    eng.wait_ge(sem, tot)

    def emit_phase(self):
        for e in COMPUTE:
            c = self.base[e]
            for o in self.streams[e]:
                if o.signal and o.dsem is None:
                    assert o.fn is not None
                    c += 1
                o.cnt = c
            self.base[e] = c
        with self.nc.Block() as block:
            @block.tensor
            def _(eng):
                self._emit_stream("pe", eng)

            @block.scalar
            def _(eng):
                self._emit_stream("act", eng)

            @block.vector
            def _(eng):
                self._emit_stream("dve", eng)

            @block.gpsimd
            def _(eng):
                self._emit_stream("pool", eng)

            @block.sync
            def _(eng):
                self._emit_stream("sp", eng)
        self.streams = {e: [] for e in ENGS}
        self.phase += 1


class Ctx:
    def __init__(self, nc, stack):
        self.nc = nc
        self.stack = stack
        self.P = Prog(nc, stack)

    def sb(self, name, shape, dt, st=None):
        self.nuniq = getattr(self, "nuniq", 0) + 1
        return (st or self.stack).enter_context(self.nc.sbuf_tensor("sb%d_%s" % (self.nuniq, name), list(shape), dt))

    def ps(self, name, shape, dt):
        return self.stack.enter_context(self.nc.psum_tensor(name, list(shape), dt))

    def dram_in(self, name, shape, dt):
        return self.nc.dram_tensor(name, list(shape), dt, kind="ExternalInput").ap()

    def dram_out(self, name, shape, dt):
        return self.nc.dram_tensor(name, list(shape), dt, kind="ExternalOutput").ap()

    def dram_scr(self, name, shape, dt):
        return self.nc.dram_tensor(name, list(shape), dt).ap()

    def mm(self, out, lhsT, rhs, start, stop, reads, pkey):
        self.P.op("pe", lambda e: e.matmul(out=out, lhsT=lhsT, rhs=rhs, start=start, stop=stop),
                  reads=reads, writes=[pkey])

    def tr(self, out, in_, ident, reads, pkey):
        self.P.op("pe", lambda e: e.transpose(out=out, in_=in_, identity=ident), reads=reads, writes=[pkey])

    def act(self, out, in_, func, reads, writes, bias=None, scale=None, accum_out=None, eng="act"):
        kw = {}
        if bias is not None:
            kw["bias"] = bias
        if scale is not None:
            kw["scale"] = scale
        if accum_out is not None:
            kw["accum_out"] = accum_out
        self.P.op(eng, lambda e: e.activation(out=out, in_=in_, func=func, **kw), reads=reads, writes=writes)

    def tt(self, eng, out, in0, in1, op, reads, writes):
        self.P.op(eng, lambda e: e.tensor_tensor(out=out, in0=in0, in1=in1, op=op), reads=reads, writes=writes)

    def ts(self, eng, out, in0, s1, s2, op0, op1, reads, writes):
        if s2 is None:
            self.P.op(eng, lambda e: e.tensor_scalar(out=out, in0=in0, scalar1=s1, scalar2=None, op0=op0),
                      reads=reads, writes=writes)
        else:
            self.P.op(eng, lambda e: e.tensor_scalar(out=out, in0=in0, scalar1=s1, scalar2=s2, op0=op0, op1=op1),
                      reads=reads, writes=writes)

    def stt(self, out, in0, scalar, in1, op0, op1, reads, writes):
        self.P.op("dve", lambda e: e.scalar_tensor_tensor(out=out, in0=in0, scalar=scalar, in1=in1,
                                                           op0=op0, op1=op1), reads=reads, writes=writes)

    def copy(self, eng, out, in_, reads, writes):
        if eng == "act":
            self.P.op("act", lambda e: e.activation(out=out, in_=in_, func=AF.Copy), reads=reads, writes=writes)
        else:
            self.P.op(eng, lambda e: e.tensor_copy(out=out, in_=in_), reads=reads, writes=writes)

    def memset(self, eng, ap, val, writes):
        self.P.op(eng, lambda e: e.memset(ap, val), writes=writes)

    def dma(self, q, out, in_, reads, writes, key):
        self.P.op(q, lambda e: e.dma_start(out=out, in_=in_), reads=reads, writes=writes, dma=key)


def setup_globals(C, cin):
    P = C.P
    C.x_sb = C.sb("x_sb", [128, 16, D], F32)
    C.hnT = C.sb("hnT", [128, 8, S], BF16)
    C.ident = C.sb("ident_bf", [128, 128], BF16)
    C.identf = C.sb("ident_f", [128, 128], F32)
    C.tri = C.sb("tri_f", [128, 128], F32)
    C.trin = C.sb("trin_f", [128, 128], F32)
    C.trin16 = C.sb("trin16_f", [128, 128], F32)
    C.ones = C.sb("ones_f", [128, 128], F32)
    C.cb = C.sb("cbias", [128, 4], F32)
    C.memset("dve", C.cb[:, 0:1], math.log(1.0 / 16.0), ["const"])
    C.memset("dve", C.cb[:, 1:2], 1.0, ["const"])
    C.psf = [C.ps("psf%d" % i, [128, 512], F32) for i in range(6)]
    C.psb = [C.ps("psb%d" % i, [128, 1024], BF16) for i in range(2)]
    for t, nm in ((C.ident, "ident_bf"), (C.identf, "ident_f"), (C.tri, "tri_f"), (C.trin, "trin_f"),
                  (C.trin16, "trin16_f"), (C.ones, "ones_f")):
        C.dma("sp", t[:], cin[nm], [], ["const"], "const")


def phase_load_x(C, x_d):
    xv = x_d.rearrange("(c s) d -> c s d", s=16)
    for q in range(4):
        C.dma("sp", C.x_sb[:, 4 * q:4 * q + 4, :], xv[:, 4 * q:4 * q + 4, :], [], [("x", s) for s in range(4 * q, 4 * q + 4)],
              ("x", q))


def rms_stats(C, st, tag):
    junk = C.sb("junk_" + tag, [128, D], BF16, st)
    ssq = C.sb("ssq_" + tag, [128, 16], F32, st)
    rstd = C.sb("rstd_" + tag, [128, 16], F32, st)
    for s in range(16):
        C.act(junk[:], C.x_sb[:, s, :], AF.Square, [("x", s)], ["junk", ("ssq", s)], accum_out=ssq[:, s:s + 1])
    C.ts("dve", rstd[:], ssq[:], 1.0 / D, EPS, ALU.mult, ALU.add, [("ssq", s) for s in range(16)], ["rstd"])
    C.act(rstd[:], rstd[:], AF.Sqrt, ["rstd"], ["rstd"])
    C.P.op("dve", lambda e: e.reciprocal(out=rstd[:], in_=rstd[:]), reads=["rstd"], writes=["rstd"])
    return rstd


def phase_norm(C, g_d, tag):
    st = ExitStack()
    with st:
        g_sb = C.sb("g_" + tag, [128, D], F32, st)
        hn = [C.sb("hn%d_%s" % (i, tag), [128, D], BF16, st) for i in range(3)]
        C.dma("sp", g_sb[:], g_d, [], ["g"], "g")
        rstd = rms_stats(C, st, tag)
        def mk_hn(s):
            C.stt(hn[s % 3][:], C.x_sb[:, s, :], rstd[:, s:s + 1], g_sb[:], ALU.mult, ALU.mult,
                  [("x", s), "rstd", "g"], [("hn", s % 3)])

        mk_hn(0)
        mk_hn(1)
        for s in range(16):
            h = hn[s % 3]
            hk = ("hn", s % 3)
            pt = C.psb[s % 2]
            pk = ("psb", s % 2)
            for j in range(8):
                C.tr(pt[:, 128 * j:128 * j + 128], h[:, 128 * j:128 * j + 128], C.ident[:], [hk, "const"], pk)
            if s + 2 < 16:
                mk_hn(s + 2)
            dst = C.hnT[:].rearrange("p j (c s) -> p j c s", s=16)[:, :, :, s]
            src = pt[:].rearrange("p (j c) -> p j c", j=8)
            C.copy("act" if s % 2 else "dve", dst, src, [pk], [("hnT", s)])
        C.P.emit_phase()


def phase_final(C, g_d, out_d):
    st = ExitStack()
    with st:
        g_sb = C.sb("g_fin", [128, D], F32, st)
        stage = [C.sb("ostage%d" % i, [128, 4, D], F32, st) for i in range(2)]
        C.dma("sp", g_sb[:], g_d, [], ["g"], "g")
        rstd = rms_stats(C, st, "fin")
        ov = out_d.rearrange("(c s) d -> c s d", s=16)
        for q in range(4):
            sg = stage[q % 2]
            for s4 in range(4):
                s = 4 * q + s4
                C.stt(sg[:, s4, :], C.x_sb[:, s, :], rstd[:, s:s + 1], g_sb[:], ALU.mult, ALU.mult,
                      [("x", s), "rstd", "g"], [("ostage", q % 2)])
            C.dma("sp", ov[:, 4 * q:4 * q + 4, :], sg[:], [("ostage", q % 2)], [], ("out", q % 2))
        C.P.emit_phase()


def run_interleaved(gens):
    gens = list(gens)
    while gens:
        for g in list(gens):
            try:
                next(g)
            except StopIteration:
                gens.remove(g)


def load_w(C, dst, w2d, f0, nf, key, rows0=0, nj=8):
    src = w2d[rows0:rows0 + 128 * nj, f0:f0 + nf].rearrange("(j p) f -> p j f", p=128)
    C.dma("pool", dst, src, [], [key], key)


def out_proj(C, yT, nfc, wo, ykey, wkey):
    n = 0
    for s in range(16):
        for dh in range(2):
            pm = C.psf[n % 2]
            pk = ("psf", n % 2)
            n += 1
            yv = yT[:, :, :].rearrange("p f (c s) -> p f c s", s=16)
            for fc in range(nfc):
                C.mm(pm[:], yv[:, fc, :, s], wo[:, fc, 512 * dh:512 * dh + 512], fc == 0, fc == nfc - 1,
                     [ykey, wkey], pk)
            xs = C.x_sb[:, s, 512 * dh:512 * dh + 512]
            C.tt("dve", xs, pm[:], xs, ALU.add, [pk, ("x", s)], [("x", s)])


GLA_Q0, GLA_K0, GLA_V0, GLA_Z0, GLA_R0 = 0, 1024, 2048, 4096, 6144


def phase_gla_prep(C, w_in, wab_d):
    st = ExitStack()
    with st:
        wr = C.sb("wr", [128, 8, 16], BF16, st)
        load_w(C, wr[:], w_in, GLA_R0, 16, "wr")
        C.memset("dve", C.rT1[:], 1.0, ["rT1"])
        C.dma("sp", C.wab[0:17, :], wab_d, [], ["wab"], "wab")
        for tg in range(4):
            pm = C.psf[tg % 2]
            pk = ("psf", tg % 2)
            for j in range(8):
                C.mm(pm[0:16, :], wr[:, j, :], C.hnT[:, j, 512 * tg:512 * tg + 512], j == 0, j == 7, ["wr", "hnT"], pk)
            C.copy("dve", C.rT1[0:16, 512 * tg:512 * tg + 512], pm[0:16, :], [pk, "rT1"], ["rT1"])
        C.P.emit_phase()


def phase_gla_head(C, h, w_in, hg_d, w_out):
    st = ExitStack()
    with st:
        try:
            _gla_head(C, st, h, w_in, hg_d, w_out)
        except StopPhase:
            pass
        C.P.emit_phase()


def _gla_head(C, st, h, w_in, hg_d, w_out):
    if True:
        wbuf = C.sb("g_wbuf", [128, 8, 512], BF16, st)
        wo = wbuf[:].rearrange("p (fc a) f -> p fc (a f)", a=2)
        wqk = C.wqk
        qT = C.sb("g_qT", [128, 2, S], BF16, st)
        kT = C.sb("g_kT", [128, 2, S], BF16, st)
        v_sb = C.sb("g_v", [128, 16, 512], BF16, st)
        zT = C.sb("g_zT", [128, 4, S], BF16, st)
        yT = C.sb("g_yT", [128, 4, S], BF16, st)
        hgc = C.sb("g_hgc", [128, 4], F32, st)
        Sf = C.sb("g_Sf", [128, 2, 512], F32, st)
        Sb = C.sb("g_Sb", [128, 2, 512], BF16, st)
        la = C.sb("g_la", [128, 256], F32, st)
        ep = C.sb("g_ep", [128, 128], F32, st)
        em = C.sb("g_em", [128, 128], F32, st)
        ee = C.sb("g_ee", [128, 128], F32, st)
        gcol = C.sb("g_gcol", [128, 2], F32, st)
        egc = C.sb("g_egc", [128, 2], F32, st)
        qs = C.sb("g_qs", [128, 2, 128], BF16, st)
        ks = C.sb("g_ks", [128, 2, 128], BF16, st)
        keT = C.sb("g_keT", [128, 2, 128], BF16, st)
        ke = C.sb("g_ke", [128, 256], BF16, st)
        attn = C.sb("g_attn", [128, 128], BF16, st)
        ssq = C.sb("g_ssq", [128, 2], F32, st)
        rstd = C.sb("g_rstd", [128, 2], F32, st)
        C.memset("dve", ssq[:], 1.0, ["g_ssq"])
        on = C.sb("g_on", [128, 512], BF16, st)
        junk = on

        C.dma("sp", hgc[:], hg_d[:, 4 * h:4 * h + 4], [], ["hgc"], "hgc")
        load_w(C, wbuf[:], w_in, GLA_V0 + 512 * h, 512, "g_wbuf")
        n = 0
        for which, dstT in ((0, qT), (1, kT)):
            for kt in range(2):
                for tg in range(4):
                    pm = C.psf[n % 4]
                    pk = ("psf", n % 4)
                    for j in range(8):
                        C.mm(pm[:], wqk[:, j, 256 * which + 128 * kt:256 * which + 128 * kt + 128],
                             C.hnT[:, j, 512 * tg:512 * tg + 512], j == 0, j == 7, ["g_wqk", "hnT"], pk)
                    C.copy("act" if n % 2 else "dve", dstT[:, kt, 512 * tg:512 * tg + 512], pm[:], [pk],
                           ["g_qT" if which == 0 else "g_kT"])
                    n += 1
        if h < 3:
            load_w(C, wqk[:, :, 0:256], w_in, GLA_Q0 + 256 * (h + 1), 256, "g_wqk")
            load_w(C, wqk[:, :, 256:512], w_in, GLA_K0 + 256 * (h + 1), 256, "g_wqk")
        for k in range(16):
            pm = C.psf[n % 4]
            pk = ("psf", n % 4)
            for j in range(8):
                C.mm(pm[:], C.hnT[:, j, 128 * k:128 * k + 128], wbuf[:, j, :], j == 0, j == 7, ["g_wbuf", "hnT"], pk)
            C.copy("act" if n % 2 else "dve", v_sb[:, k, :], pm[:], [pk], ["g_v"])
            n += 1
        load_w(C, wbuf[:], w_in, GLA_Z0 + 512 * h, 512, "g_wbuf")
        for fc in range(4):
            for tg in range(4):
                pm = C.psf[n % 4]
                pk = ("psf", n % 4)
                for j in range(8):
                    C.mm(pm[:], wbuf[:, j, 128 * fc:128 * fc + 128], C.hnT[:, j, 512 * tg:512 * tg + 512],
                         j == 0, j == 7, ["g_wbuf", "hnT"], pk)
                C.act(zT[:, fc, 512 * tg:512 * tg + 512], pm[:], AF.Silu, [pk], ["g_zT"])
                n += 1

        load_w(C, wo, w_out, 0, D, "g_wbuf", rows0=512 * h, nj=4)
        nchunk = {"proj": 0, "chunk1": 1, "chunk2": 2}.get(BISECT, 16)
        qs2 = [qs, C.sb("g_qs1", [128, 2, 128], BF16, st)]
        ke2 = [ke, C.sb("g_ke1", [128, 256], BF16, st)]
        attn2 = [attn, C.sb("g_attn1", [128, 128], BF16, st)]
        egc2 = [egc, C.sb("g_egc1", [128, 2], F32, st)]

        ep2 = [ep, ep]
        em2 = [em, C.sb("g_em1", [128, 128], F32, st)]
        ee2 = [ee, C.sb("g_ee1", [128, 128], F32, st)]

        def genA(k):
            b = k % 2
            tsl = slice(128 * k, 128 * k + 128)
            qsb, keb, attnb, egcb = qs2[b], ke2[b], attn2[b], egc2[b]
            p0, k0 = C.psf[0], ("psf", 0)
            C.mm(p0[:, 0:256], C.rT1[0:17, tsl], C.wab[0:17, 256 * h:256 * h + 256], True, True, ["rT1", "wab"], k0)
            yield
            C.act(la[:], p0[:, 0:256], AF.Exp, [k0], ["g_la"], scale=-1.0)
            yield
            C.act(la[:], la[:], AF.Ln, ["g_la", "const"], ["g_la"], bias=C.cb[:, 1:2])
            yield
            pk_ = [(C.psf[1], ("psf", 1)), (C.psf[2], ("psf", 2))]
            for kt in range(2):
                p1, k1 = pk_[kt]
                C.mm(p1[:, 0:128], la[:, 128 * kt:128 * kt + 128], C.trin16[:], True, True, ["g_la", "const"], k1)
            yield
            for kt in range(2):
                p1, k1 = pk_[kt]
                C.copy("dve", gcol[:, kt:kt + 1], p1[:, 127:128], [k1], [("g_gcol", kt)])
                C.act(em2[kt][:], p1[:, 0:128], AF.Exp, [k1], [("g_em", kt)], scale=-1.0)
            yield
            for kt in range(2):
                p1, k1 = pk_[kt]
                C.act(ep[:], p1[:, 0:128], AF.Exp, [k1, "const"], ["g_ep"], bias=C.cb[:, 0:1])
                C.tt("dve", qsb[:, kt, :], qT[:, kt, tsl], ep[:], ALU.mult, ["g_qT", "g_ep"], [("g_qs", kt, b)])
                yield
            for kt in range(2):
                p1, k1 = pk_[kt]
                C.act(ee2[kt][:], p1[:, 0:128], AF.Exp, [k1, ("g_gcol", kt)], [("g_ee", kt)], scale=-1.0,
                      bias=gcol[:, kt:kt + 1])
                C.tt("pool", ks[:, kt, :], kT[:, kt, tsl], em2[kt][:], ALU.mult, ["g_kT", ("g_em", kt)], [("g_ks", kt)])
            yield
            C.act(egcb[:, 0:2], gcol[:, 0:2], AF.Exp, [("g_gcol", 0), ("g_gcol", 1)], [("g_egc", 0, b), ("g_egc", 1, b)])
            for kt in range(2):
                C.tt("pool", keT[:, kt, :], kT[:, kt, tsl], ee2[kt][:], ALU.mult, ["g_kT", ("g_ee", kt)], [("g_keT", kt)])
            yield
            for kt in range(2):
                C.mm(p0[:, 0:128], ks[:, kt, :], qsb[:, kt, :], kt == 0, kt == 1, [("g_ks", kt), ("g_qs", kt, b)], k0)
            yield
            pb, kb = C.psb[0], ("psb", 0)
            for kt in range(2):
                C.tr(pb[:, 128 * kt:128 * kt + 128], keT[:, kt, :], C.ident[:], [("g_keT", kt), "const"], kb)
            C.tt("dve", attnb[:], p0[:, 0:128], C.tri[:], ALU.mult, [k0, "const"], [("g_attn", b)])
            yield
            C.copy("act", keb[:], pb[:, 0:256], [kb], [("g_ke", 0, b), ("g_ke", 1, b)])
            yield

        def genBs(k):
            b = k % 2
            keb, egcb = ke2[b], egc2[b]
            if k < 15:
                for kt in range(2):
                    p4, k4 = C.psf[4 + kt], ("psf", 4 + kt)
                    C.mm(p4[:], keb[:, 128 * kt:128 * kt + 128], v_sb[:, k, :], True, True, [("g_ke", kt, b), "g_v"], k4)
                yield
                for kt in range(2):
                    p4, k4 = C.psf[4 + kt], ("psf", 4 + kt)
                    if k == 0:
                        C.copy("dve", Sf[:, kt, :], p4[:], [k4], [("g_Sf", kt)])
                    else:
                        C.stt(Sf[:, kt, :], Sf[:, kt, :], egcb[:, kt:kt + 1], p4[:], ALU.mult, ALU.add,
                              [k4, ("g_egc", kt, b), ("g_Sf", kt)], [("g_Sf", kt)])
                    yield
                for kt in range(2):
                    C.copy("act", Sb2[(k + 1) % 2][:, kt, :], Sf[:, kt, :], [("g_Sf", kt)], [("g_Sb", kt, (k + 1) % 2)])
                    yield

        def genBo(k):
            b = k % 2
            tsl = slice(128 * k, 128 * k + 128)
            qsb, attnb = qs2[b], attn2[b]
            p3, k3 = C.psf[3], ("psf", 3)
            C.mm(p3[:], attnb[:], v_sb[:, k, :], True, k == 0, [("g_attn", b), "g_v"], k3)
            if k > 0:
                for kt in range(2):
                    C.mm(p3[:], qsb[:, kt, :], Sb2[b][:, kt, :], False, kt == 1, [("g_qs", kt, b), ("g_Sb", kt, b)], k3)
            yield
            C.act(junk[:], p3[:], AF.Square, [k3], ["g_on", "g_ssq"], accum_out=ssq[:, 0:1])
            yield
            C.ts("dve", rstd[:], ssq[:], 1.0 / 512, EPS, ALU.mult, ALU.add, ["g_ssq"], ["g_rstd"])
            yield
            C.act(rstd[:], rstd[:], AF.Sqrt, ["g_rstd"], ["g_rstd"])
            yield
            C.P.op("dve", lambda e: e.reciprocal(out=rstd[:], in_=rstd[:]), reads=["g_rstd"], writes=["g_rstd"])
            yield
            C.act(on[:], p3[:], AF.Copy, [k3, "g_rstd"], ["g_on"], scale=rstd[:, 0:1])
            yield
            pb, kb = C.psb[1], ("psb", 1)
            for fc in range(4):
                C.tr(pb[:, 128 * fc:128 * fc + 128], on[:, 128 * fc:128 * fc + 128], C.ident[:], ["g_on", "const"], kb)
            yield
            for fc in range(4):
                C.stt(yT[:, fc, tsl], pb[:, 128 * fc:128 * fc + 128], hgc[:, fc:fc + 1], zT[:, fc, tsl],
                      ALU.mult, ALU.mult, [kb, "hgc", "g_zT"], ["g_yT"])
                yield

        Sb2 = [Sb, C.sb("g_Sb1", [128, 2, 512], BF16, st)]
        if nchunk > 0:
            run_interleaved([genA(0)])
        for k in range(nchunk):
            gens = [genBs(k), genBo(k)]
            if k + 1 < nchunk:
                gens.insert(0, genA(k + 1))
            run_interleaved(gens)
        if BISECT in (None, "all1"):
            out_proj(C, yT, 4, wo, "g_yT", "g_wbuf")


BISECT = None


class StopPhase(Exception):
    pass


def cut(tag):
    if BISECT == tag:
        raise StopPhase()


def layer1(C, cin):
    C.rT1 = C.sb("rT1", [32, S], F32)
    C.wab = C.sb("wab", [32, 1024], F32)
    C.wqk = C.sb("g_wqk", [128, 8, 512], BF16)
    load_w(C, C.wqk[:, :, 0:256], cin["od_w_in"], GLA_Q0, 256, "g_wqk")
    load_w(C, C.wqk[:, :, 256:512], cin["od_w_in"], GLA_K0, 256, "g_wqk")
    phase_norm(C, cin["norm_g1"], "l1")
    if BISECT == "norm":
        return
    phase_gla_prep(C, cin["od_w_in"], cin["gla_wab"])
    if BISECT == "prep":
        return
    for h in range(4 if BISECT is None else 1):
        phase_gla_head(C, h, cin["od_w_in"], cin["gla_head_g"], cin["od_w_out"])


EV_Q0, EV_K0, EV_V0, EV_O0, EV_I0, EV_U0, EV_Z0 = 0, 512, 1024, 2048, 3072, 3080, 4104


def phase_ml_prep(C, w_in, bias_d):
    st = ExitStack()
    with st:
        wif = C.sb("wif", [128, 8, 8], BF16, st)
        bias = C.sb("ifbias", [128, 128], F32, st)
        G = C.sb("gatesG", [128, 16, 8], F32, st)
        load_w(C, wif[:], w_in, EV_I0, 8, "wif")
        C.dma("sp", bias[:], bias_d, [], ["ifbias"], "ifbias")
        pm, pk = C.psf[0], ("psf", 0)
        for k in range(16):
            for j in range(8):
                C.mm(pm[:, 8 * k:8 * k + 8], C.hnT[:, j, 128 * k:128 * k + 128], wif[:, j, :], j == 0, j == 7,
                     ["wif", "hnT"], pk)
        C.tt("dve", G[:].rearrange("p k g -> p (k g)"), pm[:, 0:128], bias[:], ALU.add, [pk, "ifbias"], ["gatesG"])
        lfv = C.lfn[:].rearrange("p (k h) -> p k h", h=4)
        C.act(lfv, G[:, :, 4:8], AF.Exp, ["gatesG"], ["lfn"], scale=-1.0)
        C.act(C.lfn[:], C.lfn[:], AF.Ln, ["lfn", "const"], ["lfn"], bias=C.cb[:, 1:2])
        p1, k1 = C.psf[1], ("psf", 1)
        C.mm(p1[:, 0:64], C.tri[:], C.lfn[:], True, True, ["lfn", "const"], k1)
        C.tt("dve", C.cbias[:].rearrange("p (k h) -> p k h", h=4), p1[:, 0:64].rearrange("p (k h) -> p k h", h=4),
             G[:, :, 0:4], ALU.add, [k1, "gatesG"], ["cbias"])
        C.P.emit_phase()


def phase_ml_head(C, h, w_in, w_out, cw_d, cb_d, hg_d):
    st = ExitStack()
    with st:
        try:
            _ml_head(C, st, h, w_in, w_out, cw_d, cb_d, hg_d)
        except StopPhase:
            pass
        C.P.emit_phase()


def _ml_head(C, st, h, w_in, w_out, cw_d, cb_d, hg_d):
    wq = C.sb("m_wq", [128, 8, 256], BF16, st)
    wvo = C.sb("m_wvo", [128, 8, 512], BF16, st)
    wz = C.sb("m_wz", [128, 8, 256], BF16, st)
    wo = C.sb("m_wo", [128, 2, D], BF16, st)
    cw = C.sb("m_cw", [128, 2, 4], F32, st)
    cbv = C.sb("m_cb", [128, 8], F32, st)
    hgc = C.sb("m_hgc", [128, 8], F32, st)
    pre = C.sb("m_pre", [128, S + 4], F32, st)
    acc = C.sb("m_acc", [128, S], F32, st)
    qT = C.sb("m_qT", [128, S], BF16, st)
    kT = C.sb("m_kT", [128, S], BF16, st)
    v_sb = C.sb("m_v", [128, 16, 258], BF16, st)
    o_sb = C.sb("m_o", [128, 16, 256], BF16, st)
    zT = C.sb("m_zT", [128, 2, S], BF16, st)
    yT = C.sb("m_yT", [128, 2, S], BF16, st)
    Cf = C.sb("m_Cf", [128, 258], F32, st)
    Cb = C.sb("m_Cb", [128, 258], BF16, st)
    lfbc = C.sb("m_lfbc", [128, 128], F32, st)
    eB = C.sb("m_eB", [128, 128], F32, st)
    w = C.sb("m_w", [128, 128], F32, st)
    wm = C.sb("m_wm", [128, 128], F32, st)
    sc = C.sb("m_sc", [128, 128], BF16, st)
    qs = C.sb("m_qs", [128, 128], BF16, st)
    vt = C.sb("m_vt", [128, 258], BF16, st)
    kTok = C.sb("m_kTok", [128, 128], BF16, st)
    hg = C.sb("m_hg", [128, 256], F32, st)
    hgn = C.sb("m_hgn", [128, 256], BF16, st)
    junk = C.sb("m_junk", [128, 256], BF16, st)
    ssq = C.sb("m_ssq", [128, 2], F32, st)
    rstd = C.sb("m_rstd", [128, 2], F32, st)
    r = C.sb("m_r", [128, 2], F32, st)
    gB = C.sb("m_gB", [128, 2], F32, st)
    wk4 = C.sb("m_wk4", [128, 4], F32, st)

    C.dma("sp", cw[:, 0, :], cw_d[128 * h:128 * h + 128, :], [], ["m_cw"], "m_cw")
    C.dma("sp", cw[:, 1, :], cw_d[512 + 128 * h:512 + 128 * h + 128, :], [], ["m_cw"], "m_cw")
    C.dma("sp", cbv[:], cb_d, [], ["m_cb"], "m_cb")
    C.dma("sp", hgc[:], hg_d, [], ["m_hgc"], "m_hgc")
    load_w(C, wq[:, :, 0:128], w_in, EV_Q0 + 128 * h, 128, "m_wq")
    load_w(C, wq[:, :, 128:256], w_in, EV_K0 + 128 * h, 128, "m_wq")
    load_w(C, wvo[:, :, 0:256], w_in, EV_V0 + 256 * h, 256, "m_wvo")
    load_w(C, wvo[:, :, 256:512], w_in, EV_O0 + 256 * h, 256, "m_wvo")
    load_w(C, wz[:], w_in, EV_Z0 + 256 * h, 256, "m_wz")
    load_w(C, wo[:], w_out, 0, D, "m_wo", rows0=256 * h, nj=2)
    C.memset("dve", ssq[:], 1.0, ["m_ssq"])
    C.memset("dve", pre[:, 0:4], 0.0, ["m_pre0"])
    C.memset("pool", v_sb[:, :, 256:258], 1.0, ["m_vones"])
    cut("mc_dma")

    n = 0
    for which in range(2):
        for tg in range(4):
            pm, pk = C.psf[n % 4], ("psf", n % 4)
            n += 1
            for j in range(8):
                C.mm(pm[:], wq[:, j, 128 * which:128 * which + 128], C.hnT[:, j, 512 * tg:512 * tg + 512],
                     j == 0, j == 7, ["m_wq", "hnT"], pk)
            C.copy("act", pre[:, 4 + 512 * tg:4 + 512 * tg + 512], pm[:], [pk], ["m_pre"])
        cut("mc_proj")
        C.ts("dve", acc[:], pre[:, 1:1 + S], cw[:, which, 0:1], None, ALU.mult, None, ["m_pre", "m_pre0", "m_cw"], ["m_acc"])
        for j in range(1, 4):
            C.stt(acc[:], pre[:, 1 + j:1 + j + S], cw[:, which, j:j + 1], acc[:], ALU.mult, ALU.add,
                  ["m_pre", "m_pre0", "m_cw", "m_acc"], ["m_acc"])
        cut("mc_conv")
        bcol = cbv[:, 4 * which + h:4 * which + h + 1]
        if which == 0:
            C.act(acc[:], acc[:], AF.Silu, ["m_acc", "m_cb"], ["m_acc"], bias=bcol)
            C.ts("dve", qT[:], acc[:], 128.0 ** -0.5, None, ALU.mult, None, ["m_acc"], ["m_qT"])
        else:
            C.act(kT[:], acc[:], AF.Silu, ["m_acc", "m_cb"], ["m_kT"], bias=bcol)
    cut("mc_qk")
    for k in range(16):
        pm, pk = C.psf[n % 4], ("psf", n % 4)
        n += 1
        for j in range(8):
            C.mm(pm[:], C.hnT[:, j, 128 * k:128 * k + 128], wvo[:, j, :], j == 0, j == 7, ["m_wvo", "hnT"], pk)
        C.copy("dve", v_sb[:, k, 0:256], pm[:, 0:256], [pk], [("m_v", k)])
        C.act(o_sb[:, k, :], pm[:, 256:512], AF.Sigmoid, [pk], [("m_o", k)])
    cut("mc_vo")
    for fc in range(2):
        for tg in range(4):
            pm, pk = C.psf[n % 4], ("psf", n % 4)
            n += 1
            for j in range(8):
                C.mm(pm[:], wz[:, j, 128 * fc:128 * fc + 128], C.hnT[:, j, 512 * tg:512 * tg + 512], j == 0, j == 7,
                     ["m_wz", "hnT"], pk)
            C.act(zT[:, fc, 512 * tg:512 * tg + 512], pm[:], AF.Silu, [pk], ["m_zT"])

    nchunk = 16
    if BISECT and BISECT.startswith("m_"):
        nchunk = int(BISECT[2:])
    sc2 = [sc, C.sb("m_sc1", [128, 128], BF16, st)]
    qs2 = [qs, C.sb("m_qs1", [128, 128], BF16, st)]
    eg2 = [C.sb("m_eg%d" % i, [128, 2], F32, st) for i in range(2)]

    Cb2 = [Cb, C.sb("m_Cb1", [128, 258], BF16, st)]

    def genA(k):
        b = k % 2
        tsl = slice(128 * k, 128 * k + 128)
        col = 4 * k + h
        p0, k0 = C.psf[0], ("psf", 0)
        p1, k1 = C.psf[1], ("psf", 1)
        p3, k3 = C.psf[3 + b], ("psf", 3 + b)
        C.ts("pool", lfbc[:], C.ones[:], C.lfn[:, col:col + 1], 0.0, ALU.mult, ALU.add, ["const", "lfn"], ["m_lfbc"])
        C.mm(p0[:, 0:128], kT[:, tsl], qT[:, tsl], True, True, ["m_kT", "m_qT"], k0)
        yield
        C.mm(p1[:, 0:128], lfbc[:], C.trin[:], True, True, ["m_lfbc", "const"], k1)
        pb0, kb0 = C.psb[0], ("psb", 0)
        if k < 15:
            C.tr(pb0[:, 0:128], kT[:, tsl], C.ident[:], ["m_kT", "const"], kb0)
        yield
        C.copy("dve", gB[:, 0:1], p1[:, 127:128], [k1], ["m_gB"])
        C.act(w[:], p1[:, 0:128], AF.Exp, [k1, "cbias"], ["m_w"], bias=C.cbias[:, col:col + 1])
        yield
        C.act(eB[:], p1[:, 0:128], AF.Exp, [k1], ["m_eB"])
        C.tt("pool", wm[:], w[:], C.tri[:], ALU.mult, ["m_w", "const"], ["m_wm"])
        yield
        if k < 15:
            C.act(wk4[:], C.cbias[:, 4 * k:4 * k + 4], AF.Exp, ["cbias", "m_gB"], ["m_wk4"], bias=gB[:, 0:1])
        C.tt("dve", sc2[b][:], p0[:, 0:128], wm[:], ALU.mult, [k0, "m_wm"], [("m_sc", b)])
        yield
        C.tt("pool", qs2[b][:], qT[:, tsl], eB[:], ALU.mult, ["m_qT", "m_eB"], [("m_qs", b)])
        C.copy("dve", eg2[b][:, 0:2], eB[:, 126:128], ["m_eB"], [("m_eg", b)])
        yield
        if k < 15:
            C.copy("act", kTok[:], pb0[:, 0:128], [kb0], ["m_kTok"])
            C.ts("dve", vt[:, 0:257], v_sb[:, k, 0:257], wk4[:, h:h + 1], None, ALU.mult, None,
                 [("m_v", k), "m_vones", "m_wk4"], ["m_vt"])
            yield
            C.mm(p3[:, 0:257], kTok[:], vt[:, 0:257], True, True, ["m_kTok", "m_vt"], k3)
            yield

    def genBs(k):
        b = k % 2
        p3, k3 = C.psf[3 + b], ("psf", 3 + b)
        if k < 15:
            if k == 0:
                C.copy("dve", Cf[:, 0:257], p3[:, 0:257], [k3], ["m_Cf"])
            else:
                C.stt(Cf[:, 0:257], Cf[:, 0:257], eg2[b][:, 1:2], p3[:, 0:257], ALU.mult, ALU.add,
                      [k3, ("m_eg", b), "m_Cf"], ["m_Cf"])
            yield
            C.copy("act", Cb2[(k + 1) % 2][:, 0:257], Cf[:, 0:257], ["m_Cf"], [("m_Cb", (k + 1) % 2)])
            yield

    r2 = [r, C.sb("m_r1", [128, 2], F32, st)]
    hg2 = [hg, C.sb("m_hg1", [128, 256], F32, st)]
    hgn2 = [hgn, C.sb("m_hgn1", [128, 256], BF16, st)]
    junk2 = [junk, C.sb("m_junk1", [128, 256], BF16, st)]
    ssq2 = [ssq, C.sb("m_ssq1", [128, 2], F32, st)]
    rstd2 = [rstd, C.sb("m_rstd1", [128, 2], F32, st)]
    C.memset("dve", ssq2[1][:], 1.0, [("m_ssq", 1)])

    def genBo(k):
        b = k % 2
        tsl = slice(128 * k, 128 * k + 128)
        p2, k2 = (C.psf[2], ("psf", 2)) if b == 0 else (C.psf[5], ("psf", 5))
        r_, hg_, hgn_, junk_, ssq_, rstd_ = r2[b], hg2[b], hgn2[b], junk2[b], ssq2[b], rstd2[b]
        kr, kr1, khg, kjk, kss, krs, khn = (("m_r", b), ("m_r1", b), ("m_hg", b), ("m_junk", b), ("m_ssq", b),
                                            ("m_rstd", b), ("m_hgn", b))
        C.mm(p2[:, 0:257], sc2[b][:], v_sb[:, k, 0:257], True, k == 0, [("m_sc", b), ("m_v", k), "m_vones"], k2)
        if k > 0:
            C.mm(p2[:, 0:257], qs2[b][:], Cb2[b][:, 0:257], False, True, [("m_qs", b), ("m_Cb", b)], k2)
        yield
        C.ts("dve", r_[:, 0:1], p2[:, 256:257], -1.0, 1.0, ALU.mult, ALU.max, [k2], [kr])
        C.ts("dve", r_[:, 1:2], p2[:, 256:257], 1.0, None, ALU.max, None, [k2], [kr1])
        yield
        C.tt("dve", r_[:, 0:1], r_[:, 0:1], r_[:, 1:2], ALU.max, [kr, kr1], [kr])
        yield
        C.P.op("dve", lambda e: e.reciprocal(out=r_[:, 0:1], in_=r_[:, 0:1]), reads=[kr], writes=[kr])
        yield
        C.stt(hg_[:], p2[:, 0:256], r_[:, 0:1], o_sb[:, k, :], ALU.mult, ALU.mult, [k2, kr, ("m_o", k)], [khg])
        yield
        C.act(junk_[:], hg_[:], AF.Square, [khg], [kjk, kss], accum_out=ssq_[:, 0:1])
        yield
        C.ts("dve", rstd_[:], ssq_[:], 1.0 / 256, EPS, ALU.mult, ALU.add, [kss], [krs])
        yield
        C.act(rstd_[:], rstd_[:], AF.Sqrt, [krs], [krs])
        yield
        C.P.op("dve", lambda e: e.reciprocal(out=rstd_[:], in_=rstd_[:]), reads=[krs], writes=[krs])
        yield
        C.act(hgn_[:], hg_[:], AF.Copy, [khg, krs], [khn], scale=rstd_[:, 0:1])
        yield
        pb, kb = C.psb[1], ("psb", 1)
        for fc in range(2):
            C.tr(pb[:, 128 * fc:128 * fc + 128], hgn_[:, 128 * fc:128 * fc + 128], C.ident[:], [khn, "const"], kb)
        yield
        for fc in range(2):
            C.stt(yT[:, fc, tsl], pb[:, 128 * fc:128 * fc + 128], hgc[:, 2 * h + fc:2 * h + fc + 1], zT[:, fc, tsl],
                  ALU.mult, ALU.mult, [kb, "m_hgc", "m_zT"], ["m_yT"])
            yield

    if nchunk > 0:
        run_interleaved([genA(0)])
    carry = []
    for k in range(nchunk):
        must = [genBs(k)]
        if k + 1 < nchunk:
            must.insert(0, genA(k + 1))
        bo = genBo(k)
        active = must + carry + [bo]
        must_left = list(must)
        while must_left:
            for g in list(active):
                try:
                    next(g)
                except StopIteration:
                    active.remove(g)
                    if g in must_left:
                        must_left.remove(g)
        for g in carry:
            if g in active:
                for _ in g:
                    pass
                active.remove(g)
        carry = [g for g in active]
    for g in carry:
        for _ in g:
            pass
    out_proj(C, yT, 2, wo, "m_yT", "m_wo")


def cmul(C, eng, outR, outI, aR, aI, bR, bI, t1, t2, rk, wk, coarse=False):
    k1, k2, kR, kI = (wk, wk, wk, wk) if coarse else (wk + "_t1", wk + "_t2", wk + "_R", wk + "_I")
    C.tt(eng, t1, aR, bR, ALU.mult, rk, [k1])
    C.tt(eng, t2, aI, bI, ALU.mult, rk, [k2])
    C.tt(eng, outR, t1, t2, ALU.subtract, [k1, k2], [kR])
    C.tt(eng, t1, aR, bI, ALU.mult, rk, [k1])
    C.tt(eng, t2, aI, bR, ALU.mult, rk, [k2])
    C.tt(eng, outI, t1, t2, ALU.add, [k1, k2], [kI])


def phase_s5_setup(C, cin, scr):
    st = ExitStack()
    with st:
        LR = C.sb("s_LR", [128, 32], F32, st)
        LI = C.sb("s_LI", [128, 32], F32, st)
        DT = C.sb("s_DT", [128, 32], F32, st)
        TH = C.sb("s_TH", [128, 32], F32, st)
        LD = C.sb("s_LD", [128, 32], F32, st)
        cs = C.sb("s_cs", [128, 32], F32, st)
        sn = C.sb("s_sn", [128, 32], F32, st)
        rho = C.sb("s_rho", [128, 32], F32, st)
        rhi = C.sb("s_rhi", [128, 32], F32, st)
        aiR = C.sb("s_aiR", [128, 32], F32, st)
        aiI = C.sb("s_aiI", [128, 32], F32, st)
        t1 = C.sb("s_t1", [128, 32], F32, st)
        t2 = C.sb("s_t2", [128, 32], F32, st)
        t3 = C.sb("s_t3", [128, 32], F32, st)
        fR = C.sb("s_fR", [128, 32], F32, st)
        fI = C.sb("s_fI", [128, 32], F32, st)
        pi2 = C.sb("s_pi2", [128, 1], F32, st)
        mD = C.sb("s_mD", [128, 128], F32, st)
        P0 = C.sb("s_P0", [128, 128], F32, st)
        dcol = C.sb("s_dcol", [128, 64], F32, st)
        ER, EI = C.ER, C.EI
        C.dma("sp", LR[:], cin["s5_lr"], [], ["s_in"], "s_in")
        C.dma("sp", LI[:], cin["s5_li"], [], ["s_in"], "s_in")
        C.dma("sp", DT[:], cin["s5_ldt"], [], ["s_in"], "s_in")
        C.dma("sp", mD[:], cin["s5_maskD"], [], ["s_in"], "s_in")
        C.dma("sp", P0[:], cin["s5_P0"], [], ["s_in"], "s_in")
        C.dma("sp", dcol[:], cin["s5_dcol"], [], ["s_in"], "s_in")
        C.memset("dve", pi2[:], math.pi / 2.0, ["s_pi2"])
        K = ["s_in", "s_k"]
        C.act(DT[:], DT[:], AF.Exp, ["s_in"], ["s_k"])
        C.tt("dve", TH[:], LI[:], DT[:], ALU.mult, K, ["s_k"])
        C.tt("dve", LD[:], LR[:], DT[:], ALU.mult, K, ["s_k"])
        C.act(rho[:], LD[:], AF.Exp, K, ["s_k"])
        C.act(rhi[:], LD[:], AF.Exp, K, ["s_k"], scale=-1.0)
        C.act(sn[:], TH[:], AF.Sin, K, ["s_k"], scale=1.0 / 16.0)
        C.act(cs[:], TH[:], AF.Sin, K + ["s_pi2"], ["s_k"], scale=-1.0 / 16.0, bias=pi2[:, 0:1])
        for _ in range(4):
            C.tt("dve", t1[:], cs[:], cs[:], ALU.mult, K, ["s_k"])
            C.tt("dve", t2[:], sn[:], sn[:], ALU.mult, K, ["s_k"])
            C.tt("dve", t3[:], cs[:], sn[:], ALU.mult, K, ["s_k"])
            C.tt("dve", cs[:], t1[:], t2[:], ALU.subtract, K, ["s_k"])
            C.ts("dve", sn[:], t3[:], 2.0, None, ALU.mult, None, K, ["s_k"])
        C.memset("dve", ER[:, :, 7:8], 1.0, ["s_k"])
        C.memset("dve", EI[:, :, 7:8], 0.0, ["s_k"])
        C.tt("dve", ER[:, :, 8], rho[:], cs[:], ALU.mult, K, ["s_k"])
        C.tt("dve", EI[:, :, 8], rho[:], sn[:], ALU.mult, K, ["s_k"])
        C.tt("dve", aiR[:], rhi[:], cs[:], ALU.mult, K, ["s_k"])
        C.tt("dve", aiI[:], rhi[:], sn[:], ALU.mult, K, ["s_k"])
        C.ts("dve", aiI[:], aiI[:], -1.0, None, ALU.mult, None, K, ["s_k"])
        for e in range(1, 16):
            cmul(C, "dve", ER[:, :, 8 + e], EI[:, :, 8 + e], ER[:, :, 7 + e], EI[:, :, 7 + e], ER[:, :, 8], EI[:, :, 8],
                 t1[:], t2[:], K, "s_k", coarse=True)
        C.copy("dve", ER[:, :, 6], aiR[:], K, ["s_k"])
        C.copy("dve", EI[:, :, 6], aiI[:], K, ["s_k"])
        for e in range(1, 7):
            cmul(C, "dve", ER[:, :, 6 - e], EI[:, :, 6 - e], ER[:, :, 7 - e], EI[:, :, 7 - e], aiR[:], aiI[:],
                 t1[:], t2[:], K, "s_k", coarse=True)
        C.tt("dve", t1[:], LR[:], LR[:], ALU.mult, K, ["s_k"])
        C.tt("dve", t2[:], LI[:], LI[:], ALU.mult, K, ["s_k"])
        C.tt("dve", t1[:], t1[:], t2[:], ALU.add, K, ["s_k"])
        C.P.op("dve", lambda e: e.reciprocal(out=t3[:], in_=t1[:]), reads=K, writes=["s_k"])
        C.ts("dve", t1[:], ER[:, :, 8], -1.0, None, ALU.add, None, K, ["s_k"])
        C.tt("dve", fR[:], t1[:], LR[:], ALU.mult, K, ["s_k"])
        C.tt("dve", t2[:], EI[:, :, 8], LI[:], ALU.mult, K, ["s_k"])
        C.tt("dve", fR[:], fR[:], t2[:], ALU.add, K, ["s_k"])
        C.tt("dve", fR[:], fR[:], t3[:], ALU.mult, K, ["s_k"])
        C.tt("dve", fI[:], EI[:, :, 8], LR[:], ALU.mult, K, ["s_k"])
        C.tt("dve", t2[:], t1[:], LI[:], ALU.mult, K, ["s_k"])
        C.tt("dve", fI[:], fI[:], t2[:], ALU.subtract, K, ["s_k"])
        C.tt("dve", fI[:], fI[:], t3[:], ALU.mult, K, ["s_k"])

        bR = C.sb("s_bR", [128, 4, 16], F32, st)
        bI = C.sb("s_bI", [128, 4, 16], F32, st)
        cR = C.sb("s_cR", [128, 4, 16], F32, st)
        cI = C.sb("s_cI", [128, 4, 16], F32, st)
        BbR = C.sb("s_BbR", [128, 4, 16], F32, st)
        BbI = C.sb("s_BbI", [128, 4, 16], F32, st)
        u1 = C.sb("s_u1", [128, 4, 16], F32, st)
        u2 = C.sb("s_u2", [128, 4, 16], F32, st)
        KWR = C.sb("s_KWR", [128, 4, 16, 16], F32, st)
        KWI = C.sb("s_KWI", [128, 4, 16, 16], F32, st)
        QR = C.sb("s_QR", [128, 4, 24, 16], F32, st)
        QI = C.sb("s_QI", [128, 4, 24, 16], F32, st)
        v1 = C.sb("s_v1", [128, 4, 24, 16], F32, st)
        v2 = C.sb("s_v2", [128, 4, 24, 16], F32, st)
        v3 = C.sb("s_v3", [128, 4, 24, 16], F32, st)
        v4 = C.sb("s_v4", [128, 4, 24, 16], F32, st)
        w3 = C.sb("s_w3", [128, 4, 16, 16], F32, st)
        w4 = C.sb("s_w4", [128, 4, 16, 16], F32, st)
        w1 = C.sb("s_w1", [128, 4, 16, 16], F32, st)
        w2 = C.sb("s_w2", [128, 4, 16, 16], F32, st)
        Tsb = C.sb("s_Tsb", [128, 8, 256], BF16, st)
        Wsb = C.sb("s_Wsb", [128, 8, 256], BF16, st)
        OQb = C.sb("s_OQb", [128, 4, 2, 256], BF16, st)
        tmpT = C.sb("s_tmpT", [128, 128], F32, st)
        for b in range(8):
            g2s = slice(4 * b, 4 * b + 4)
            for t, nm in ((bR, "s5_br"), (bI, "s5_bi"), (cR, "s5_cr"), (cI, "s5_ci")):
                C.dma("sp", t[:], cin[nm][:, g2s, :], [], ["s_bc"], "s_bc")
            KB = ["s_bc", "s_k", "s_b"]
            fRb = fR[:, g2s].unsqueeze(2).to_broadcast([128, 4, 16])
            fIb = fI[:, g2s].unsqueeze(2).to_broadcast([128, 4, 16])
            cmul(C, "dve", BbR[:], BbI[:], bR[:], bI[:], fRb, fIb, u1[:], u2[:], KB, "s_b")
            KBB = KB + ["s_b_R", "s_b_I"]
            eR = ER[:, g2s, :].unsqueeze(3).to_broadcast([128, 4, 24, 16])
            eI = EI[:, g2s, :].unsqueeze(3).to_broadcast([128, 4, 24, 16])
            ccR = cR[:].unsqueeze(2).to_broadcast([128, 4, 24, 16])
            ccI = cI[:].unsqueeze(2).to_broadcast([128, 4, 24, 16])
            KQ0 = ["s_bc", "s_k"]
            C.tt("pool", v3[:], eR, ccI, ALU.mult, KQ0, ["s_q_t3"])
            C.tt("pool", v4[:], eI, ccR, ALU.mult, KQ0, ["s_q_t4"])
            C.tt("dve", v1[:], eR, ccR, ALU.mult, KQ0, ["s_q_t1"])
            C.tt("dve", v2[:], eI, ccI, ALU.mult, KQ0, ["s_q_t2"])
            C.tt("dve", QR[:], v1[:], v2[:], ALU.subtract, ["s_q_t1", "s_q_t2"], ["s_q_R"])
            C.stt(QI[:], v3[:], -1.0, v4[:], ALU.mult, ALU.subtract, ["s_q_t3", "s_q_t4"], ["s_q_I"])
            eR = ER[:, g2s, 7:23].unsqueeze(3).to_broadcast([128, 4, 16, 16])
            eI = EI[:, g2s, 7:23].unsqueeze(3).to_broadcast([128, 4, 16, 16])
            bbR = BbR[:].unsqueeze(2).to_broadcast([128, 4, 16, 16])
            bbI = BbI[:].unsqueeze(2).to_broadcast([128, 4, 16, 16])
            C.tt("pool", w3[:], eR, bbI, ALU.mult, KBB, ["s_kw_t3"])
            C.tt("pool", w4[:], eI, bbR, ALU.mult, KBB, ["s_kw_t4"])
            C.tt("pool", KWI[:], w3[:], w4[:], ALU.add, ["s_kw_t3", "s_kw_t4"], ["s_kw_I"])
            C.tt("dve", w1[:], eR, bbR, ALU.mult, KBB, ["s_kw_t1"])
            C.tt("dve", w2[:], eI, bbI, ALU.mult, KBB, ["s_kw_t2"])
            C.tt("dve", KWR[:], w1[:], w2[:], ALU.subtract, ["s_kw_t1", "s_kw_t2"], ["s_kw_R"])
            KQ = ["s_kw_R", "s_kw_I", "s_q_R", "s_q_I", "const", "s_in"]
            C.copy("act", OQb[:, :, 0, :], QR[:, :, 8:24, :].rearrange("p g e o -> p g (e o)"), KQ, ["s_OQb"])
            C.copy("act", OQb[:, :, 1, :], QI[:, :, 8:24, :].rearrange("p g e o -> p g (e o)"), KQ, ["s_OQb"])
            C.dma("sp", scr["OQ"][4 * b:4 * b + 4].rearrange("g p r c -> p g r c"), OQb[:], ["s_OQb"], [], "s_oq_out")
            for g2l in range(4):
                for gp in range(2):
                    gl = 2 * g2l + gp
                    ps_ = slice(64 * gp, 64 * gp + 64)
                    pm, pk = C.psf[gl % 2], ("psf", gl % 2)
                    C.mm(pm[:, 0:256], KWR[ps_, g2l, 0:8, :].rearrange("p s i -> p (s i)"),
                         QR[ps_, g2l, 0:16, :].rearrange("p e o -> p (e o)"), True, False, KQ, pk)
                    C.mm(pm[:, 0:256], KWI[ps_, g2l, 0:8, :].rearrange("p s i -> p (s i)"),
                         QI[ps_, g2l, 0:16, :].rearrange("p e o -> p (e o)"), False, True, KQ, pk)
                    C.tt("dve", tmpT[:], pm[:, 0:128], mD[:], ALU.mult, [pk, "s_in"], ["s_tmpT"])
                    g = 8 * b + gl
                    C.stt(Tsb[:, gl, 0:128], P0[:], dcol[:, g:g + 1], tmpT[:], ALU.mult, ALU.add,
                          ["s_tmpT", "s_in"], ["s_Tsb"])
                    C.copy("act", Tsb[:, gl, 128:256], pm[:, 128:256], [pk], ["s_Tsb"])
                    pw, pkw = C.psf[2 + gl % 2], ("psf", 2 + gl % 2)
                    idn = C.identf[ps_, 64 * gp:64 * gp + 64]
                    for n, (src, half) in enumerate(((KWR, 0), (KWI, 0), (KWR, 1), (KWI, 1))):
                        C.tr(pw[:, 64 * n:64 * n + 64], src[ps_, g2l, 8 * half:8 * half + 8, :].rearrange("p s i -> p (s i)"),
                             idn, KQ, pkw)
                    C.copy("dve", Wsb[:, gl, :], pw[:, 0:256], [pkw], ["s_Wsb"])
            C.dma("sp", scr["T"][8 * b:8 * b + 8].rearrange("g p c -> p g c"), Tsb[:], ["s_Tsb"], [], "s_t_out")
            C.dma("sp", scr["W"][8 * b:8 * b + 8].rearrange("g p c -> p g c"), Wsb[:], ["s_Wsb"], [], "s_w_out")
        C.P.emit_phase()


def phase_s5_in(C, cin, scr, XR, XI, u_cm):
    st = ExitStack()
    with st:
        wu = C.sb("s_wu", [128, 8, 512], BF16, st)
        Wsb = C.sb("s1_Wsb", [128, 8, 256], BF16, st)
        Usb = [C.sb("s1_Usb%d" % i, [128, 256], BF16, st) for i in range(2)]
        n = 0
        for half in range(2):
            load_w(C, wu[:], cin["ev_w_in"], EV_U0 + 512 * half, 512, "s_wu")
            for s in range(16):
                pm, pk = C.psf[n % 2], ("psf", n % 2)
                n += 1
                hv = C.hnT[:].rearrange("p j (c s) -> p j c s", s=16)
                for j in range(8):
                    C.mm(pm[:], hv[:, j, :, s], wu[:, j, :], j == 0, j == 7, ["s_wu", "hnT"], pk)
                C.copy("act" if n % 2 else "dve", u_cm[:, 32 * half:32 * half + 32, 15 - s, :],
                       pm[:].rearrange("p (g i) -> p g i", i=16), [pk], ["s_ucm"])
        for b in range(8):
            C.dma("sp", Wsb[:], scr["W"][8 * b:8 * b + 8].rearrange("g p c -> p g c"), [], ["s1_Wsb"], "s1_Wsb")
            def gen_in(gl):
                g = 8 * b + gl
                gp, g2 = g % 2, g // 2
                U = Usb[g % 2]
                uk = ("s1_U", g % 2)
                pb, kb = C.psb[g % 2], ("psb", g % 2)
                for hf in range(2):
                    C.tr(pb[:, 128 * hf:128 * hf + 128], u_cm[:, g, 8 * hf:8 * hf + 8, :].rearrange("p s i -> p (s i)"),
                         C.ident[:], ["s_ucm", "const"], kb)
                yield
                C.copy("act" if g % 2 else "dve", U[:], pb[:, 0:256], [kb], [uk])
                yield
                C.dma("sp", scr["U"][g], U[:], [uk], [], ("s1_uo", g % 2))
                px, kx = C.psf[2 + g2 % 2], ("psf", 2 + g2 % 2)
                ps_ = slice(64 * gp, 64 * gp + 64)
                for ri in range(2):
                    C.mm(px[ps_, 128 * ri:128 * ri + 128], Wsb[:, gl, 64 * ri:64 * ri + 64], U[:, 0:128], True, False,
                         ["s1_Wsb", uk], kx)
                    C.mm(px[ps_, 128 * ri:128 * ri + 128], Wsb[:, gl, 128 + 64 * ri:128 + 64 * ri + 64], U[:, 128:256],
                         False, True, ["s1_Wsb", uk], kx)
                yield
                if gp == 1:
                    C.copy("dve", XR[:, g2, :], px[:, 0:128], [kx], ["s_XR"])
                    C.copy("act", XI[:, g2, :], px[:, 128:256], [kx], ["s_XI"])
                yield

            for gl in range(0, 8, 2):
                run_interleaved([gen_in(gl), gen_in(gl + 1)])
        C.P.emit_phase()


def phase_s5_scan(C, XR, XI, XRb, XIb):
    st = ExitStack()
    with st:
        t1 = C.sb("sc_t1", [128, 32], F32, st)
        t2 = C.sb("sc_t2", [128, 32], F32, st)
        t3 = C.sb("sc_t3", [128, 32], F32, st)
        t4 = C.sb("sc_t4", [128, 32], F32, st)
        AR, AI = C.ER[:, :, 23], C.EI[:, :, 23]
        for c in range(1, 128):
            C.tt("dve", t1[:], AR, XR[:, :, c - 1], ALU.mult, ["s_XR"], ["sc_t1"])
            C.tt("dve", t2[:], AI, XI[:, :, c - 1], ALU.mult, ["s_XI"], ["sc_t2"])
            C.tt("dve", t3[:], AR, XI[:, :, c - 1], ALU.mult, ["s_XI"], ["sc_t3"])
            C.tt("dve", t4[:], AI, XR[:, :, c - 1], ALU.mult, ["s_XR"], ["sc_t4"])
            C.tt("dve", t1[:], t1[:], t2[:], ALU.subtract, ["sc_t1", "sc_t2"], ["sc_t1"])
            C.tt("dve", t3[:], t3[:], t4[:], ALU.add, ["sc_t3", "sc_t4"], ["sc_t3"])
            C.tt("dve", XR[:, :, c], XR[:, :, c], t1[:], ALU.add, ["s_XR", "sc_t1"], ["s_XR"])
            C.tt("dve", XI[:, :, c], XI[:, :, c], t3[:], ALU.add, ["s_XI", "sc_t3"], ["s_XI"])
        C.memset("dve", XRb[:, :, 0:1], 0.0, ["s_XRb0"])
        C.memset("dve", XIb[:, :, 0:1], 0.0, ["s_XIb0"])
        C.copy("dve", XRb[:, :, 1:128], XR[:, :, 0:127], ["s_XR"], ["s_XRb"])
        C.copy("act", XIb[:, :, 1:128], XI[:, :, 0:127], ["s_XI"], ["s_XIb"])
        C.P.emit_phase()


GELU_C = 0.7978845608028654


def phase_s5_out(C, cin, scr, XRb, XIb, yg_cm):
    st = ExitStack()
    with st:
        Tsb = C.sb("s3_Tsb", [128, 8, 256], BF16, st)
        OQb = C.sb("s3_OQb", [128, 4, 2, 256], BF16, st)
        Ub = C.sb("s3_Ub", [128, 8, 256], BF16, st)
        Ysb = [C.sb("s3_Y%d" % i, [128, 256], F32, st) for i in range(2)]
        xs2 = [C.sb("s3_xs%d" % i, [128, 256], F32, st) for i in range(2)]
        x22 = [C.sb("s3_x2%d" % i, [128, 256], F32, st) for i in range(2)]
        sg2 = [C.sb("s3_sg%d" % i, [128, 256], F32, st) for i in range(2)]
        XK = ["s_XRb", "s_XIb", "s_XRb0", "s_XIb0"]
        for b in range(8):
            C.dma("sp", Tsb[:], scr["T"][8 * b:8 * b + 8].rearrange("g p c -> p g c"), [], ["s3_Tsb"], "s3_Tsb")
            C.dma("sp", OQb[:], scr["OQ"][4 * b:4 * b + 4].rearrange("g p r c -> p g r c"), [], ["s3_OQb"], "s3_OQb")
            C.dma("sp", Ub[:], scr["U"][8 * b:8 * b + 8].rearrange("g p c -> p g c"), [], ["s3_Ub"], "s3_Ub")
            def gen_out(gl):
                g = 8 * b + gl
                par = g % 2
                gp, g2, g2l = g % 2, g // 2, gl // 2
                ps_ = slice(64 * gp, 64 * gp + 64)
                W = ["s3_Tsb", "s3_OQb", "s3_Ub"] + XK
                pa, ka = C.psf[3 * par], ("psf", 3 * par)
                pbk, kbk = C.psf[3 * par + 1], ("psf", 3 * par + 1)
                pt, kt = C.psf[3 * par + 2], ("psf", 3 * par + 2)
                xs, x2, sg = xs2[par], x22[par], sg2[par]
                kxs, kx2, ksg = ("s3_xs", par), ("s3_x2", par), ("s3_sg", par)
                UA, UB = Ub[:, gl, 0:128], Ub[:, gl, 128:256]
                TD, TO = Tsb[:, gl, 0:128], Tsb[:, gl, 128:256]
                C.mm(pa[:, 0:128], TD, UB, True, False, W, ka)
                C.mm(pa[:, 0:128], OQb[ps_, g2l, 0, 0:128], XRb[ps_, g2, :], False, False, W, ka)
                C.mm(pa[:, 0:128], OQb[ps_, g2l, 1, 0:128], XIb[ps_, g2, :], False, True, W, ka)
                C.mm(pbk[:, 0:128], TD, UA, True, False, W, kbk)
                C.mm(pbk[:, 0:128], TO, UB, False, False, W, kbk)
                C.mm(pbk[:, 0:128], OQb[ps_, g2l, 0, 128:256], XRb[ps_, g2, :], False, False, W, kbk)
                C.mm(pbk[:, 0:128], OQb[ps_, g2l, 1, 128:256], XIb[ps_, g2, :], False, True, W, kbk)
                yield
                Y = Ysb[par]
                yk = ("s3_Y", par)
                C.copy("act", Y[:, 0:128], pa[:, 0:128], [ka], [yk])
                C.copy("dve", Y[:, 128:256], pbk[:, 0:128], [kbk], [yk])
                yield
                for hf in range(2):
                    C.tr(pt[:, 128 * hf:128 * hf + 128], Y[:, 128 * hf:128 * hf + 128], C.identf[:], [yk, "const"], kt)
                yield
                C.copy("act", xs[:], pt[:, 0:256], [kt], [kxs])
                yield
                C.tt("pool", x2[:], xs[:], xs[:], ALU.mult, [kxs], [kx2])
                yield
                C.ts("dve", x2[:], x2[:], 0.044715, 1.0, ALU.mult, ALU.add, [kx2], [kx2])
                yield
                C.tt("pool", x2[:], x2[:], xs[:], ALU.mult, [kx2, kxs], [kx2])
                yield
                C.act(sg[:], x2[:], AF.Sigmoid, [kx2], [ksg], scale=2.0 * GELU_C)
                yield
                C.tt("dve", yg_cm[:, :, 16 * g:16 * g + 16], xs[:].rearrange("p (t o) -> p t o", o=16),
                     sg[:].rearrange("p (t o) -> p t o", o=16), ALU.mult, [kxs, ksg], ["s_ygcm"])
                yield

            for gl in range(0, 8, 2):
                run_interleaved([gen_out(gl), gen_out(gl + 1)])
        C.P.emit_phase()


def phase_s5_glu(C, cin, yg_cm, ygT, yT):
    for s in range(16):
        pb, kb = C.psb[s % 2], ("psb", s % 2)
        for j in range(8):
            C.tr(pb[:, 128 * j:128 * j + 128], yg_cm[:, s, 128 * j:128 * j + 128], C.ident[:], ["s_ygcm", "const"], kb)
        dst = ygT.rearrange("p j (c s) -> p j c s", s=16)[:, :, :, s]
        C.copy("act" if s % 2 else "dve", dst, pb[:].rearrange("p (j c) -> p j c", j=8), [kb], ["s4_ygT"])
    C.P.emit_phase()
    sC = ExitStack()
    with sC:
        gw = C.sb("s4_gw", [128, 8, 1024], BF16, sC)
        wz = C.sb("s4_wz", [128, 8, 128], BF16, sC)
        gb = C.sb("s4_gb", [128, 8], F32, sC)
        sgt = [C.sb("s4_sg%d" % i, [128, 512], BF16, sC) for i in range(2)]
        zs = [C.sb("s4_zs%d" % i, [128, 512], BF16, sC) for i in range(2)]
        load_w(C, gw[:, :, 0:512], cin["s5_glu_w"], 0, 512, "s4_gw")
        load_w(C, gw[:, :, 512:1024], cin["s5_glu_w"], 512, 512, "s4_gw")
        C.dma("sp", gb[:], cin["s5_glu_bc"], [], ["s4_gb"], "s4_gb")
        n = 0
        for fo in range(8):
            load_w(C, wz[:], cin["ev_w_in"], EV_Z0 + 1024 + 128 * fo, 128, "s4_wz")
            for tg in range(4):
                tsl = slice(512 * tg, 512 * tg + 512)
                pm, pk = C.psf[n % 2], ("psf", n % 2)
                pz, kz = C.psf[2 + n % 2], ("psf", 2 + n % 2)
                sgk, zsk = ("s4_sg", n % 2), ("s4_zs", n % 2)
                for j in range(8):
                    C.mm(pm[:], gw[:, j, 128 * fo:128 * fo + 128], ygT[:, j, tsl], j == 0, j == 7,
                         ["s4_gw", "s4_ygT"], pk)
                for j in range(8):
                    C.mm(pz[:], wz[:, j, :], C.hnT[:, j, tsl], j == 0, j == 7, ["s4_wz", "hnT"], kz)
                C.act(sgt[n % 2][:], pm[:], AF.Sigmoid, [pk, "s4_gb"], [sgk], bias=gb[:, fo:fo + 1])
                C.act(zs[n % 2][:], pz[:], AF.Silu, [kz], [zsk])
                C.tt("dve", sgt[n % 2][:], sgt[n % 2][:], ygT[:, fo, tsl], ALU.mult, [sgk, "s4_ygT"], [sgk])
                C.tt("pool", yT[:, fo, tsl], sgt[n % 2][:], zs[n % 2][:], ALU.mult, [sgk, zsk], ["s4_yT"])
                n += 1
        C.P.emit_phase()
    sD = ExitStack()
    with sD:
        wo = C.sb("s4_wo", [128, 8, D], BF16, sD)
        load_w(C, wo[:], cin["ev_w_out"], 0, D, "s4_wo", rows0=1024, nj=8)
        out_proj(C, yT, 8, wo, "s4_yT", "s4_wo")
        C.P.emit_phase()


def layer0_s5(C, cin):
    scr = {
        "T": C.dram_scr("scr_T", [64, 128, 256], BF16),
        "W": C.dram_scr("scr_W", [64, 128, 256], BF16),
        "OQ": C.dram_scr("scr_OQ", [32, 128, 2, 256], BF16),
        "U": C.dram_scr("scr_U", [64, 128, 256], BF16),
    }
    sE = ExitStack()
    with sE:
        C.ER = C.sb("s_ER", [128, 32, 24], F32, sE)
        C.EI = C.sb("s_EI", [128, 32, 24], F32, sE)
        phase_s5_setup(C, cin, scr)
        bufA = C.sb("s_bufA", [128, 16384], BF16, sE)
        bufB = C.sb("s_bufB", [128, 16384], BF16, sE)
        u_cm = bufA[:].rearrange("p (g s i) -> p g s i", s=16, i=16)
        yg_cm = bufA[:].rearrange("p (s c) -> p s c", c=1024)
        yT = bufA[:].rearrange("p (j t) -> p j t", t=S)
        XR = bufB[:, 0:8192].bitcast(F32).rearrange("p (g c) -> p g c", c=128)
        XI = bufB[:, 8192:16384].bitcast(F32).rearrange("p (g c) -> p g c", c=128)
        ygT = bufB[:].rearrange("p (j t) -> p j t", t=S)
        sXb = ExitStack()
        with sXb:
            XRb = C.sb("s_XRb", [128, 32, 128], BF16, sXb)
            XIb = C.sb("s_XIb", [128, 32, 128], BF16, sXb)
            phase_s5_in(C, cin, scr, XR, XI, u_cm)
            phase_s5_scan(C, XR, XI, XRb, XIb)
            phase_s5_out(C, cin, scr, XRb, XIb, yg_cm)
        phase_s5_glu(C, cin, yg_cm, ygT, yT)


def layer0(C, cin):
    phase_norm(C, cin["norm_g0"], "l0")
    if BISECT == "l0norm":
        return
    if "ml" not in SKIP:
        phase_ml_prep(C, cin["ev_w_in"], cin["ev_if_bias"])
        if BISECT == "ml_prep":
            return
        for h in range(1 if BISECT else 4):
            phase_ml_head(C, h, cin["ev_w_in"], cin["ev_w_out"], cin["ev_conv_wT"], cin["ev_conv_bc"], cin["ev_head_gc"])
    if "s5" not in SKIP:
        layer0_s5(C, cin)


SKIP = set()


CONST_SPECS = {
    "ident_bf": ([128, 128], BF16), "ident_f": ([128, 128], F32), "tri_f": ([128, 128], F32),
    "trin_f": ([128, 128], F32), "trin16_f": ([128, 128], F32), "ones_f": ([128, 128], F32),
}

IN_SPECS = {
    "x": ([S, D], F32),
    "norm_g0": ([128, D], F32), "norm_g1": ([128, D], F32), "norm_gf": ([128, D], F32),
    "od_w_in": ([D, 6160], F32), "gla_wab": ([17, 1024], F32), "gla_head_g": ([128, 16], F32),
    "od_w_out": ([2048, D], F32),
    "ev_w_in": ([D, 6152], F32), "ev_w_out": ([2048, D], F32), "ev_if_bias": ([128, 128], F32),
    "ev_conv_wT": ([1024, 4], F32), "ev_conv_bc": ([128, 8], F32), "ev_head_gc": ([128, 8], F32),
    "s5_lr": ([128, 32], F32), "s5_li": ([128, 32], F32), "s5_ldt": ([128, 32], F32),
    "s5_br": ([128, 32, 16], F32), "s5_bi": ([128, 32, 16], F32), "s5_cr": ([128, 32, 16], F32),
    "s5_ci": ([128, 32, 16], F32), "s5_maskD": ([128, 128], F32), "s5_P0": ([128, 128], F32),
    "s5_dcol": ([128, 64], F32), "s5_glu_w": ([1024, 1024], F32), "s5_glu_bc": ([128, 8], F32),
}


def host_consts():
    tri = np.triu(np.ones((128, 128), np.float32))
    return {
        "ident_bf": np.eye(128, dtype=ml_dtypes.bfloat16), "ident_f": np.eye(128, dtype=np.float32),
        "tri_f": tri, "trin_f": -tri, "trin16_f": -tri / 16.0, "ones_f": np.ones((128, 128), np.float32),
    }


def build_program(layers=(0, 1), final=True):
    nc = bass.Bass("TRN2", target_bir_lowering=False)
    st = ExitStack()
    with st:
        C = Ctx(nc, st)
        cin = {}
        for nm, (shp, dt) in list(CONST_SPECS.items()) + list(IN_SPECS.items()):
            cin[nm] = C.dram_in(nm, shp, dt)
        out_d = C.dram_out("out", [S, D], F32)
        setup_globals(C, cin)
        C.lfn = C.sb("lfn", [128, 64], F32)
        C.cbias = C.sb("cbias", [128, 64], F32)
        phase_load_x(C, cin["x"])
        if 0 in layers:
            layer0(C, cin)
        if 1 in layers:
            layer1(C, cin)
        if final:
            phase_final(C, cin["norm_gf"], out_d)
        else:
            ov = out_d.rearrange("(c s) d -> c s d", s=16)
            for q in range(4):
                C.dma("sp", ov[:, 4 * q:4 * q + 4, :], C.x_sb[:, 4 * q:4 * q + 4, :],
                      [("x", s) for s in range(4 * q, 4 * q + 4)], [], ("out", q % 2))
            C.P.emit_phase()
    return nc


def host_inputs(inp, b):
    f32 = np.float32
    d = dict(host_consts())
    d["x"] = np.ascontiguousarray(inp["x"][b], dtype=f32)
    d["norm_g0"] = np.ascontiguousarray(np.broadcast_to(inp["norm_g"][0], (128, D)), dtype=f32)
    d["norm_g1"] = np.ascontiguousarray(np.broadcast_to(inp["norm_g"][1], (128, D)), dtype=f32)
    d["norm_gf"] = np.ascontiguousarray(np.broadcast_to(inp["final_norm_g"], (128, D)), dtype=f32)
    d["od_w_in"] = np.ascontiguousarray(inp["od_w_in"][0], dtype=f32)
    d["gla_wab"] = np.ascontiguousarray(np.concatenate([inp["gla_w_alpha"][0], inp["gla_b_alpha"][0][None, :]], 0), dtype=f32)
    d["gla_head_g"] = np.ascontiguousarray(inp["gla_head_g"][0].reshape(16, 128).T, dtype=f32)
    d["od_w_out"] = np.ascontiguousarray(inp["od_w_out"][0], dtype=f32)
    d["ev_w_in"] = np.ascontiguousarray(inp["ev_w_in"][0], dtype=f32)
    d["ev_w_out"] = np.ascontiguousarray(inp["ev_w_out"][0], dtype=f32)
    ifb = np.concatenate([inp["ev_i_bias"][0], inp["ev_f_bias"][0]])
    d["ev_if_bias"] = np.ascontiguousarray(np.broadcast_to(np.tile(ifb, 16), (128, 128)), dtype=f32)
    d["ev_conv_wT"] = np.ascontiguousarray(inp["ev_conv_w"][0].T, dtype=f32)
    d["ev_conv_bc"] = np.ascontiguousarray(inp["ev_conv_b"][0].reshape(8, 128).T, dtype=f32)
    d["ev_head_gc"] = np.ascontiguousarray(inp["ev_head_g"][0].reshape(8, 128).T, dtype=f32)

    def pair(a):
        a = a.reshape((32, 2, 64) + a.shape[2:])
        return np.ascontiguousarray(np.moveaxis(a, 0, 2).reshape((128, 32) + a.shape[3:]), dtype=f32)

    d["s5_lr"] = pair(inp["s5_lam_re"][0])
    d["s5_li"] = pair(inp["s5_lam_im"][0])
    d["s5_ldt"] = pair(np.broadcast_to(inp["s5_log_dt"][0][:, None], (64, 64)))
    d["s5_br"] = pair(inp["s5_b_re"][0])
    d["s5_bi"] = pair(inp["s5_b_im"][0])
    d["s5_cr"] = pair(np.swapaxes(inp["s5_c_re"][0], 1, 2))
    d["s5_ci"] = pair(np.swapaxes(inp["s5_c_im"][0], 1, 2))
    sig = np.arange(128) // 16
    ii = np.arange(128) % 16
    d["s5_maskD"] = (sig[:, None] + sig[None, :] >= 7).astype(f32)
    d["s5_P0"] = ((sig[:, None] + sig[None, :] == 7) & (ii[:, None] == ii[None, :])).astype(f32)
    d["s5_dcol"] = np.ascontiguousarray(np.tile(inp["s5_d"][0].reshape(64, 16).T, (8, 1)), dtype=f32)
    d["s5_glu_w"] = np.ascontiguousarray(inp["s5_glu_w"][0], dtype=f32)
    d["s5_glu_bc"] = np.ascontiguousarray(inp["s5_glu_b"][0].reshape(8, 128).T, dtype=f32)
    return d


def kernel(**inputs):
    inp = {k: np.asarray(v) for k, v in inputs.items()}
    nc = build_program()
    in_maps = [host_inputs(inp, b) for b in range(8)]
    res = run_bass_kernel_spmd(nc, in_maps, core_ids=list(range(8)))
    return np.stack([np.asarray(r["out"], dtype=np.float32) for r in res.results], 0)
```
